# Optimizing a Trainium2 kernel written in Bass

```python
import math
import jax, jax.numpy as jnp
from jax import lax
import numpy as np

D_MODEL = 1024
BATCH = 4
SEQ = 8192
DEPTH = 2

F32 = jnp.float32
GRID_W = 64
CTX_LEN = 256
EPS = 1e-6
POOL_WIDTH = 256
POOL_WINDOWS = (2, 4, 8, 16)
POOL_GROUPS = len(POOL_WINDOWS)
POOL_GROUP_DIM = POOL_WIDTH // POOL_GROUPS
HY_WIDTH = 256
HY_ORDER = 2
HY_IN = (HY_ORDER + 1) * HY_WIDTH
HY_BANDS = 8
HY_EMB = 1 + 2 * HY_BANDS
HY_FILTER_HIDDEN = 64
HY_MIN_DECAY = math.log(1e-2) / 1.5
HY_MAX_DECAY = math.log(1e-2) / 0.3
MLA_HEADS = 8
MLA_Q_RANK = 256
MLA_KV_RANK = 128
MLA_NOPE = 64
MLA_ROPE = 32
MLA_V = 64
MLA_QK = MLA_NOPE + MLA_ROPE
MLA_WIDTH = MLA_HEADS * MLA_V
ATTN_SCALE = MLA_QK ** -0.5
MIX_WIDTH = POOL_WIDTH + HY_WIDTH + MLA_WIDTH
OFF_HY = POOL_WIDTH
OFF_Q = OFF_HY + HY_IN
OFF_KV = OFF_Q + MLA_Q_RANK
OFF_KR = OFF_KV + MLA_KV_RANK
N_IN = OFF_KR + MLA_ROPE
ROPE_BASE = 10000.0
ROPE_AXIS_PAIRS = MLA_ROPE // 4
Q_BLOCK = 128
N_EXPERTS = 32
TOP_K = 4
D_EXPERT = 1024
SWIGLU_LIMIT = 7.0
SWIGLU_ALPHA = 1.702
EXPERT_BLOCK = 512

kernel_name = "hybrid_pool_hyena_mla_moe_dit"


def rms_norm(x, g):
    xf = x.astype(F32)
    y = xf * lax.rsqrt(jnp.mean(xf * xf, axis=-1, keepdims=True) + EPS)
    return (y * g.astype(F32)).astype(x.dtype)


def axial_rope(n_rows):
    row = jnp.repeat(jnp.arange(n_rows, dtype=F32), GRID_W)
    col = jnp.tile(jnp.arange(GRID_W, dtype=F32), n_rows)
    inv = ROPE_BASE ** (-jnp.arange(ROPE_AXIS_PAIRS, dtype=F32) / ROPE_AXIS_PAIRS)
    ang = jnp.concatenate([row[:, None] * inv, col[:, None] * inv], axis=-1)
    return jnp.cos(ang), jnp.sin(ang)


def apply_rope(x, cos, sin):
    half = MLA_ROPE // 2
    x1 = x[..., :half].astype(F32)
    x2 = x[..., half:].astype(F32)
    c = cos[None, :, None, :]
    s = sin[None, :, None, :]
    return jnp.concatenate([x1 * c - x2 * s, x1 * s + x2 * c], axis=-1).astype(x.dtype)


def pool_mixer(u, pool_w, pool_scale):
    B, L, _ = u.shape
    uf = u.astype(F32)
    cs = jnp.concatenate([jnp.zeros((B, 1, POOL_WIDTH), F32), jnp.cumsum(uf, axis=1)], axis=1)
    t = jnp.arange(L)
    diffs = []
    for gi, w in enumerate(POOL_WINDOWS):
        lo = jnp.clip(t - w // 2, 0, L)
        hi = jnp.clip(t + w // 2, 0, L)
        sl = slice(gi * POOL_GROUP_DIM, (gi + 1) * POOL_GROUP_DIM)
        csg = cs[:, :, sl]
        mean = (csg[:, hi] - csg[:, lo]) / (hi - lo).astype(F32)[None, :, None]
        diffs.append(mean - uf[:, :, sl])
    d = jnp.concatenate(diffs, axis=-1).astype(u.dtype).reshape(B, L, POOL_GROUPS, POOL_GROUP_DIM)
    y = jnp.einsum('blgc,gcd->blgd', d, pool_w).reshape(B, L, POOL_WIDTH)
    return y * pool_scale


def short_conv(u, w, b):
    L = u.shape[1]
    up = jnp.pad(u, ((0, 0), (1, 1), (0, 0)))
    return up[:, :L] * w[0] + up[:, 1:L + 1] * w[1] + up[:, 2:L + 2] * w[2] + b


def hyena_filter(L, w1, b1, w2, b2, w3, freq):
    t = jnp.linspace(0.0, 1.0, L, dtype=F32)[:, None]
    wpos = (2.0 * math.pi / L) * jnp.arange(L, dtype=F32)[:, None]
    f = jnp.linspace(1e-4, HY_BANDS - 1, HY_BANDS, dtype=F32)[None, :]
    z = jnp.concatenate([t, jnp.cos(f * wpos), -jnp.sin(f * wpos)], axis=-1)
    h = jnp.sin(freq * (z @ w1 + b1))
    h = jnp.sin(freq * (h @ w2 + b2))
    h = (h @ w3).astype(F32)
    deltas = jnp.abs(jnp.linspace(HY_MIN_DECAY, HY_MAX_DECAY, HY_WIDTH, dtype=F32))
    decay = jnp.exp(-t * deltas[None, :])
    h_fwd = h[:, :HY_WIDTH] * decay
    h_bwd = h[:, HY_WIDTH:] * decay
    return jnp.concatenate([h_fwd, jnp.zeros((1, HY_WIDTH), F32), h_bwd[1:][::-1]], axis=0)


def bidir_fftconv(u, k2, bias):
    L = u.shape[1]
    uf = u.astype(F32)
    U = jnp.fft.rfft(uf, n=2 * L, axis=1)
    K = jnp.fft.rfft(k2, n=2 * L, axis=0)
    y = jnp.fft.irfft(U * K[None], n=2 * L, axis=1)[:, :L]
    return (y + uf * bias.astype(F32)).astype(u.dtype)


def hyena_mixer(u, lp):
    L = u.shape[1]
    uc = short_conv(u, lp['hy_conv_w'], lp['hy_conv_b'])
    x0 = uc[..., :HY_WIDTH]
    x1 = uc[..., HY_WIDTH:2 * HY_WIDTH]
    v = uc[..., 2 * HY_WIDTH:]
    k2 = hyena_filter(L, lp['hy_f_w1'], lp['hy_f_b1'], lp['hy_f_w2'], lp['hy_f_b2'], lp['hy_f_w3'], lp['hy_freq'])
    return x0 * bidir_fftconv(x1 * v, k2, lp['hy_bias'])


def mla_queries(p, lp, rope):
    B, L, _ = p.shape
    cq = rms_norm(p[..., OFF_Q:OFF_KV], lp['mla_q_norm_g'])
    q = (cq @ lp['mla_w_uq']).reshape(B, L, MLA_HEADS, MLA_QK)
    q = rms_norm(q, lp['qk_norm_q'])
    if rope is not None:
        q = jnp.concatenate([q[..., :MLA_NOPE], apply_rope(q[..., MLA_NOPE:], rope[0], rope[1])], axis=-1)
    return q


def mla_keys_values(p, lp, rope):
    B, L, _ = p.shape
    ckv = rms_norm(p[..., OFF_KV:OFF_KR], lp['mla_kv_norm_g'])
    kv = (ckv @ lp['mla_w_ukv']).reshape(B, L, MLA_HEADS, MLA_NOPE + MLA_V)
    k_rope = jnp.broadcast_to(p[..., None, OFF_KR:N_IN], (B, L, MLA_HEADS, MLA_ROPE))
    k = rms_norm(jnp.concatenate([kv[..., :MLA_NOPE], k_rope], axis=-1), lp['qk_norm_k'])
    if rope is not None:
        k = jnp.concatenate([k[..., :MLA_NOPE], apply_rope(k[..., MLA_NOPE:], rope[0], rope[1])], axis=-1)
    return k, kv[..., MLA_NOPE:]


def softmax_attention(q, k, v):
    s = jnp.einsum('bqhd,bkhd->bhqk', q, k).astype(F32) * ATTN_SCALE
    pr = jax.nn.softmax(s, axis=-1).astype(v.dtype)
    return jnp.einsum('bhqk,bkhd->bqhd', pr, v)


def latent_attention(q, k_all, v_all):
    B, S, H, dk = q.shape
    nb = S // Q_BLOCK
    qb = q.reshape(B, nb, Q_BLOCK, H, dk).swapaxes(0, 1)
    o = lax.map(lambda qblk: softmax_attention(qblk, k_all, v_all), qb)
    return o.swapaxes(0, 1).reshape(B, S, MLA_WIDTH)


def expert_ffn(h, router_w, router_b, w1, b1, w2, b2):
    n_tok, d = h.shape
    logits = h.astype(F32) @ router_w.astype(F32) + router_b.astype(F32)
    top_v, top_i = lax.top_k(logits, TOP_K)
    gates = jax.nn.softmax(top_v, axis=-1)
    n_slots = n_tok * TOP_K
    flat_e = top_i.reshape(n_slots).astype(jnp.int32)
    flat_g = gates.reshape(n_slots)
    flat_tok = jnp.arange(n_slots, dtype=jnp.int32) // TOP_K
    order = jnp.argsort(flat_e)
    sorted_e = flat_e[order]
    counts = jnp.bincount(flat_e, length=N_EXPERTS).astype(jnp.int32)
    padded = (counts + EXPERT_BLOCK - 1) // EXPERT_BLOCK * EXPERT_BLOCK
    padded_end = jnp.cumsum(padded)
    padded_start = padded_end - padded
    start = jnp.cumsum(counts) - counts
    dest = padded_start[sorted_e] + jnp.arange(n_slots, dtype=jnp.int32) - start[sorted_e]
    n_blocks = -(-n_slots // EXPERT_BLOCK) + N_EXPERTS
    n_buf = n_blocks * EXPERT_BLOCK
    buf_tok = jnp.full((n_buf,), n_tok, jnp.int32).at[dest].set(flat_tok[order])
    buf_g = jnp.zeros((n_buf,), F32).at[dest].set(flat_g[order])
    block_start = jnp.arange(n_blocks, dtype=jnp.int32) * EXPERT_BLOCK
    block_e = jnp.minimum(jnp.searchsorted(padded_end, block_start, side='right'), N_EXPERTS - 1).astype(jnp.int32)
    h_pad = jnp.concatenate([h, jnp.zeros((1, d), h.dtype)], axis=0)

    def block_step(out, blk):
        tok, g, e = blk
        a = h_pad[tok] @ w1[e] + b1[e]
        a_glu = jnp.minimum(a[:, :D_EXPERT], SWIGLU_LIMIT)
        a_lin = jnp.clip(a[:, D_EXPERT:], -SWIGLU_LIMIT, SWIGLU_LIMIT)
        act = a_glu * jax.nn.sigmoid(SWIGLU_ALPHA * a_glu) * (a_lin + 1.0)
        y = act @ w2[e] + b2[e]
        return out.at[tok].add((g[:, None] * y).astype(out.dtype)), None

    out, _ = lax.scan(block_step, jnp.zeros((n_tok + 1, d), h.dtype),
                      (buf_tok.reshape(n_blocks, EXPERT_BLOCK), buf_g.reshape(n_blocks, EXPERT_BLOCK), block_e))
    return out[:n_tok]


def mixer_outputs(p, lp):
    pool_out = pool_mixer(p[..., :OFF_HY], lp['pool_w'], lp['pool_scale'])
    hy_out = hyena_mixer(p[..., OFF_HY:OFF_Q], lp)
    return pool_out, hy_out


def hybrid_layer(x, xc, c, c_ctx, lp, rope, last):
    B, S, D = x.shape
    n_ctx = xc.shape[1]
    mod = jax.nn.silu(c) @ lp['w_mod'] + lp['b_mod']
    mod_c = jax.nn.silu(c_ctx) @ lp['w_mod'] + lp['b_mod']
    sh1, sc1, g1, sh2, sc2, g2 = jnp.split(mod[:, None, :], 6, axis=-1)
    csh1, csc1, cg1, csh2, csc2, cg2 = jnp.split(mod_c, 6, axis=-1)

    h = rms_norm(x, lp['norm1_g']) * (1.0 + sc1) + sh1
    hc = rms_norm(xc, lp['norm1_g']) * (1.0 + csc1) + csh1
    p = h @ lp['w_in']
    pc = hc @ lp['w_in']
    kc, vc = mla_keys_values(pc, lp, None)
    kl, vl = mla_keys_values(p, lp, rope)
    ql = mla_queries(p, lp, rope)
    attn = latent_attention(ql, jnp.concatenate([kc, kl], axis=1), jnp.concatenate([vc, vl], axis=1))
    pool_l, hy_l = mixer_outputs(p, lp)
    x = x + g1 * (jnp.concatenate([pool_l, hy_l, attn], axis=-1) @ lp['w_out'])
    if not last:
        qc = mla_queries(pc, lp, None)
        attn_c = softmax_attention(qc, kc, vc).reshape(B, n_ctx, MLA_WIDTH)
        pool_c, hy_c = mixer_outputs(pc, lp)
        xc = xc + cg1 * (jnp.concatenate([pool_c, hy_c, attn_c], axis=-1) @ lp['w_out'])

    h2 = rms_norm(x, lp['norm2_g']) * (1.0 + sc2) + sh2
    moe_args = (lp['router_w'], lp['router_b'], lp['moe_w1'], lp['moe_b1'], lp['moe_w2'], lp['moe_b2'])
    if last:
        y = expert_ffn(h2.reshape(B * S, D), *moe_args)
        x = x + g2 * y.reshape(B, S, D)
    else:
        h2c = rms_norm(xc, lp['norm2_g']) * (1.0 + csc2) + csh2
        n_c = B * n_ctx
        y = expert_ffn(jnp.concatenate([h2c.reshape(n_c, D), h2.reshape(B * S, D)], axis=0), *moe_args)
        xc = xc + cg2 * y[:n_c].reshape(B, n_ctx, D)
        x = x + g2 * y[n_c:].reshape(B, S, D)
    return x, xc


def setup_inputs(seed: int = 0) -> dict:
    key = jax.random.key(seed)
    ks = iter(jax.random.split(key, 40))

    def nrm(shape, scale):
        return jax.random.normal(next(ks), shape, F32) * scale

    D = D_MODEL
    L = DEPTH
    return {
        'x': nrm((BATCH, SEQ, D), 1.0),
        'c': nrm((BATCH, D), 1.0),
        'ctx': nrm((BATCH, CTX_LEN, D), 1.0),
        'c_ctx': nrm((D,), 1.0),
        'norm1_g': 1.0 + nrm((L, D), 0.02),
        'norm2_g': 1.0 + nrm((L, D), 0.02),
        'w_mod': nrm((L, D, 6 * D), 0.5 * D ** -0.5),
        'b_mod': nrm((L, 6 * D), 0.02),
        'w_in': nrm((L, D, N_IN), D ** -0.5),
        'pool_w': nrm((L, POOL_GROUPS, POOL_GROUP_DIM, POOL_GROUP_DIM), POOL_GROUP_DIM ** -0.5),
        'pool_scale': 1.0 + nrm((L, POOL_WIDTH), 0.02),
        'hy_conv_w': nrm((L, 3, HY_IN), 3 ** -0.5),
        'hy_conv_b': nrm((L, HY_IN), 0.02),
        'hy_f_w1': nrm((L, HY_EMB, HY_FILTER_HIDDEN), HY_EMB ** -0.5),
        'hy_f_b1': nrm((L, HY_FILTER_HIDDEN), 0.02),
        'hy_f_w2': nrm((L, HY_FILTER_HIDDEN, HY_FILTER_HIDDEN), HY_FILTER_HIDDEN ** -0.5),
        'hy_f_b2': nrm((L, HY_FILTER_HIDDEN), 0.02),
        'hy_f_w3': nrm((L, HY_FILTER_HIDDEN, 2 * HY_WIDTH), 0.05 * HY_FILTER_HIDDEN ** -0.5),
        'hy_freq': 1.0 + nrm((L, HY_FILTER_HIDDEN), 0.02),
        'hy_bias': nrm((L, HY_WIDTH), 0.1),
        'mla_q_norm_g': 1.0 + nrm((L, MLA_Q_RANK), 0.02),
        'mla_w_uq': nrm((L, MLA_Q_RANK, MLA_HEADS * MLA_QK), MLA_Q_RANK ** -0.5),
        'mla_kv_norm_g': 1.0 + nrm((L, MLA_KV_RANK), 0.02),
        'mla_w_ukv': nrm((L, MLA_KV_RANK, MLA_HEADS * (MLA_NOPE + MLA_V)), MLA_KV_RANK ** -0.5),
        'qk_norm_q': 1.0 + nrm((L, MLA_QK), 0.02),
        'qk_norm_k': 1.0 + nrm((L, MLA_QK), 0.02),
        'w_out': nrm((L, MIX_WIDTH, D), MIX_WIDTH ** -0.5),
        'router_w': nrm((L, D, N_EXPERTS), D ** -0.5),
        'router_b': nrm((L, N_EXPERTS), 0.01),
        'moe_w1': nrm((L, N_EXPERTS, D, 2 * D_EXPERT), D ** -0.5),
        'moe_b1': nrm((L, N_EXPERTS, 2 * D_EXPERT), 0.02),
        'moe_w2': nrm((L, N_EXPERTS, D_EXPERT, D), D_EXPERT ** -0.5),
        'moe_b2': nrm((L, N_EXPERTS, D), 0.02),
    }


def reference(x, c, ctx, c_ctx, norm1_g, norm2_g, w_mod, b_mod, w_in, pool_w, pool_scale,
              hy_conv_w, hy_conv_b, hy_f_w1, hy_f_b1, hy_f_w2, hy_f_b2, hy_f_w3, hy_freq, hy_bias,
              mla_q_norm_g, mla_w_uq, mla_kv_norm_g, mla_w_ukv, qk_norm_q, qk_norm_k, w_out,
              router_w, router_b, moe_w1, moe_b1, moe_w2, moe_b2):
    n_rows = x.shape[1] // GRID_W
    rope = axial_rope(n_rows)
    xc = ctx
    for l in range(DEPTH):
        lp = {
            'norm1_g': norm1_g[l], 'norm2_g': norm2_g[l], 'w_mod': w_mod[l], 'b_mod': b_mod[l],
            'w_in': w_in[l], 'pool_w': pool_w[l], 'pool_scale': pool_scale[l],
            'hy_conv_w': hy_conv_w[l], 'hy_conv_b': hy_conv_b[l], 'hy_f_w1': hy_f_w1[l], 'hy_f_b1': hy_f_b1[l],
            'hy_f_w2': hy_f_w2[l], 'hy_f_b2': hy_f_b2[l], 'hy_f_w3': hy_f_w3[l], 'hy_freq': hy_freq[l],
            'hy_bias': hy_bias[l], 'mla_q_norm_g': mla_q_norm_g[l], 'mla_w_uq': mla_w_uq[l],
            'mla_kv_norm_g': mla_kv_norm_g[l], 'mla_w_ukv': mla_w_ukv[l], 'qk_norm_q': qk_norm_q[l],
            'qk_norm_k': qk_norm_k[l], 'w_out': w_out[l], 'router_w': router_w[l], 'router_b': router_b[l],
            'moe_w1': moe_w1[l], 'moe_b1': moe_b1[l], 'moe_w2': moe_w2[l], 'moe_b2': moe_b2[l],
        }
        x, xc = hybrid_layer(x, xc, c, c_ctx, lp, rope, l == DEPTH - 1)
    return x
```

```python
import math
from contextlib import ExitStack
import numpy as np
import ml_dtypes
import concourse.bass as bass
import concourse.mybir as mybir
from concourse.bass_utils import run_bass_kernel_spmd

F32 = mybir.dt.float32
BF16 = mybir.dt.bfloat16
ALU = mybir.AluOpType
AF = mybir.ActivationFunctionType
AX = mybir.AxisListType

D = 1024
CT = 256
NE = 32
NPT = 1472
EPS = 1e-6
GRID_W = 64
POOL_WINDOWS = (2, 4, 8, 16)
HY_MIN_DECAY = math.log(1e-2) / 1.5
HY_MAX_DECAY = math.log(1e-2) / 0.3
ATTN_SCALE = 96 ** -0.5
SIG_CLAMP = float(1.0 / (1.0 + math.exp(-1.702 * 7.0)))

VCOLS = {}
_o = 0
for _n, _w in [("n1g", 8), ("n2g", 8), ("bmod", 48), ("c", 8), ("cctx", 8), ("psc", 2), ("hcw", 18), ("hcb", 6),
               ("hyb", 2), ("qng", 2), ("kvng", 1), ("gqm", 1), ("gqs", 1), ("gkm", 1), ("gks", 1),
               ("fb1", 1), ("fb2", 1), ("freq", 1), ("b1", 512)]:
    VCOLS[_n] = (_o, _w)
    _o += _w
NV = _o


class Buf:
    __slots__ = ("w", "r")

    def __init__(self):
        self.w = {}
        self.r = {}


class Prog:
    NS = 32

    def __init__(self, nc, es):
        self.nc = nc
        self.E = {"pe": nc.tensor, "act": nc.scalar, "dve": nc.vector, "pool": nc.gpsimd, "sp": nc.sync}
        self.sem = {k: es.enter_context(nc.semaphore("s_" + k)) for k in self.E}
        self.cnt = {k: 0 for k in self.E}
        self.dsem = [es.enter_context(nc.semaphore("d%d" % i)) for i in range(self.NS)]
        self.dtot = [0] * self.NS
        self.dn = 0
        self.seen = {k: {} for k in self.E}

    def _wait(self, e, deps):
        for key, n in deps.items():
            if n <= 0 or (key == ("e", "pe") and e == "pe"):
                continue
            if self.seen[e].get(key, 0) >= n:
                continue
            sem = self.sem[key[1]] if key[0] == "e" else self.dsem[key[1]]
            self.E[e].wait_ge(sem, n)
            self.seen[e][key] = n

    @staticmethod
    def _deps(R, W, Wd):
        d = {}
        for b in R:
            for k, n in b.w.items():
                d[k] = max(d.get(k, 0), n)
        for b in W:
            for k, n in b.w.items():
                d[k] = max(d.get(k, 0), n)
            for k, n in b.r.items():
                d[k] = max(d.get(k, 0), n)
        for b in Wd:
            for k, n in b.r.items():
                d[k] = max(d.get(k, 0), n)
        return d

    @staticmethod
    def _commit(key, n, R, W, Wd):
        for b in R:
            b.r[key] = max(b.r.get(key, 0), n)
        for b in W:
            b.w = {key: n}
            b.r = {}
        for b in Wd:
            b.w[key] = max(b.w.get(key, 0), n)

    def op(self, e, fn, R=(), W=(), Wd=()):
        self._wait(e, self._deps(R, W, Wd))
        ins = fn(self.E[e])
        self.cnt[e] += 1
        ins.then_inc(self.sem[e], 1)
        self._commit(("e", e), self.cnt[e], R, W, Wd)

    def mm(self, items, R=(), W=(), Wd=()):
        self._wait("pe", self._deps(R, W, Wd))
        ins = None
        for (o, l, r, st, sp) in items:
            ins = self.nc.tensor.matmul(o, l, r, start=st, stop=sp)
        self.cnt["pe"] += 1
        ins.then_inc(self.sem["pe"], 1)
        self._commit(("e", "pe"), self.cnt["pe"], R, W, Wd)

    def dma(self, e, out, in_, R=(), W=(), Wd=(), **kw):
        i = self.dn
        self.dn = (i + 1) % self.NS
        d = self._deps(R, W, Wd)
        if self.dtot[i]:
            d[("d", i)] = max(d.get(("d", i), 0), self.dtot[i])
        self._wait(e, d)
        ins = self.E[e].dma_start(out=out, in_=in_, **kw)
        self.dtot[i] += 16
        ins.then_inc(self.dsem[i], 16)
        self._commit(("d", i), self.dtot[i], R, W, Wd)

    def barrier(self):
        for e in self.E:
            d = {}
            for k in self.E:
                if k != e and self.cnt[k]:
                    d[("e", k)] = self.cnt[k]
            for i in range(self.NS):
                if self.dtot[i]:
                    d[("d", i)] = self.dtot[i]
            self._wait(e, d)

    def finish(self):
        for i in range(self.NS):
            if self.dtot[i]:
                self.nc.sync.wait_ge(self.dsem[i], self.dtot[i])
        for k in self.E:
            if k != "sp" and self.cnt[k]:
                self.nc.sync.wait_ge(self.sem[k], self.cnt[k])


def build(S, NL, dbg=False):
    T = S + CT
    NLC = S // 512
    chunks = [(i * 512, 512, 0) for i in range(NLC)] + [(S, CT, 1)]
    PTW = S + 24 + CT + 8
    RL = 2 * S + 512
    RC = 2 * CT + 512
    nc = bass.Bass("TRN2", target_bir_lowering=False)
    es = ExitStack()
    P = Prog(nc, es)

    def din(name, shape, dt=F32):
        return nc.dram_tensor(name, list(shape), dt, kind="ExternalInput")

    def dscr(name, shape, dt):
        return nc.dram_tensor(name, list(shape), dt, kind="ExternalOutput" if dbg else "Internal")

    x_in = din("x", [S, D])
    ctx_in = din("ctx", [CT, D])
    vecs = din("vecs", [NL, 128, NV])
    w_mod = din("w_mod", [NL, D, 6 * D])
    w_in = din("w_in", [NL, D, 1440])
    pool_w = din("pool_w", [NL, 4, 64, 64])
    f_w1 = din("hy_f_w1", [NL, 17, 64])
    f_w2 = din("hy_f_w2", [NL, 64, 64])
    f_w3 = din("hy_f_w3", [NL, 64, 512])
    w_uq = din("mla_w_uq", [NL, 256, 768])
    w_ukv = din("mla_w_ukv", [NL, 128, 1024])
    w_out = din("w_out", [NL, D, D])
    r_w = din("router_w", [NL, D, NE])
    r_b = din("router_b", [NL, 1, NE])
    m_w1 = din("moe_w1", [NL, NE, D, 2 * D])
    m_w2 = din("moe_w2", [NL, NE, D, D])
    m_b2 = din("moe_b2", [NL, NE, D])
    c_idf = din("c_idf", [128, 128])
    c_idb = din("c_idb", [128, 128], BF16)
    c_anti = din("c_anti", [128, 128], BF16)
    c_cos = din("c_cos", [32, T])
    c_sin = din("c_sin", [32, T])
    c_invc = din("c_invc", [128, 2, T])
    c_zl = din("c_zl", [17, RL])
    c_dfl = din("c_dfl", [256, RL])
    c_dbl = din("c_dbl", [256, RL])
    c_zc = din("c_zc", [17, RC])
    c_dfc = din("c_dfc", [256, RC])
    c_dbc = din("c_dbc", [256, RC])
    y_out = nc.dram_tensor("y", [S, D], F32, kind="ExternalOutput")

    XT = dscr("XT", [8, 128, T], F32)
    PT = dscr("PT", [NPT, PTW], BF16)
    X0C = dscr("X0C", [256, T], BF16)
    KDL = dscr("KDL", [256, RL], BF16)
    KDC = dscr("KDC", [256, RC], BF16)
    KT = dscr("KT", [8, 128, T], BF16)
    QT = dscr("QT", [8, 128, T], BF16)
    VT = dscr("VT", [8, T, 64], BF16)
    MIXT = dscr("MIXT", [D, T], BF16)
    H2T = dscr("H2T", [D, T], BF16)
    GT = dscr("GT", [NE, T], F32)
    bXT, bPT, bX0C, bKDL, bKDC, bKT, bQT, bVT, bMIXT, bH2T, bGT = [Buf() for _ in range(11)]

    XTv = XT.ap().rearrange("j p t -> p j t")

    uid = [0]

    class phase:
        def __enter__(self_):
            self_.st = ExitStack()
            return self_.st

        def __exit__(self_, *a):
            if a[0] is None:
                P.barrier()
            self_.st.close()
            return False

    def sb(st, name, shape, dt=F32):
        uid[0] += 1
        return st.enter_context(nc.sbuf_tensor("%s_%d" % (name, uid[0]), list(shape), dt))

    def ps(st, name, shape, dt=F32):
        uid[0] += 1
        return st.enter_context(nc.psum_tensor("%s_%d" % (name, uid[0]), list(shape), dt))

    def pcol(seg):
        return 8 if seg == 0 else S + 24

    idf = sb(es, "idf", [128, 128])
    idb = sb(es, "idb", [128, 128], BF16)
    anti = sb(es, "anti", [128, 128], BF16)
    onesb = sb(es, "onesb", [128, 128], BF16)
    onesf = sb(es, "onesf", [128, 128])
    vec = sb(es, "vec", [128, NL, NV])
    bconst = Buf()
    P.dma("sp", idf[:], c_idf.ap(), W=[bconst])
    P.dma("sp", idb[:], c_idb.ap(), Wd=[bconst])
    P.dma("sp", anti[:], c_anti.ap(), Wd=[bconst])
    P.dma("sp", vec[:], vecs.ap().rearrange("l p v -> p l v"), Wd=[bconst])
    P.op("dve", lambda e: e.memset(onesb[:], 1.0), Wd=[bconst])
    P.op("dve", lambda e: e.memset(onesf[:], 1.0), Wd=[bconst])

    def V(l, name, j=0, n=1):
        o, w = VCOLS[name]
        return vec[:, l, o + j:o + j + n]

    with phase() as st:
        zt = sb(st, "zt", [128, 16], BF16)
        bz = Buf()
        P.op("dve", lambda e: e.memset(zt[:], 0.0), W=[bz])
        for r0 in range(0, NPT, 128):
            nr = min(128, NPT - r0)
            for (c0, w) in [(0, 8), (8 + S, 16), (S + 24 + CT, 8)]:
                P.dma("sp", PT.ap()[r0:r0 + nr, c0:c0 + w], zt[0:nr, 0:w], R=[bz], Wd=[bPT])
        xin = [sb(st, "xin%d" % i, [128, D]) for i in range(2)]
        xtt = [sb(st, "xtt%d" % i, [128, 8, 128]) for i in range(2)]
        pst = [ps(st, "pst%d" % i, [128, 8, 128]) for i in range(2)]
        bxin = [Buf(), Buf()]
        bxtt = [Buf(), Buf()]
        bpst = [Buf(), Buf()]
        for ti in range(T // 128):
            k = ti % 2
            src = x_in.ap()[ti * 128:(ti + 1) * 128, :] if ti * 128 < S else ctx_in.ap()[ti * 128 - S:(ti + 1) * 128 - S, :]
            P.dma("sp", xin[k][:], src, W=[bxin[k]])
            P._wait("pe", P._deps([bxin[k], bconst], [bpst[k]], []))
            ins = None
            for j in range(8):
                ins = nc.tensor.transpose(pst[k][:, j, :], xin[k][:, j * 128:(j + 1) * 128], idf[:])
            P.cnt["pe"] += 1
            ins.then_inc(P.sem["pe"], 1)
            P._commit(("e", "pe"), P.cnt["pe"], [bxin[k], bconst], [bpst[k]], [])
            P.op("act" if ti % 2 else "dve",
                 (lambda e, k=k: e.activation(out=xtt[k][:], in_=pst[k][:], func=AF.Copy)) if ti % 2 else
                 (lambda e, k=k: e.tensor_copy(out=xtt[k][:], in_=pst[k][:])), R=[bpst[k]], W=[bxtt[k]])
            P.dma("sp", XTv[:, :, ti * 128:(ti + 1) * 128], xtt[k][:], R=[bxtt[k]], Wd=[bXT])

    for l in range(NL):
        last = (l == NL - 1)
        modst = ExitStack()
        sT = sb(modst, "sT", [128, 8, 2])
        modT = sb(modst, "modT", [128, 48, 2])
        A1 = sb(modst, "A1", [128, 8, 2])
        A2 = sb(modst, "A2", [128, 8, 2])
        bmodv = Buf()
        with phase() as st:
            wm = [sb(st, "wm%d" % i, [128, 8, 768]) for i in range(2)]
            bwm = [Buf(), Buf()]
            psm = ps(st, "psm", [128, 48, 2])
            bpsm = Buf()
            bsT = Buf()
            P.op("act", lambda e: e.activation(out=sT[:, :, 0], in_=V(l, "c", 0, 8), func=AF.Silu), R=[bconst], W=[bsT])
            P.op("act", lambda e: e.activation(out=sT[:, :, 1], in_=V(l, "cctx", 0, 8), func=AF.Silu), R=[bconst], W=[bsT])
            for s in range(8):
                k = s % 2
                P.dma("sp", wm[k][:], w_mod.ap()[l, :, s * 768:(s + 1) * 768].rearrange("(kc p) n -> p kc n", p=128),
                      W=[bwm[k]])
                items = []
                for oc in range(6):
                    for kc in range(8):
                        items.append((psm[:, s * 6 + oc, :], wm[k][:, kc, oc * 128:(oc + 1) * 128], sT[:, kc, :],
                                      kc == 0, kc == 7))
                P.mm(items, R=[bwm[k], bsT], Wd=[bpsm])
            for i in range(2):
                P.op("dve", lambda e, i=i: e.tensor_tensor(out=modT[:, :, i], in0=psm[:, :, i], in1=V(l, "bmod", 0, 48),
                                                          op=ALU.add), R=[bpsm, bconst], W=[bmodv])
                P.op("dve", lambda e, i=i: e.scalar_tensor_tensor(out=A1[:, :, i], in0=modT[:, 8:16, i], scalar=1.0,
                                                                 in1=V(l, "n1g", 0, 8), op0=ALU.add, op1=ALU.mult),
                     R=[bmodv], W=[bmodv])
                P.op("dve", lambda e, i=i: e.scalar_tensor_tensor(out=A2[:, :, i], in0=modT[:, 32:40, i], scalar=1.0,
                                                                 in1=V(l, "n2g", 0, 8), op0=ALU.add, op1=ALU.mult),
                     R=[bmodv], W=[bmodv])

        def MOD(m, j, i):
            return modT[:, m * 8 + j, i:i + 1]

        def norm_chunk(st_tiles, t0, n, seg, Asel, shm, hT, bhT, hF=None):
            xt, sq, rstd, tmp, pss, bxt, bsq, bpss, brs, btmp = st_tiles
            P.dma("sp", xt[:, :, :n], XTv[:, :, t0:t0 + n], R=[bXT], W=[bxt])
            P.op("act", lambda e: e.activation(out=sq[:, :, :n], in_=xt[:, :, :n], func=AF.Square), R=[bxt], W=[bsq])
            P.mm([(pss[:, :n], onesb[:], sq[:, j, :n], j == 0, j == 7) for j in range(8)], R=[bsq, bconst], W=[bpss])
            P.op("act", lambda e: e.activation(out=rstd[:, :n], in_=pss[:, :n], func=AF.Sqrt, scale=1.0 / D, bias=EPS),
                 R=[bpss], W=[brs])
            P.op("dve", lambda e: e.reciprocal(out=rstd[:, :n], in_=rstd[:, :n]), W=[brs])
            for j in range(8):
                P.op("dve", lambda e, j=j: e.scalar_tensor_tensor(out=tmp[:, :n], in0=xt[:, j, :n],
                                                                 scalar=Asel[:, j, seg:seg + 1], in1=rstd[:, :n],
                                                                 op0=ALU.mult, op1=ALU.mult),
                     R=[bxt, brs, bmodv], W=[btmp])
                if hF is not None:
                    P.op("act", lambda e, j=j: e.activation(out=hF[:, j, :n], in_=tmp[:, :n], func=AF.Identity,
                                                           bias=MOD(shm, j, seg)), R=[btmp, bmodv], Wd=[bhT])
                    P.op("pool", lambda e, j=j: e.tensor_copy(out=hT[:, j, :n], in_=hF[:, j, :n]), R=[bhT], Wd=[bhT])
                else:
                    P.op("act", lambda e, j=j: e.activation(out=hT[:, j, :n], in_=tmp[:, :n], func=AF.Identity,
                                                           bias=MOD(shm, j, seg)), R=[btmp, bmodv], Wd=[bhT])

        def norm_tiles(st):
            return (sb(st, "n_xt", [128, 8, 512]), sb(st, "n_sq", [128, 8, 512], BF16), sb(st, "n_rstd", [128, 512]),
                    sb(st, "n_tmp", [128, 512]), ps(st, "n_pss", [128, 512]), Buf(), Buf(), Buf(), Buf(), Buf())

        with phase() as st:
            wi = sb(st, "wi", [128, 8, NPT], BF16)
            bwi = Buf()
            wsrc = w_in.ap()[l].rearrange("(kc p) n -> p kc n", p=128)
            P.dma("pool", wi[:, :, 0:1440], wsrc, Wd=[bwi])
            P.dma("pool", wi[:, :, 1440:1456], wsrc[:, :, 1424:1440], Wd=[bwi])
            P.dma("pool", wi[:, :, 1456:1472], wsrc[:, :, 1408:1424], Wd=[bwi])
            nt = norm_tiles(st)
            hT = sb(st, "hT", [128, 8, 512], BF16)
            bhT = Buf()
            ptsb = sb(st, "ptsb", [128, 12, 512], BF16)
            bptsb = Buf()
            pp = [ps(st, "pp%d" % i, [128, 512]) for i in range(2)]
            bpp = [Buf(), Buf()]
            for (t0, n, seg) in chunks:
                norm_chunk(nt, t0, n, seg, A1, 0, hT, bhT)
                for oc in range(12):
                    r0 = oc * 128
                    nr = min(128, NPT - r0)
                    k = oc % 2
                    P.mm([(pp[k][:nr, :n], wi[:, kc, r0:r0 + nr], hT[:, kc, :n], kc == 0, kc == 7) for kc in range(8)],
                         R=[bwi, bhT], W=[bpp[k]])
                    if k:
                        P.op("act", lambda e, k=k, nr=nr, oc=oc: e.activation(out=ptsb[:nr, oc, :n], in_=pp[k][:nr, :n],
                                                                             func=AF.Copy), R=[bpp[k]], Wd=[bptsb])
                    else:
                        P.op("dve", lambda e, k=k, nr=nr, oc=oc: e.tensor_copy(out=ptsb[:nr, oc, :n], in_=pp[k][:nr, :n]),
                             R=[bpp[k]], Wd=[bptsb])
                c0 = pcol(seg) + (t0 - (0 if seg == 0 else S))
                P.dma("sp", PT.ap()[0:1408, c0:c0 + n].rearrange("(oc p) t -> p oc t", p=128), ptsb[:, 0:11, :n],
                      R=[bptsb], Wd=[bPT])
                P.dma("sp", PT.ap()[1408:1472, c0:c0 + n], ptsb[0:64, 11, :n], R=[bptsb], Wd=[bPT])
                P._wait("act", P._deps([], [bptsb], []))
                P._wait("dve", P._deps([], [bptsb], []))
                bptsb.w = {}

        with phase() as st:
            pw = sb(st, "pw", [128, 2, 128], BF16)
            bpw = Buf()
            P.op("dve", lambda e: e.memset(pw[:], 0.0), W=[bpw])
            for g in range(4):
                p0 = (g % 2) * 64
                P.dma("pool", pw[p0:p0 + 64, g // 2, p0:p0 + 64], pool_w.ap()[l, g], Wd=[bpw])
            u = sb(st, "pl_u", [128, 2, 528], BF16)
            a = sb(st, "pl_a", [128, 528])
            b = sb(st, "pl_b", [128, 528])
            iv = sb(st, "pl_iv", [128, 2, 512])
            dd = sb(st, "pl_d", [128, 512], BF16)
            po = sb(st, "pl_o", [128, 2, 512], BF16)
            pps = ps(st, "pl_ps", [128, 512])
            bu, ba, bb, biv, bdd, bpo, bpps = [Buf() for _ in range(7)]
            for (t0, n, seg) in chunks:
                c0 = pcol(seg) + (t0 - (0 if seg == 0 else S))
                P.dma("sp", u[:, :, :n + 16], PT.ap()[0:256, c0 - 8:c0 + n + 8].rearrange("(cj p) t -> p cj t", p=128),
                      R=[bPT], W=[bu])
                P.dma("sp", iv[:, :, :n], c_invc.ap()[:, :, t0:t0 + n], W=[biv])
                for cj in range(2):
                    for hf in range(2):
                        g = cj * 2 + hf
                        w = POOL_WINDOWS[g]
                        pr = slice(hf * 64, hf * 64 + 64)
                        P.op("dve", lambda e, pr=pr, cj=cj: e.tensor_tensor(out=a[pr, 1:n + 16], in0=u[pr, cj, 0:n + 15],
                                                                          in1=u[pr, cj, 1:n + 16], op=ALU.add),
                             R=[bu], W=[ba])
                        cur, oth, bc_, bo_ = a, b, ba, bb
                        lo, hi = 1, n + 16
                        sh = 1
                        while sh * 2 < w:
                            nlo, nhi = lo + sh, hi - sh
                            P.op("dve", lambda e, pr=pr, cur=cur, oth=oth, nlo=nlo, nhi=nhi, sh=sh: e.tensor_tensor(
                                out=oth[pr, nlo:nhi], in0=cur[pr, nlo - sh:nhi - sh], in1=cur[pr, nlo + sh:nhi + sh],
                                op=ALU.add), R=[bc_], W=[bo_])
                            cur, oth, bc_, bo_ = oth, cur, bo_, bc_
                            lo, hi = nlo, nhi
                            sh *= 2
                        P.op("dve", lambda e, pr=pr, cur=cur, cj=cj: e.tensor_tensor(out=cur[pr, 8:8 + n], in0=cur[pr, 8:8 + n],
                                                                                   in1=iv[pr, cj, :n], op=ALU.mult),
                             R=[biv], W=[bc_])
                        P.op("dve", lambda e, pr=pr, cur=cur, cj=cj: e.tensor_tensor(out=dd[pr, :n], in0=cur[pr, 8:8 + n],
                                                                                   in1=u[pr, cj, 8:8 + n], op=ALU.subtract),
                             R=[bc_, bu], Wd=[bdd])
                    P.mm([(pps[:, :n], pw[:, cj, :], dd[:, :n], True, True)], R=[bpw, bdd], W=[bpps])
                    bdd.w = dict(bdd.w)
                    P.op("act", lambda e, cj=cj: e.activation(out=po[:, cj, :n], in_=pps[:, :n], func=AF.Copy,
                                                             scale=V(l, "psc", cj, 1)), R=[bpps, bconst], Wd=[bpo])
                    P._wait("dve", P._deps([], [bdd], []))
                    bdd.w = {}
                P.dma("sp", MIXT.ap()[0:256, t0:t0 + n].rearrange("(cj p) t -> p cj t", p=128), po[:, :, :n],
                      R=[bpo], Wd=[bMIXT])
                P._wait("act", P._deps([], [bpo], []))
                bpo.w = {}

        with phase() as st:
            fw1 = sb(st, "fw1", [17, 64])
            fw2 = sb(st, "fw2", [64, 64])
            fw3 = sb(st, "fw3", [64, 512])
            fsc = sb(st, "fsc", [64, 4])
            bfw = Buf()
            P.dma("sp", fw1[:], f_w1.ap()[l], Wd=[bfw])
            P.dma("sp", fw2[:], f_w2.ap()[l], Wd=[bfw])
            P.dma("sp", fw3[:], f_w3.ap()[l], Wd=[bfw])
            P.op("dve", lambda e: e.tensor_scalar(out=fsc[:, 0:1], in0=vec[0:64, l, VCOLS["freq"][0]:VCOLS["freq"][0] + 1],
                                                  scalar1=1.0 / 3.0, scalar2=None, op0=ALU.mult), R=[bconst], W=[bfw])
            P.op("dve", lambda e: e.tensor_tensor(out=fsc[:, 1:2], in0=fsc[:, 0:1],
                                                  in1=vec[0:64, l, VCOLS["fb1"][0]:VCOLS["fb1"][0] + 1], op=ALU.mult),
                 R=[bconst], W=[bfw])
            P.op("dve", lambda e: e.tensor_tensor(out=fsc[:, 2:3], in0=fsc[:, 0:1],
                                                  in1=vec[0:64, l, VCOLS["fb2"][0]:VCOLS["fb2"][0] + 1], op=ALU.mult),
                 R=[bconst], W=[bfw])
            zt_ = sb(st, "f_z", [17, 512])
            h1 = sb(st, "f_h1", [64, 512])
            h2 = sb(st, "f_h2", [64, 512])
            s2 = sb(st, "f_s2", [64, 512])
            dfc = sb(st, "f_df", [128, 2, 512])
            dbc = sb(st, "f_db", [128, 2, 512])
            kk = sb(st, "f_k", [128, 2, 512])
            kb = sb(st, "f_kb", [128, 2, 512], BF16)
            p1 = ps(st, "f_p1", [64, 512])
            p3 = [ps(st, "f_p3%d" % i, [128, 512]) for i in range(4)]
            bz_, bh1, bh2, bs2, bdf, bdb, bkk, bkb, bp1 = [Buf() for _ in range(9)]
            bp3 = [Buf() for _ in range(4)]

            def sin3(src_ps, dst, bdst, col):
                P.op("act", lambda e: e.activation(out=dst[:], in_=src_ps[:], func=AF.Sin, scale=fsc[:, 0:1],
                                                   bias=fsc[:, col:col + 1]), R=[bp1, bfw], W=[bdst])
                P.op("dve", lambda e: e.tensor_tensor(out=s2[:], in0=dst[:], in1=dst[:], op=ALU.mult), R=[bdst], W=[bs2])
                P.op("dve", lambda e: e.tensor_scalar(out=s2[:], in0=s2[:], scalar1=-4.0, scalar2=3.0, op0=ALU.mult,
                                                      op1=ALU.add), W=[bs2])
                P.op("dve", lambda e: e.tensor_tensor(out=dst[:], in0=dst[:], in1=s2[:], op=ALU.mult), R=[bs2], W=[bdst])

            for (ztab, dftab, dbtab, KD, bKD, R_, Lseg) in [(c_zl, c_dfl, c_dbl, KDL, bKDL, RL, S),
                                                           (c_zc, c_dfc, c_dbc, KDC, bKDC, RC, CT)]:
                for r0 in range(0, R_, 512):
                    P.dma("sp", zt_[:], ztab.ap()[:, r0:r0 + 512], W=[bz_])
                    P.dma("sp", dfc[:], dftab.ap()[:, r0:r0 + 512].rearrange("(cj p) r -> p cj r", p=128), W=[bdf])
                    P.dma("sp", dbc[:], dbtab.ap()[:, r0:r0 + 512].rearrange("(cj p) r -> p cj r", p=128), W=[bdb])
                    P.mm([(p1[:], fw1[:], zt_[:], True, True)], R=[bfw, bz_], W=[bp1])
                    sin3(p1, h1, bh1, 1)
                    P.mm([(p1[:], fw2[:], h1[:], True, True)], R=[bfw, bh1], W=[bp1])
                    sin3(p1, h2, bh2, 2)
                    for q in range(4):
                        P.mm([(p3[q][:], fw3[:, q * 128:(q + 1) * 128], h2[:], True, True)], R=[bfw, bh2], W=[bp3[q]])
                    for cj in range(2):
                        P.op("dve", lambda e, cj=cj: e.tensor_tensor(out=kk[:, cj, :], in0=p3[cj][:], in1=dfc[:, cj, :],
                                                                    op=ALU.mult), R=[bp3[cj], bdf], Wd=[bkk])
                        P.op("dve", lambda e, cj=cj: e.tensor_tensor(out=dbc[:, cj, :], in0=p3[2 + cj][:], in1=dbc[:, cj, :],
                                                                    op=ALU.mult), R=[bp3[2 + cj]], Wd=[bdb])
                        P.op("pool", lambda e, cj=cj: e.tensor_tensor(out=kk[:, cj, :], in0=kk[:, cj, :], in1=dbc[:, cj, :],
                                                                     op=ALU.add), R=[bdb], W=[bkk])
                        m0 = Lseg - 1
                        if r0 <= m0 < r0 + 512:
                            P.op("pool", lambda e, cj=cj, m0=m0, r0=r0: e.tensor_tensor(
                                out=kk[:, cj, m0 - r0:m0 - r0 + 1], in0=kk[:, cj, m0 - r0:m0 - r0 + 1],
                                in1=V(l, "hyb", cj, 1), op=ALU.add), R=[bconst], W=[bkk])
                        P.op("act", lambda e, cj=cj: e.activation(out=kb[:, cj, :], in_=kk[:, cj, :], func=AF.Copy),
                             R=[bkk], Wd=[bkb])
                    P.dma("sp", KD.ap()[:, r0:r0 + 512].rearrange("(cj p) r -> p cj r", p=128), kb[:], R=[bkb], Wd=[bKD])
                    for en in ("act",):
                        P._wait(en, P._deps([], [bkb], []))
                    bkb.w = {}
                    P._wait("dve", P._deps([], [bkk, bdb], []))
                    bkk.w = {}
                    bdb.w = dict(bdb.w)

        for (seg, Lseg, KD, bKD, tbase) in [(0, S, KDL, bKDL, 0), (1, CT, KDC, bKDC, S)]:
            if seg == 1 and last:
                continue
            NB = Lseg // 128
            with phase() as st:
                U = sb(st, "hy_U", [128, 256, NB], BF16)
                bU = Buf()
                with phase() as st2:
                    hin = sb(st2, "hy_in", [128, 6, 514], BF16)
                    hc = sb(st2, "hy_c", [128, 6, 512])
                    x0b = sb(st2, "hy_x0b", [128, 2, 512], BF16)
                    ub = sb(st2, "hy_ub", [128, 2, 512], BF16)
                    ptp = ps(st2, "hy_ptp", [128, 4, 128], BF16)
                    bhin, bhc, bx0b, bub, bptp = [Buf() for _ in range(5)]
                    for t0 in range(0, Lseg, 512):
                        n = min(512, Lseg - t0)
                        c0 = pcol(seg) + t0
                        P.dma("sp", hin[:, :, :n + 2],
                              PT.ap()[256:1024, c0 - 1:c0 + n + 1].rearrange("(cj p) t -> p cj t", p=128), R=[bPT], W=[bhin])
                        for cj in range(6):
                            eng = "dve" if cj % 2 == 0 else "pool"
                            o = VCOLS["hcw"][0]
                            P.op("dve", lambda e, cj=cj: e.tensor_scalar(out=hc[:, cj, :n], in0=hin[:, cj, 0:n],
                                                                        scalar1=V(l, "hcw", 0 * 6 + cj, 1),
                                                                        scalar2=V(l, "hcb", cj, 1), op0=ALU.mult,
                                                                        op1=ALU.add), R=[bhin, bconst], Wd=[bhc])
                            for tap in (1, 2):
                                P.op("dve", lambda e, cj=cj, tap=tap: e.scalar_tensor_tensor(
                                    out=hc[:, cj, :n], in0=hin[:, cj, tap:tap + n], scalar=V(l, "hcw", tap * 6 + cj, 1),
                                    in1=hc[:, cj, :n], op0=ALU.mult, op1=ALU.add), R=[bhin], Wd=[bhc])
                        for cj in range(2):
                            P.op("act", lambda e, cj=cj: e.activation(out=x0b[:, cj, :n], in_=hc[:, cj, :n], func=AF.Copy),
                                 R=[bhc], Wd=[bx0b])
                            P.op("pool", lambda e, cj=cj: e.tensor_tensor(out=ub[:, cj, :n], in0=hc[:, 2 + cj, :n],
                                                                         in1=hc[:, 4 + cj, :n], op=ALU.mult),
                                 R=[bhc], Wd=[bub])
                        P.dma("sp", X0C.ap()[:, tbase + t0:tbase + t0 + n].rearrange("(cj p) t -> p cj t", p=128),
                              x0b[:, :, :n], R=[bx0b], Wd=[bX0C])
                        for cj in range(2):
                            nb_ = n // 128
                            P._wait("pe", P._deps([bub, bconst], [bptp], []))
                            ins = None
                            for q in range(nb_):
                                ins = nc.tensor.transpose(ptp[:, q, :], ub[:, cj, q * 128:(q + 1) * 128], idb[:])
                            P.cnt["pe"] += 1
                            ins.then_inc(P.sem["pe"], 1)
                            P._commit(("e", "pe"), P.cnt["pe"], [bub, bconst], [bptp], [])
                            P.op("act", lambda e, cj=cj, nb_=nb_, t0=t0: e.activation(
                                out=U[:, cj * 128:(cj + 1) * 128, t0 // 128:t0 // 128 + nb_].rearrange("p c q -> p q c"),
                                in_=ptp[:, 0:nb_, :], func=AF.Copy), R=[bptp], Wd=[bU])
                        P._wait("dve", P._deps([], [bhc], []))
                        P._wait("act", P._deps([], [bx0b], []))
                        P._wait("pool", P._deps([], [bub], []))
                        bhc.w = {}
                        bx0b.w = {}
                        bub.w = {}
                KW = 2 * Lseg
                ksh = [sb(st, "hy_ks%d" % i, [128, KW], BF16) for i in range(2)]
                bks = [Buf(), Buf()]
                ytok = sb(st, "hy_y", [128, NB, 128], BF16)
                bytk = Buf()
                pcv = [ps(st, "hy_pc%d" % i, [128, NB]) for i in range(2)]
                bpcv = [Buf(), Buf()]
                pyt = [ps(st, "hy_pt%d" % i, [128, 512]) for i in range(2)]
                bpyt = [Buf(), Buf()]
                x0l = sb(st, "hy_x0l", [128, 512], BF16)
                yo = sb(st, "hy_yo", [128, 512], BF16)
                bx0l, byo = Buf(), Buf()
                for cj in range(2):
                    for cc in range(128):
                        c = cj * 128 + cc
                        k = c % 2
                        src = bass.AP(KD, c * (KD.shape[1]), [[1, 128], [1, KW]])
                        P.dma("sp", ksh[k][:], src, R=[bKD], W=[bks[k]])
                        items = []
                        dl = [0] + [d for d in range(-(NB - 1), NB) if d != 0]
                        for qi, d in enumerate(dl):
                            i0, i1 = max(0, d), min(NB - 1, NB - 1 + d)
                            ns = Lseg - 128 - 128 * d
                            items.append((pcv[k][:, i0:i1 + 1], ksh[k][:, ns:ns + 128], U[:, c, i0 - d:i1 + 1 - d],
                                          qi == 0, qi == len(dl) - 1))
                        P.mm(items, R=[bks[k], bU], W=[bpcv[k]])
                        if c % 2:
                            P.op("act", lambda e, k=k, cc=cc: e.activation(out=ytok[:, :, cc], in_=pcv[k][:, :], func=AF.Copy),
                                 R=[bpcv[k]], Wd=[bytk])
                        else:
                            P.op("dve", lambda e, k=k, cc=cc: e.tensor_copy(out=ytok[:, :, cc], in_=pcv[k][:, :]),
                                 R=[bpcv[k]], Wd=[bytk])
                    for t0 in range(0, Lseg, 512):
                        n = min(512, Lseg - t0)
                        kq = (t0 // 512) % 2
                        P.dma("sp", x0l[:, :n], X0C.ap()[cj * 128:(cj + 1) * 128, tbase + t0:tbase + t0 + n], R=[bX0C], W=[bx0l])
                        P.mm([(pyt[kq][:, q * 128:(q + 1) * 128], ytok[:, t0 // 128 + q, :], anti[:], True, True)
                              for q in range(n // 128)], R=[bytk, bconst], W=[bpyt[kq]])
                        P.op("dve", lambda e, kq=kq: e.tensor_tensor(out=yo[:, :n], in0=pyt[kq][:, :n], in1=x0l[:, :n],
                                                                    op=ALU.mult), R=[bpyt[kq], bx0l], W=[byo])
                        P.dma("sp", MIXT.ap()[256 + cj * 128:256 + (cj + 1) * 128, tbase + t0:tbase + t0 + n], yo[:, :n],
                              R=[byo], Wd=[bMIXT])
                    P._wait("act", P._deps([], [bytk], []))
                    P._wait("dve", P._deps([], [bytk], []))
                    bytk.w = {}

        with phase() as st:
            wq = sb(st, "a_wq", [128, 2, 8, 128], BF16)
            wqs = sb(st, "a_wqs", [128, 2, 8, 32], BF16)
            wk = sb(st, "a_wk", [128, 8, 128], BF16)
            wv = sb(st, "a_wv", [128, 8, 64], BF16)
            bw = Buf()
            P.op("dve", lambda e: e.memset(wq[:], 0.0), W=[bw])
            P.op("dve", lambda e: e.memset(wk[:], 0.0), W=[bw])
            for kc in range(2):
                srcq = w_uq.ap()[l, kc * 128:(kc + 1) * 128, :].rearrange("p (h d) -> p h d", d=96)
                P.dma("pool", wq[:, kc, :, 0:32], srcq[:, :, 64:96], Wd=[bw])
                P.dma("pool", wq[:, kc, :, 64:128], srcq[:, :, 0:64], Wd=[bw])
                P.dma("pool", wqs[:, kc, :, 0:16], srcq[:, :, 80:96], Wd=[bw])
                P.dma("pool", wqs[:, kc, :, 16:32], srcq[:, :, 64:80], Wd=[bw])
            srck = w_ukv.ap()[l].rearrange("p (h d) -> p h d", d=128)
            P.dma("pool", wk[:, :, 64:128], srck[:, :, 0:64], Wd=[bw])
            P.dma("pool", wv[:, :, :], srck[:, :, 64:128], Wd=[bw])
            gsc = sb(st, "a_gsc", [128, 1])
            P.op("dve", lambda e: e.tensor_scalar(out=gsc[:], in0=V(l, "gqm"), scalar1=ATTN_SCALE, scalar2=None,
                                                  op0=ALU.mult), R=[bconst], W=[bw])
            gscs = sb(st, "a_gscs", [128, 1])
            P.op("dve", lambda e: e.tensor_scalar(out=gscs[:], in0=V(l, "gqs"), scalar1=ATTN_SCALE, scalar2=None,
                                                  op0=ALU.mult), R=[bconst], W=[bw])
            lat = sb(st, "a_lat", [128, 3, 512], BF16)
            sq = sb(st, "a_sq", [128, 3, 512], BF16)
            latn = sb(st, "a_latn", [128, 3, 512], BF16)
            rs = sb(st, "a_rs", [128, 512])
            cs = sb(st, "a_cos", [32, 512])
            sn = sb(st, "a_sin", [32, 512])
            kr = sb(st, "a_kr", [32, 2, 512], BF16)
            hm = sb(st, "a_hm", [128, 512])
            hs = sb(st, "a_hs", [32, 512])
            hsq = sb(st, "a_hsq", [128, 512], BF16)
            hrs = sb(st, "a_hrs", [128, 512])
            ho = sb(st, "a_ho", [128, 8, 512], BF16)
            vo = sb(st, "a_vo", [128, 4, 8, 64], BF16)
            pss = ps(st, "a_pss", [128, 512])
            pm = ps(st, "a_pm", [128, 512])
            psw = ps(st, "a_psw", [32, 512])
            pss2 = ps(st, "a_pss2", [128, 512])
            pv = ps(st, "a_pv", [128, 512])
            blat, bsq, blatn, brs, bcs, bkr, bhm, bhs, bhsq, bhrs, bho, bvo, bpss, bpm, bpsw, bpss2, bpv = [Buf() for _ in range(17)]

            def latent_norm(rows, nchunk, gname, n, c0):
                P.dma("sp", lat[:, 0:nchunk, :n], PT.ap()[rows:rows + nchunk * 128, c0:c0 + n].rearrange("(j p) t -> p j t", p=128),
                      R=[bPT], W=[blat])
                P.op("act", lambda e: e.activation(out=sq[:, 0:nchunk, :n], in_=lat[:, 0:nchunk, :n], func=AF.Square),
                     R=[blat], W=[bsq])
                P.mm([(pss[:, :n], onesb[:], sq[:, j, :n], j == 0, j == nchunk - 1) for j in range(nchunk)],
                     R=[bsq, bconst], W=[bpss])
                P.op("act", lambda e: e.activation(out=rs[:, :n], in_=pss[:, :n], func=AF.Sqrt, scale=1.0 / (128 * nchunk),
                                                   bias=EPS), R=[bpss], W=[brs])
                P.op("dve", lambda e: e.reciprocal(out=rs[:, :n], in_=rs[:, :n]), W=[brs])
                for j in range(nchunk):
                    P.op("dve", lambda e, j=j: e.scalar_tensor_tensor(out=latn[:, j, :n], in0=lat[:, j, :n],
                                                                     scalar=V(l, gname, j, 1), in1=rs[:, :n], op0=ALU.mult,
                                                                     op1=ALU.mult), R=[blat, brs, bconst], Wd=[blatn])

            def head_finish(h, n, t0, gm, gs, sw_from_psum, DST, bDST):
                P.op("act", lambda e: e.activation(out=hsq[:, :n], in_=hm_src[0][:, :n], func=AF.Square), R=[hm_src[1]], W=[bhsq])
                if sw_from_psum:
                    P.op("dve", lambda e: e.memset(hsq[32:64, :n], 0.0), W=[bhsq])
                P.mm([(pss2[:, :n], onesb[:], hsq[:, :n], True, True)], R=[bhsq, bconst], W=[bpss2])
                P.op("act", lambda e: e.activation(out=hrs[:, :n], in_=pss2[:, :n], func=AF.Sqrt, scale=1.0 / 96.0, bias=EPS),
                     R=[bpss2], W=[bhrs])
                P.op("dve", lambda e: e.reciprocal(out=hrs[:, :n], in_=hrs[:, :n]), W=[bhrs])
                P.op("dve", lambda e: e.scalar_tensor_tensor(out=hm[:, :n], in0=hm_src[0][:, :n], scalar=gm, in1=hrs[:, :n],
                                                             op0=ALU.mult, op1=ALU.mult), R=[hm_src[1], bhrs, bw], W=[bhm])
                P.op("dve", lambda e: e.scalar_tensor_tensor(out=hs[:, :n], in0=sw_src[0][0:32, :n], scalar=gs[0:32, :],
                                                             in1=hrs[0:32, :n], op0=ALU.mult, op1=ALU.mult),
                     R=[sw_src[1], bhrs, bw], W=[bhs])
                P.op("dve", lambda e: e.tensor_tensor(out=hm[0:32, :n], in0=hm[0:32, :n], in1=cs[:, :n], op=ALU.mult),
                     R=[bcs], W=[bhm])
                P.op("dve", lambda e: e.tensor_tensor(out=hs[:, :n], in0=hs[:, :n], in1=sn[:, :n], op=ALU.mult),
                     R=[bcs], W=[bhs])
                P.op("dve", lambda e: e.tensor_tensor(out=hm[0:32, :n], in0=hm[0:32, :n], in1=hs[:, :n], op=ALU.add),
                     R=[bhs], W=[bhm])
                P.op("act", lambda e: e.activation(out=ho[:, h, :n], in_=hm[:, :n], func=AF.Copy), R=[bhm], Wd=[bho])

            hm_src = [None, None]
            sw_src = [None, None]
            for (t0, n, seg) in chunks:
                c0 = pcol(seg) + (t0 - (0 if seg == 0 else S))
                P.dma("sp", cs[:, :n], c_cos.ap()[:, t0:t0 + n], W=[bcs])
                P.dma("sp", sn[:, :n], c_sin.ap()[:, t0:t0 + n], Wd=[bcs])
                latent_norm(1280, 1, "kvng", n, c0)
                P.dma("sp", kr[:, :, :n], PT.ap()[1408:1472, c0:c0 + n].rearrange("(a p) t -> p a t", p=32), R=[bPT], W=[bkr])
                P.mm([(pv[:, :].rearrange("p (a f) -> p a f", f=512)[:, 0, :] if False else pv[:, :],
                       latn[:, 0, q * 128:(q + 1) * 128], wv[:, :, :].rearrange("p h d -> p (h d)"), True, True)
                      for q in range(0)], R=[], W=[]) if False else None
                for q in range(n // 128):
                    P.mm([(pv[:, :], latn[:, 0, q * 128:(q + 1) * 128], wv[:, :, :].rearrange("p h d -> p (h d)"), True, True)],
                         R=[blatn, bw], W=[bpv])
                    P.op("act", lambda e, q=q: e.activation(out=vo[:, q, :, :].rearrange("p h d -> p (h d)"), in_=pv[:, :],
                                                           func=AF.Copy), R=[bpv], Wd=[bvo])
                for q in range(n // 128):
                    P.dma("sp", VT.ap()[:, t0 + q * 128:t0 + (q + 1) * 128, :].rearrange("h p d -> p h d"), vo[:, q, :, :],
                          R=[bvo], Wd=[bVT])
                for h in range(8):
                    P.mm([(pm[:, :n], wk[:, h, :], latn[:, 0, :n], True, True)], R=[blatn, bw], W=[bpm])
                    P.op("act", lambda e: e.activation(out=hm[64:128, :n], in_=pm[64:128, :n], func=AF.Copy), R=[bpm], W=[bhm])
                    P.op("pool", lambda e: e.tensor_copy(out=hm[0:32, :n], in_=kr[:, 0, :n]), R=[bkr], W=[bhm])
                    P.op("pool", lambda e: e.memset(hm[32:64, :n], 0.0), W=[bhm])
                    hm_src[0], hm_src[1] = hm, bhm
                    sw_src[0], sw_src[1] = kr[:, 1, :], bkr
                    head_finish(h, n, t0, V(l, "gkm"), V(l, "gks"), False, KT, bKT)
                P.dma("sp", KT.ap()[:, :, t0:t0 + n].rearrange("h p t -> p h t"), ho[:, :, :n], R=[bho], Wd=[bKT])
                P._wait("act", P._deps([], [bho, bvo], []))
                bho.w = {}
                bvo.w = {}
                if seg == 1 and last:
                    continue
                latent_norm(1024, 2, "qng", n, c0)
                for h in range(8):
                    P.mm([(pm[:, :n], wq[:, kc, h, :], latn[:, kc, :n], kc == 0, kc == 1) for kc in range(2)],
                         R=[blatn, bw], W=[bpm])
                    P.mm([(psw[:, :n], wqs[:, kc, h, :], latn[:, kc, :n], kc == 0, kc == 1) for kc in range(2)],
                         R=[blatn, bw], W=[bpsw])
                    hm_src[0], hm_src[1] = pm, bpm
                    sw_src[0], sw_src[1] = psw, bpsw
                    head_finish(h, n, t0, gsc[:, :], gscs, True, QT, bQT)
                P.dma("sp", QT.ap()[:, :, t0:t0 + n].rearrange("h p t -> p h t"), ho[:, :, :n], R=[bho], Wd=[bQT])
                P._wait("act", P._deps([], [bho], []))
                bho.w = {}

        with phase() as st:
            NKT = T // 128
            kh = [sb(st, "at_k%d" % i, [128, T], BF16) for i in range(2)]
            vh = [sb(st, "at_v%d" % i, [128, NKT, 64], BF16) for i in range(2)]
            bkh = [Buf(), Buf()]
            qc = [sb(st, "at_q%d" % i, [128, 512], BF16) for i in range(2)]
            bqc = [Buf(), Buf()]
            et = [sb(st, "at_e%d" % i, [128, 512], BF16) for i in range(3)]
            bet = [Buf() for _ in range(3)]
            pS = [ps(st, "at_ps%d" % i, [128, 512]) for i in range(3)]
            bpS = [Buf() for _ in range(3)]
            pO = [ps(st, "at_po%d" % i, [64, 512]) for i in range(2)]
            pD = [ps(st, "at_pd%d" % i, [64, 512]) for i in range(2)]
            bpO = [Buf(), Buf()]
            rc = sb(st, "at_rc", [64, 512])
            oo = sb(st, "at_oo", [64, 512], BF16)
            brc, boo = Buf(), Buf()
            cnt = 0
            qi = 0
            for h in range(8):
                hk = h % 2
                P.dma("sp", kh[hk][:], KT.ap()[h], R=[bKT], W=[bkh[hk]])
                P.dma("sp", vh[hk][:], VT.ap()[h].rearrange("(q p) d -> p q d", p=128), R=[bVT], Wd=[bkh[hk]])
                for (t0, n, seg) in chunks:
                    if seg == 1 and last:
                        continue
                    kts = list(range(S // 128, NKT)) + (list(range(0, S // 128)) if seg == 0 else [])
                    q_ = qi % 2
                    qi += 1
                    P.dma("sp", qc[q_][:, :n], QT.ap()[h, :, t0:t0 + n], R=[bQT], W=[bqc[q_]])
                    for ki, kt in enumerate(kts):
                        s_ = cnt % 3
                        cnt += 1
                        P.mm([(pS[s_][:, :n], kh[hk][:, kt * 128:(kt + 1) * 128], qc[q_][:, :n], True, True)],
                             R=[bkh[hk], bqc[q_]], W=[bpS[s_]])
                        P.op("act", lambda e, s_=s_: e.activation(out=et[s_][:, :n], in_=pS[s_][:, :n], func=AF.Exp),
                             R=[bpS[s_]], W=[bet[s_]])
                        P.mm([(pO[q_][:, :n], vh[hk][:, kt, :], et[s_][:, :n], ki == 0, ki == len(kts) - 1),
                              (pD[q_][:, :n], onesb[:, 0:64], et[s_][:, :n], ki == 0, ki == len(kts) - 1)],
                             R=[bet[s_], bkh[hk], bconst], Wd=[bpO[q_]] if ki else (), W=[bpO[q_]] if ki == 0 else ())
                    P.op("dve", lambda e, q_=q_: e.reciprocal(out=rc[:, :n], in_=pD[q_][:, :n]), R=[bpO[q_]], W=[brc])
                    P.op("dve", lambda e, q_=q_: e.tensor_tensor(out=oo[:, :n], in0=pO[q_][:, :n], in1=rc[:, :n], op=ALU.mult),
                         R=[bpO[q_], brc], W=[boo])
                    P.dma("sp", MIXT.ap()[512 + h * 64:512 + (h + 1) * 64, t0:t0 + n], oo[:, :n], R=[boo], Wd=[bMIXT])

        with phase() as st:
            wo = sb(st, "o_w", [128, 8, D], BF16)
            rw = sb(st, "o_rw", [128, 8, NE])
            rb = sb(st, "o_rb", [1, NE])
            bwo = Buf()
            P.dma("pool", wo[:], w_out.ap()[l].rearrange("(kc p) n -> p kc n", p=128), Wd=[bwo])
            P.dma("sp", rw[:], r_w.ap()[l].rearrange("(kc p) n -> p kc n", p=128), Wd=[bwo])
            P.dma("sp", rb[:], r_b.ap()[l], Wd=[bwo])
            mx = sb(st, "o_mx", [128, 8, 512], BF16)
            xt = sb(st, "o_xt", [128, 8, 512])
            bmx, bxt = Buf(), Buf()
            po_ = [ps(st, "o_ps%d" % i, [128, 512]) for i in range(2)]
            bpo_ = [Buf(), Buf()]
            nt = norm_tiles(st)
            h2 = sb(st, "o_h2", [128, 8, 512], BF16)
            h2f = sb(st, "o_h2f", [128, 8, 512])
            bh2 = Buf()
            plg = ps(st, "o_plg", [128, NE])
            pgt = ps(st, "o_pgt", [NE, 512])
            lg = sb(st, "o_lg", [128, NE])
            m8 = sb(st, "o_m8", [128, 8])
            msk = sb(st, "o_msk", [128, NE])
            ex = sb(st, "o_ex", [128, NE])
            ssum = sb(st, "o_ss", [128, 2])
            gts = sb(st, "o_gts", [NE, 512])
            bplg, bpgt, blg, bm8, bmsk, bex, bss, bgts = [Buf() for _ in range(8)]
            for (t0, n, seg) in chunks:
                if seg == 1 and last:
                    continue
                P.dma("sp", mx[:, :, :n], MIXT.ap()[:, t0:t0 + n].rearrange("(kc p) t -> p kc t", p=128), R=[bMIXT], W=[bmx])
                P.dma("sp", xt[:, :, :n], XTv[:, :, t0:t0 + n], R=[bXT], W=[bxt])
                for j in range(8):
                    k = j % 2
                    P.mm([(po_[k][:, :n], wo[:, kc, j * 128:(j + 1) * 128], mx[:, kc, :n], kc == 0, kc == 7) for kc in range(8)],
                         R=[bwo, bmx], W=[bpo_[k]])
                    P.op("dve", lambda e, j=j, k=k: e.scalar_tensor_tensor(out=xt[:, j, :n], in0=po_[k][:, :n],
                                                                          scalar=MOD(2, j, seg), in1=xt[:, j, :n],
                                                                          op0=ALU.mult, op1=ALU.add),
                         R=[bpo_[k], bmodv], W=[bxt])
                P.dma("sp", XTv[:, :, t0:t0 + n], xt[:, :, :n], R=[bxt], Wd=[bXT])
                norm_chunk(nt, t0, n, seg, A2, 3, h2, bh2, hF=h2f)
                P.dma("sp", H2T.ap()[:, t0:t0 + n].rearrange("(kc p) t -> p kc t", p=128), h2[:, :, :n], R=[bh2], Wd=[bH2T])
                for q in range(n // 128):
                    items = [(plg[:, :], h2f[:, kc, q * 128:(q + 1) * 128], rw[:, kc, :], kc == 0, False) for kc in range(8)]
                    items.append((plg[:, :], onesf[0:1, :], rb[0:1, :], False, True))
                    P.mm(items, R=[bh2, bwo, bconst], W=[bplg])
                    P.op("dve", lambda e: e.tensor_copy(out=lg[:], in_=plg[:]), R=[bplg], W=[blg])
                    P.op("dve", lambda e: e.max(out=m8[:], in_=lg[:]), R=[blg], W=[bm8])
                    P.op("dve", lambda e: e.tensor_scalar(out=msk[:], in0=lg[:], scalar1=m8[:, 3:4], scalar2=None,
                                                          op0=ALU.is_ge), R=[blg, bm8], W=[bmsk])
                    P.op("dve", lambda e: e.tensor_scalar(out=ssum[:, 1:2], in0=m8[:, 0:1], scalar1=-1.0, scalar2=None,
                                                          op0=ALU.mult), R=[bm8], W=[bss])
                    P.op("act", lambda e: e.activation(out=ex[:], in_=lg[:], func=AF.Exp, bias=ssum[:, 1:2]),
                         R=[blg, bss], W=[bex])
                    P.op("dve", lambda e: e.tensor_tensor(out=ex[:], in0=ex[:], in1=msk[:], op=ALU.mult), R=[bmsk], W=[bex])
                    P.op("dve", lambda e: e.reduce_sum(out=ssum[:, 0:1], in_=ex[:], axis=AX.X), R=[bex], W=[bss])
                    P.op("dve", lambda e: e.reciprocal(out=ssum[:, 0:1], in_=ssum[:, 0:1]), W=[bss])
                    P.op("dve", lambda e: e.tensor_scalar(out=ex[:], in0=ex[:], scalar1=ssum[:, 0:1], scalar2=None,
                                                          op0=ALU.mult), R=[bss], W=[bex])
                    P._wait("pe", P._deps([bex, bconst], [], [bpgt]))
                    ins = nc.tensor.transpose(pgt[:, q * 128:(q + 1) * 128], ex[:], idf[:])
                    P.cnt["pe"] += 1
                    ins.then_inc(P.sem["pe"], 1)
                    P._commit(("e", "pe"), P.cnt["pe"], [bex, bconst], [], [bpgt])
                P.op("act", lambda e: e.activation(out=gts[:, :n], in_=pgt[:, :n], func=AF.Copy), R=[bpgt], W=[bgts])
                P.dma("sp", GT.ap()[:, t0:t0 + n], gts[:, :n], R=[bgts], Wd=[bGT])
                P._wait("pe", P._deps([], [bpgt], []))
                bpgt.w = {}
                P._wait("act", P._deps([], [bh2], []))
                P._wait("pool", P._deps([], [bh2], []))
                bh2.w = {}

        with phase() as st:
            TG = 1024
            NSL = 5
            wsl = [sb(st, "m_w%d" % i, [128, 8, 1024], BF16) for i in range(NSL)]
            bws = [Buf() for _ in range(NSL)]
            b2 = sb(st, "m_b2", [NE, D], BF16)
            sel = sb(st, "m_sel", [NE, NE, 128], BF16)
            bb2 = Buf()
            P.dma("pool", b2[:], m_b2.ap()[l], Wd=[bb2])
            P.op("dve", lambda e: e.memset(sel[:], 0.0), W=[bb2])
            for e_ in range(NE):
                pass
            P.op("dve", lambda e: e.tensor_tensor(out=sel[:], in0=sel[:],
                                                  in1=idf[0:NE, 0:NE].unsqueeze(2).to_broadcast([NE, NE, 128]), op=ALU.add),
                 R=[bconst], W=[bb2])
            hh = sb(st, "m_h", [128, 8, TG], BF16)
            acc = sb(st, "m_acc", [128, 8, TG])
            at = sb(st, "m_at", [128, 8, TG], BF16)
            gt = sb(st, "m_gt", [NE, TG], BF16)
            gtf = sb(st, "m_gtf", [NE, TG])
            xt = sb(st, "m_xt", [128, 512])
            bhh, bacc, bat, bgt, bxt = [Buf() for _ in range(5)]
            glu = [sb(st, "m_glu%d" % i, [128, 512]) for i in range(2)]
            sg = [sb(st, "m_sg%d" % i, [128, 512]) for i in range(2)]
            ln = [sb(st, "m_ln%d" % i, [128, 512]) for i in range(2)]
            bglu = [Buf(), Buf()]
            bsg = [Buf(), Buf()]
            bln = [Buf(), Buf()]
            pg = [ps(st, "m_pg%d" % i, [128, 512]) for i in range(2)]
            pl = [ps(st, "m_pl%d" % i, [128, 512]) for i in range(2)]
            py = [ps(st, "m_py%d" % i, [128, 512]) for i in range(2)]
            pbc = [ps(st, "m_pbc%d" % i, [128, 512]) for i in range(2)]
            bpg, bpl, bpy, bpbc = [[Buf(), Buf()] for _ in range(4)]
            Tm = T if not last else S
            slot = 0
            ycnt = 0
            tcnt = 0
            for g0 in range(0, Tm, TG):
                ng = min(TG, Tm - g0)
                halves = [(hs_, min(512, ng - hs_)) for hs_ in range(0, ng, 512)]
                P.dma("sp", hh[:, :, :ng], H2T.ap()[:, g0:g0 + ng].rearrange("(kc p) t -> p kc t", p=128), R=[bH2T], W=[bhh])
                P.dma("sp", gtf[:, :ng], GT.ap()[:, g0:g0 + ng], R=[bGT], W=[bgt])
                P.op("pool", lambda e: e.tensor_copy(out=gt[:, :ng], in_=gtf[:, :ng]), W=[bgt])
                for ex_ in range(NE):
                    s1, s2_, s3 = slot % NSL, (slot + 1) % NSL, (slot + 2) % NSL
                    slot += 3
                    w1src = m_w1.ap()[l, ex_].rearrange("(kc p) n -> p kc n", p=128)
                    P.dma("pool", wsl[s1][:], w1src[:, :, 0:1024], W=[bws[s1]])
                    P.dma("pool", wsl[s2_][:], w1src[:, :, 1024:2048], W=[bws[s2_]])
                    P.dma("pool", wsl[s3][:], m_w2.ap()[l, ex_].rearrange("(kc p) n -> p kc n", p=128), W=[bws[s3]])
                    b1o = VCOLS["b1"][0] + ex_ * 16
                    for (hs_, hn) in halves:
                        hsl = slice(hs_, hs_ + hn)
                        kb_ = tcnt % 2
                        P.mm([(pbc[kb_][:, :hn], sel[:, ex_, :], gt[:, hsl], True, True)], R=[bb2, bgt], W=[bpbc[kb_]])
                        for j in range(8):
                            k = tcnt % 2
                            tcnt += 1
                            P.mm([(pg[k][:, :hn], wsl[s1][:, kc, j * 128:(j + 1) * 128], hh[:, kc, hsl], kc == 0, kc == 7)
                                  for kc in range(8)], R=[bws[s1], bhh], W=[bpg[k]])
                            P.mm([(pl[k][:, :hn], wsl[s2_][:, kc, j * 128:(j + 1) * 128], hh[:, kc, hsl], kc == 0, kc == 7)
                                  for kc in range(8)], R=[bws[s2_], bhh], W=[bpl[k]])
                            bg_ = vec[:, l, b1o + j:b1o + j + 1]
                            bl_ = vec[:, l, b1o + 8 + j:b1o + 8 + j + 1]
                            P.op("dve", lambda e, k=k, bg_=bg_: e.tensor_scalar(out=glu[k][:, :hn], in0=pg[k][:, :hn], scalar1=bg_,
                                                                               scalar2=7.0, op0=ALU.add, op1=ALU.min),
                                 R=[bpg[k], bconst], W=[bglu[k]])
                            P.op("act", lambda e, k=k: e.activation(out=sg[k][:, :hn], in_=glu[k][:, :hn], func=AF.Sigmoid,
                                                                   scale=1.702), R=[bglu[k]], W=[bsg[k]])
                            P.op("dve", lambda e, k=k, bl_=bl_: e.tensor_scalar(out=ln[k][:, :hn], in0=pl[k][:, :hn], scalar1=bl_,
                                                                               scalar2=7.0, op0=ALU.add, op1=ALU.min),
                                 R=[bpl[k], bconst], W=[bln[k]])
                            P.op("pool", lambda e, k=k: e.tensor_scalar(out=ln[k][:, :hn], in0=ln[k][:, :hn], scalar1=-7.0,
                                                                       scalar2=1.0, op0=ALU.max, op1=ALU.add), W=[bln[k]])
                            P.op("pool", lambda e, k=k: e.tensor_tensor(out=glu[k][:, :hn], in0=glu[k][:, :hn], in1=sg[k][:, :hn],
                                                                       op=ALU.mult), R=[bsg[k]], W=[bglu[k]])
                            P.op("pool", lambda e, k=k: e.tensor_tensor(out=glu[k][:, :hn], in0=glu[k][:, :hn], in1=ln[k][:, :hn],
                                                                       op=ALU.mult), R=[bln[k]], W=[bglu[k]])
                            P.op("dve", lambda e, k=k, j=j, kb_=kb_: e.tensor_tensor(out=at[:, j, hsl], in0=glu[k][:, :hn],
                                                                                    in1=pbc[kb_][:, :hn], op=ALU.mult),
                                 R=[bglu[k], bpbc[kb_]], Wd=[bat])
                        for j in range(8):
                            k = ycnt % 2
                            ycnt += 1
                            items = [(py[k][:, :hn], wsl[s3][:, kc, j * 128:(j + 1) * 128], at[:, kc, hsl], kc == 0, False)
                                     for kc in range(8)]
                            if ex_ == 0:
                                items.append((py[k][:, :hn], b2[:, j * 128:(j + 1) * 128], gt[:, hsl], False, True))
                            else:
                                o_, l_, r_, st_, _ = items[-1]
                                items[-1] = (o_, l_, r_, st_, True)
                            P.mm(items, R=[bws[s3], bat, bb2, bgt], W=[bpy[k]])
                            if ex_ == 0:
                                P.op("act", lambda e, k=k, j=j: e.activation(out=acc[:, j, hsl], in_=py[k][:, :hn], func=AF.Copy),
                                     R=[bpy[k]], Wd=[bacc])
                            else:
                                P.op("dve", lambda e, k=k, j=j: e.tensor_tensor(out=acc[:, j, hsl], in0=acc[:, j, hsl],
                                                                               in1=py[k][:, :hn], op=ALU.add),
                                     R=[bpy[k]], Wd=[bacc])
                        P._wait("dve", P._deps([], [bat], []))
                        bat.w = {}
                for (hs_, hn) in halves:
                    t0 = g0 + hs_
                    seg = 0 if t0 < S else 1
                    for j in range(8):
                        P.dma("sp", xt[:, :hn], XT.ap()[j, :, t0:t0 + hn], R=[bXT], W=[bxt])
                        P.op("dve", lambda e, j=j, hs_=hs_, hn=hn, seg=seg: e.scalar_tensor_tensor(
                            out=xt[:, :hn], in0=acc[:, j, hs_:hs_ + hn], scalar=MOD(5, j, seg), in1=xt[:, :hn],
                            op0=ALU.mult, op1=ALU.add), R=[bacc, bmodv], W=[bxt])
                        P.dma("sp", XT.ap()[j, :, t0:t0 + hn], xt[:, :hn], R=[bxt], Wd=[bXT])
                P._wait("act", P._deps([], [bacc], []))
                P._wait("dve", P._deps([], [bacc], []))
                bacc.w = {}
        modst.close()

    with phase() as st:
        xl = [sb(st, "f_xl%d" % i, [128, 8, 128]) for i in range(2)]
        yo_ = [sb(st, "f_yo%d" % i, [128, D]) for i in range(2)]
        pf = [ps(st, "f_ps%d" % i, [128, D]) for i in range(2)]
        bxl, byo_, bpf = [Buf(), Buf()], [Buf(), Buf()], [Buf(), Buf()]
        for ti in range(S // 128):
            k = ti % 2
            P.dma("sp", xl[k][:], XTv[:, :, ti * 128:(ti + 1) * 128], R=[bXT], W=[bxl[k]])
            P._wait("pe", P._deps([bxl[k], bconst], [bpf[k]], []))
            ins = None
            for j in range(8):
                ins = nc.tensor.transpose(pf[k][:, j * 128:(j + 1) * 128], xl[k][:, j, :], idf[:])
            P.cnt["pe"] += 1
            ins.then_inc(P.sem["pe"], 1)
            P._commit(("e", "pe"), P.cnt["pe"], [bxl[k], bconst], [bpf[k]], [])
            P.op("act", lambda e, k=k: e.activation(out=yo_[k][:], in_=pf[k][:], func=AF.Copy), R=[bpf[k]], W=[byo_[k]])
            P.dma("sp", y_out.ap()[ti * 128:(ti + 1) * 128, :], yo_[k][:], R=[byo_[k]])
    P.finish()
    es.close()
    return nc


def _tables(S):
    T = S + CT
    n_rows = S // GRID_W
    row = np.repeat(np.arange(n_rows, dtype=np.float32), GRID_W)
    col = np.tile(np.arange(GRID_W, dtype=np.float32), n_rows)
    inv = (10000.0 ** (-np.arange(8, dtype=np.float32) / 8)).astype(np.float32)
    ang = np.concatenate([row[:, None] * inv, col[:, None] * inv], axis=-1).astype(np.float32)
    cos = np.ones((32, T), np.float32)
    sin = np.zeros((32, T), np.float32)
    cos[0:16, :S] = np.cos(ang).T
    cos[16:32, :S] = np.cos(ang).T
    sin[0:16, :S] = -np.sin(ang).T
    sin[16:32, :S] = np.sin(ang).T
    invc = np.zeros((128, 2, T), np.float32)
    for g, w in enumerate(POOL_WINDOWS):
        for (L, off) in [(S, 0), (CT, S)]:
            t = np.arange(L)
            lo = np.clip(t - w // 2, 0, L)
            hi = np.clip(t + w // 2, 0, L)
            invc[(g % 2) * 64:(g % 2) * 64 + 64, g // 2, off:off + L] = (1.0 / (hi - lo).astype(np.float32))[None, :]
    deltas = np.abs(np.linspace(HY_MIN_DECAY, HY_MAX_DECAY, 256, dtype=np.float32))

    def filt(L):
        R = 2 * L + 512
        z = np.zeros((17, R), np.float32)
        df = np.zeros((256, R), np.float32)
        db = np.zeros((256, R), np.float32)
        m = np.arange(R)
        lag = (L - 1) - m
        valid = np.abs(lag) <= L - 1
        idx = np.abs(lag)[valid]
        tt = np.linspace(0.0, 1.0, L, dtype=np.float32)[idx]
        wpos = ((2.0 * math.pi / L) * np.arange(L, dtype=np.float32))[idx]
        f = np.linspace(1e-4, 7, 8, dtype=np.float32)
        zz = np.concatenate([tt[None, :], np.cos(f[:, None] * wpos[None, :]), -np.sin(f[:, None] * wpos[None, :])], axis=0)
        z[:, valid] = zz.astype(np.float32)
        dec = np.exp(-tt[None, :] * deltas[:, None]).astype(np.float32)
        lv = lag[valid]
        dfv = np.where(lv[None, :] >= 0, dec, 0.0)
        dbv = np.where(lv[None, :] < 0, dec, 0.0)
        df[:, valid] = dfv
        db[:, valid] = dbv
        return z, df, db

    zl, dfl, dbl = filt(S)
    zc, dfc, dbc = filt(CT)
    idf = np.eye(128, dtype=np.float32)
    return {"c_idf": idf, "c_idb": idf.astype(ml_dtypes.bfloat16), "c_anti": idf[::-1].copy().astype(ml_dtypes.bfloat16),
            "c_cos": cos, "c_sin": sin, "c_invc": invc, "c_zl": zl, "c_dfl": dfl, "c_dbl": dbl,
            "c_zc": zc, "c_dfc": dfc, "c_dbc": dbc}


def _pack_vecs(inp, b, NL):
    v = np.zeros((NL, 128, NV), np.float32)

    def put(l, name, arr, j0=0):
        o, w = VCOLS[name]
        arr = np.asarray(arr, np.float32).reshape(-1, 128)
        v[l, :, o + j0:o + j0 + arr.shape[0]] = arr.T

    for l in range(NL):
        put(l, "n1g", inp["norm1_g"][l])
        put(l, "n2g", inp["norm2_g"][l])
        put(l, "bmod", inp["b_mod"][l])
        put(l, "c", inp["c"][b])
        put(l, "cctx", inp["c_ctx"])
        put(l, "psc", inp["pool_scale"][l])
        for tap in range(3):
            put(l, "hcw", inp["hy_conv_w"][l, tap], tap * 6)
        put(l, "hcb", inp["hy_conv_b"][l])
        put(l, "hyb", inp["hy_bias"][l])
        put(l, "qng", inp["mla_q_norm_g"][l])
        put(l, "kvng", inp["mla_kv_norm_g"][l])
        for nm, g in (("q", inp["qk_norm_q"][l]), ("k", inp["qk_norm_k"][l])):
            main = np.zeros(128, np.float32)
            main[0:32] = g[64:96]
            main[64:128] = g[0:64]
            sw = np.zeros(128, np.float32)
            sw[0:16] = g[80:96]
            sw[16:32] = g[64:80]
            put(l, "g%sm" % nm, main)
            put(l, "g%ss" % nm, sw)
        for nm, src in (("fb1", "hy_f_b1"), ("fb2", "hy_f_b2"), ("freq", "hy_freq")):
            a = np.zeros(128, np.float32)
            a[0:64] = inp[src][l]
            put(l, nm, a)
        b1 = np.asarray(inp["moe_b1"][l], np.float32).reshape(NE, 16, 128)
        o, w = VCOLS["b1"]
        v[l, :, o:o + w] = b1.transpose(2, 0, 1).reshape(128, NE * 16)
    return v


_NC_CACHE = {}


def run(inputs, S, NL, batches, dbg=False):
    inp = {k: np.asarray(v) for k, v in inputs.items()}
    key = (S, NL, dbg)
    if key not in _NC_CACHE:
        _NC_CACHE[key] = build(S, NL, dbg)
    nc = _NC_CACHE[key]
    tabs = _tables(S)
    shared = dict(tabs)
    for nm in ["w_mod", "w_in", "pool_w", "hy_f_w1", "hy_f_w2", "hy_f_w3", "mla_w_uq", "mla_w_ukv", "w_out", "router_w",
               "moe_w1", "moe_w2", "moe_b2"]:
        shared[nm] = np.ascontiguousarray(inp[nm][:NL], dtype=np.float32)
    shared["router_b"] = np.ascontiguousarray(inp["router_b"][:NL, None, :], dtype=np.float32)
    in_maps = []
    for b in batches:
        m = dict(shared)
        m["x"] = np.ascontiguousarray(inp["x"][b, :S], dtype=np.float32)
        m["ctx"] = np.ascontiguousarray(inp["ctx"][b], dtype=np.float32)
        m["vecs"] = _pack_vecs(inp, b, NL)
        in_maps.append(m)
    res = run_bass_kernel_spmd(nc, in_maps, core_ids=list(range(len(batches))))
    return res


def kernel(**inputs):
    B, S, _ = inputs["x"].shape
    res = run(inputs, S, 2, list(range(B)))
    return np.stack([np.asarray(r["y"], dtype=np.float32) for r in res.results], axis=0)
```

```python
import math
from contextlib import ExitStack
import numpy as np
import ml_dtypes
import concourse.bass as bass
import concourse.mybir as mybir
from concourse.bass_utils import run_bass_kernel_spmd

F32 = mybir.dt.float32
BF16 = mybir.dt.bfloat16
ALU = mybir.AluOpType
AF = mybir.ActivationFunctionType
AX = mybir.AxisListType

D = 1024
CT = 256
NE = 32
NPT = 1472
EPS = 1e-6
GRID_W = 64
POOL_WINDOWS = (2, 4, 8, 16)
HY_MIN_DECAY = math.log(1e-2) / 1.5
HY_MAX_DECAY = math.log(1e-2) / 0.3
ATTN_SCALE = 96 ** -0.5
SIG_CLAMP = float(1.0 / (1.0 + math.exp(-1.702 * 7.0)))

VCOLS = {}
_o = 0
for _n, _w in [("n1g", 8), ("n2g", 8), ("bmod", 48), ("c", 8), ("cctx", 8), ("psc", 2), ("hcw", 18), ("hcb", 6),
               ("hyb", 2), ("qng", 2), ("kvng", 1), ("gqm", 1), ("gqs", 1), ("gkm", 1), ("gks", 1),
               ("fb1", 1), ("fb2", 1), ("freq", 1), ("b1", 512)]:
    VCOLS[_n] = (_o, _w)
    _o += _w
NV = _o


class Buf:
    __slots__ = ("w", "r")

    def __init__(self):
        self.w = {}
        self.r = {}


class Prog:
    NS = 32

    def __init__(self, nc, es):
        self.nc = nc
        self.E = {"pe": nc.tensor, "act": nc.scalar, "dve": nc.vector, "pool": nc.gpsimd, "sp": nc.sync}
        self.sem = {k: es.enter_context(nc.semaphore("s_" + k)) for k in self.E}
        self.cnt = {k: 0 for k in self.E}
        self.dsem = [es.enter_context(nc.semaphore("d%d" % i)) for i in range(self.NS)]
        self.dtot = [0] * self.NS
        self.dn = 0
        self.NX = 8
        self.xsem = [es.enter_context(nc.semaphore("x%d" % i)) for i in range(self.NX)]
        self.xtot = [0] * self.NX
        self.xn = 0
        self.seen = {k: {} for k in self.E}

    def _wait(self, e, deps):
        for key, n in deps.items():
            if n <= 0 or (key == ("e", "pe") and e == "pe"):
                continue
            if self.seen[e].get(key, 0) >= n:
                continue
            sem = self.sem[key[1]] if key[0] == "e" else (self.dsem[key[1]] if key[0] == "d" else self.xsem[key[1]])
            self.E[e].wait_ge(sem, n)
            self.seen[e][key] = n

    @staticmethod
    def _deps(R, W, Wd):
        d = {}
        for b in R:
            for k, n in b.w.items():
                d[k] = max(d.get(k, 0), n)
        for b in W:
            for k, n in b.w.items():
                d[k] = max(d.get(k, 0), n)
            for k, n in b.r.items():
                d[k] = max(d.get(k, 0), n)
        for b in Wd:
            for k, n in b.r.items():
                d[k] = max(d.get(k, 0), n)
        return d

    @staticmethod
    def _commit(key, n, R, W, Wd):
        for b in R:
            b.r[key] = max(b.r.get(key, 0), n)
        for b in W:
            b.w = {key: n}
            b.r = {}
        for b in Wd:
            b.w[key] = max(b.w.get(key, 0), n)

    def op(self, e, fn, R=(), W=(), Wd=()):
        self._wait(e, self._deps(R, W, Wd))
        ins = fn(self.E[e])
        self.cnt[e] += 1
        ins.then_inc(self.sem[e], 1)
        self._commit(("e", e), self.cnt[e], R, W, Wd)

    def mm(self, items, R=(), W=(), Wd=()):
        self._wait("pe", self._deps(R, W, Wd))
        ins = None
        for (o, l, r, st, sp) in items:
            ins = self.nc.tensor.matmul(o, l, r, start=st, stop=sp)
        self.cnt["pe"] += 1
        ins.then_inc(self.sem["pe"], 1)
        self._commit(("e", "pe"), self.cnt["pe"], R, W, Wd)

    def dma(self, e, out, in_, R=(), W=(), Wd=(), **kw):
        i = self.dn
        self.dn = (i + 1) % self.NS
        d = self._deps(R, W, Wd)
        if self.dtot[i]:
            d[("d", i)] = max(d.get(("d", i), 0), self.dtot[i])
        self._wait(e, d)
        ins = self.E[e].dma_start(out=out, in_=in_, **kw)
        self.dtot[i] += 16
        ins.then_inc(self.dsem[i], 16)
        self._commit(("d", i), self.dtot[i], R, W, Wd)

    def barrier(self):
        for e in self.E:
            d = {}
            for k in self.E:
                if k != e and self.cnt[k]:
                    d[("e", k)] = self.cnt[k]
            for i in range(self.NS):
                if self.dtot[i]:
                    d[("d", i)] = self.dtot[i]
            self._wait(e, d)

    def bgdma(self, e, out, in_, R=(), W=(), Wd=(), **kw):
        i = self.xn
        self.xn = (i + 1) % self.NX
        d = self._deps(R, W, Wd)
        if self.xtot[i]:
            d[("x", i)] = max(d.get(("x", i), 0), self.xtot[i])
        self._wait(e, d)
        ins = self.E[e].dma_start(out=out, in_=in_, **kw)
        self.xtot[i] += 16
        ins.then_inc(self.xsem[i], 16)
        self._commit(("x", i), self.xtot[i], R, W, Wd)

    def finish(self):
        for i in range(self.NX):
            if self.xtot[i]:
                self.nc.sync.wait_ge(self.xsem[i], self.xtot[i])
        for i in range(self.NS):
            if self.dtot[i]:
                self.nc.sync.wait_ge(self.dsem[i], self.dtot[i])
        for k in self.E:
            if k != "sp" and self.cnt[k]:
                self.nc.sync.wait_ge(self.sem[k], self.cnt[k])


def build(S, NL, dbg=False):
    T = S + CT
    NLC = S // 512
    chunks = [(i * 512, 512, 0) for i in range(NLC)] + [(S, CT, 1)]
    PTW = S + 24 + CT + 8
    RL = 2 * S + 512
    RC = 2 * CT + 512
    nc = bass.Bass("TRN2", target_bir_lowering=False)
    es = ExitStack()
    P = Prog(nc, es)

    def din(name, shape, dt=F32):
        return nc.dram_tensor(name, list(shape), dt, kind="ExternalInput")

    def dscr(name, shape, dt):
        return nc.dram_tensor(name, list(shape), dt, kind="ExternalOutput" if dbg else "Internal")

    x_in = din("x", [S, D])
    ctx_in = din("ctx", [CT, D])
    vecs = din("vecs", [NL, 128, NV])
    w_mod = din("w_mod", [NL, D, 6 * D])
    w_in = din("w_in", [NL, D, 1440])
    pool_w = din("pool_w", [NL, 4, 64, 64])
    f_w1 = din("hy_f_w1", [NL, 17, 64])
    f_w2 = din("hy_f_w2", [NL, 64, 64])
    f_w3 = din("hy_f_w3", [NL, 64, 512])
    w_uq = din("mla_w_uq", [NL, 256, 768])
    w_ukv = din("mla_w_ukv", [NL, 128, 1024])
    w_out = din("w_out", [NL, D, D])
    r_w = din("router_w", [NL, D, NE])
    r_b = din("router_b", [NL, 1, NE])
    m_w1 = din("moe_w1", [NL, NE, D, 2 * D])
    m_w2 = din("moe_w2", [NL, NE, D, D])
    m_b2 = din("moe_b2", [NL, NE, D])
    c_idf = din("c_idf", [128, 128])
    c_idb = din("c_idb", [128, 128], BF16)
    c_anti = din("c_anti", [128, 128], BF16)
    c_cos = din("c_cos", [32, T])
    c_sin = din("c_sin", [32, T])
    c_invc = din("c_invc", [128, 2, T])
    c_zl = din("c_zl", [17, RL])
    c_dfl = din("c_dfl", [256, RL])
    c_dbl = din("c_dbl", [256, RL])
    c_zc = din("c_zc", [17, RC])
    c_dfc = din("c_dfc", [256, RC])
    c_dbc = din("c_dbc", [256, RC])
    y_out = nc.dram_tensor("y", [S, D], F32, kind="ExternalOutput")

    XT = dscr("XT", [8, 128, T], F32)
    PT = dscr("PT", [NPT, PTW], BF16)
    X0C = dscr("X0C", [256, T], BF16)
    KDL = dscr("KDL", [256, RL], BF16)
    KDC = dscr("KDC", [256, RC], BF16)
    KT = dscr("KT", [8, 128, T], BF16)
    QT = dscr("QT", [8, 128, T], BF16)
    VT = dscr("VT", [8, T, 64], BF16)
    MIXT = dscr("MIXT", [D, T], BF16)
    H2T = dscr("H2T", [D, T], BF16)
    GT = dscr("GT", [NE, T], F32)
    W1B = dscr("W1B", [NE, D, 2 * D], BF16)
    W2B = dscr("W2B", [NE, D, D], BF16)
    bW1B, bW2B = Buf(), Buf()
    bXT, bPT, bX0C, bKDL, bKDC, bKT, bQT, bVT, bMIXT, bH2T, bGT = [Buf() for _ in range(11)]

    XTv = XT.ap().rearrange("j p t -> p j t")

    uid = [0]

    class phase:
        def __enter__(self_):
            self_.st = ExitStack()
            return self_.st

        def __exit__(self_, *a):
            if a[0] is None:
                P.barrier()
            self_.st.close()
            return False

    def sb(st, name, shape, dt=F32):
        uid[0] += 1
        return st.enter_context(nc.sbuf_tensor("%s_%d" % (name, uid[0]), list(shape), dt))

    def ps(st, name, shape, dt=F32):
        uid[0] += 1
        return st.enter_context(nc.psum_tensor("%s_%d" % (name, uid[0]), list(shape), dt))

    def pcol(seg):
        return 8 if seg == 0 else S + 24

    idf = sb(es, "idf", [128, 128])
    idb = sb(es, "idb", [128, 128], BF16)
    anti = sb(es, "anti", [128, 128], BF16)
    onesb = sb(es, "onesb", [128, 128], BF16)
    onesf = sb(es, "onesf", [128, 128])
    vec = sb(es, "vec", [128, NL, NV])
    bconst = Buf()
    P.dma("sp", idf[:], c_idf.ap(), W=[bconst])
    P.dma("sp", idb[:], c_idb.ap(), Wd=[bconst])
    P.dma("sp", anti[:], c_anti.ap(), Wd=[bconst])
    P.dma("sp", vec[:], vecs.ap().rearrange("l p v -> p l v"), Wd=[bconst])
    P.op("dve", lambda e: e.memset(onesb[:], 1.0), Wd=[bconst])
    P.op("dve", lambda e: e.memset(onesf[:], 1.0), Wd=[bconst])

    def V(l, name, j=0, n=1):
        o, w = VCOLS[name]
        return vec[:, l, o + j:o + j + n]

    with phase() as st:
        zt = sb(st, "zt", [128, 16], BF16)
        bz = Buf()
        P.op("dve", lambda e: e.memset(zt[:], 0.0), W=[bz])
        for r0 in range(0, NPT, 128):
            nr = min(128, NPT - r0)
            for (c0, w) in [(0, 8), (8 + S, 16), (S + 24 + CT, 8)]:
                P.dma("sp", PT.ap()[r0:r0 + nr, c0:c0 + w], zt[0:nr, 0:w], R=[bz], Wd=[bPT])
        xin = [sb(st, "xin%d" % i, [128, D]) for i in range(2)]
        xtt = [sb(st, "xtt%d" % i, [128, 8, 128]) for i in range(2)]
        pst = [ps(st, "pst%d" % i, [128, 8, 128]) for i in range(2)]
        bxin = [Buf(), Buf()]
        bxtt = [Buf(), Buf()]
        bpst = [Buf(), Buf()]
        for ti in range(T // 128):
            k = ti % 2
            src = x_in.ap()[ti * 128:(ti + 1) * 128, :] if ti * 128 < S else ctx_in.ap()[ti * 128 - S:(ti + 1) * 128 - S, :]
            P.dma("sp", xin[k][:], src, W=[bxin[k]])
            P._wait("pe", P._deps([bxin[k], bconst], [bpst[k]], []))
            ins = None
            for j in range(8):
                ins = nc.tensor.transpose(pst[k][:, j, :], xin[k][:, j * 128:(j + 1) * 128], idf[:])
            P.cnt["pe"] += 1
            ins.then_inc(P.sem["pe"], 1)
            P._commit(("e", "pe"), P.cnt["pe"], [bxin[k], bconst], [bpst[k]], [])
            P.op("act" if ti % 2 else "dve",
                 (lambda e, k=k: e.activation(out=xtt[k][:], in_=pst[k][:], func=AF.Copy)) if ti % 2 else
                 (lambda e, k=k: e.tensor_copy(out=xtt[k][:], in_=pst[k][:])), R=[bpst[k]], W=[bxtt[k]])
            P.dma("sp", XTv[:, :, ti * 128:(ti + 1) * 128], xtt[k][:], R=[bxtt[k]], Wd=[bXT])

    for l in range(NL):
        last = (l == NL - 1)
        for e_ in range(NE):
            for r0 in range(0, D, 256):
                P.bgdma("pool", W1B.ap()[e_, r0:r0 + 256, :], m_w1.ap()[l, e_, r0:r0 + 256, :], Wd=[bW1B])
            for r0 in range(0, D, 512):
                P.bgdma("pool", W2B.ap()[e_, r0:r0 + 512, :], m_w2.ap()[l, e_, r0:r0 + 512, :], Wd=[bW2B])
        modst = ExitStack()
        sT = sb(modst, "sT", [128, 8, 2])
        modT = sb(modst, "modT", [128, 48, 2])
        A1 = sb(modst, "A1", [128, 8, 2])
        A2 = sb(modst, "A2", [128, 8, 2])
        bmodv = Buf()
        with phase() as st:
            wm = [sb(st, "wm%d" % i, [128, 8, 768]) for i in range(2)]
            bwm = [Buf(), Buf()]
            psm = ps(st, "psm", [128, 48, 2])
            bpsm = Buf()
            bsT = Buf()
            P.op("act", lambda e: e.activation(out=sT[:, :, 0], in_=V(l, "c", 0, 8), func=AF.Silu), R=[bconst], W=[bsT])
            P.op("act", lambda e: e.activation(out=sT[:, :, 1], in_=V(l, "cctx", 0, 8), func=AF.Silu), R=[bconst], W=[bsT])
            for s in range(8):
                k = s % 2
                P.dma("sp", wm[k][:], w_mod.ap()[l, :, s * 768:(s + 1) * 768].rearrange("(kc p) n -> p kc n", p=128),
                      W=[bwm[k]])
                items = []
                for oc in range(6):
                    for kc in range(8):
                        items.append((psm[:, s * 6 + oc, :], wm[k][:, kc, oc * 128:(oc + 1) * 128], sT[:, kc, :],
                                      kc == 0, kc == 7))
                P.mm(items, R=[bwm[k], bsT], Wd=[bpsm])
            for i in range(2):
                P.op("dve", lambda e, i=i: e.tensor_tensor(out=modT[:, :, i], in0=psm[:, :, i], in1=V(l, "bmod", 0, 48),
                                                          op=ALU.add), R=[bpsm, bconst], W=[bmodv])
                P.op("dve", lambda e, i=i: e.scalar_tensor_tensor(out=A1[:, :, i], in0=modT[:, 8:16, i], scalar=1.0,
                                                                 in1=V(l, "n1g", 0, 8), op0=ALU.add, op1=ALU.mult),
                     R=[bmodv], W=[bmodv])
                P.op("dve", lambda e, i=i: e.scalar_tensor_tensor(out=A2[:, :, i], in0=modT[:, 32:40, i], scalar=1.0,
                                                                 in1=V(l, "n2g", 0, 8), op0=ALU.add, op1=ALU.mult),
                     R=[bmodv], W=[bmodv])

        def MOD(m, j, i):
            return modT[:, m * 8 + j, i:i + 1]

        def norm_chunk(st_tiles, t0, n, seg, Asel, shm, hT, bhT, hF=None):
            xt, sq, rstd, tmp, pss, bxt, bsq, bpss, brs, btmp = st_tiles
            P.dma("sp", xt[:, :, :n], XTv[:, :, t0:t0 + n], R=[bXT], W=[bxt])
            P.op("act", lambda e: e.activation(out=sq[:, :, :n], in_=xt[:, :, :n], func=AF.Square), R=[bxt], W=[bsq])
            P.mm([(pss[:, :n], onesb[:], sq[:, j, :n], j == 0, j == 7) for j in range(8)], R=[bsq, bconst], W=[bpss])
            P.op("act", lambda e: e.activation(out=rstd[:, :n], in_=pss[:, :n], func=AF.Sqrt, scale=1.0 / D, bias=EPS),
                 R=[bpss], W=[brs])
            P.op("dve", lambda e: e.reciprocal(out=rstd[:, :n], in_=rstd[:, :n]), W=[brs])
            for j in range(8):
                P.op("dve", lambda e, j=j: e.scalar_tensor_tensor(out=tmp[:, :n], in0=xt[:, j, :n],
                                                                 scalar=Asel[:, j, seg:seg + 1], in1=rstd[:, :n],
                                                                 op0=ALU.mult, op1=ALU.mult),
                     R=[bxt, brs, bmodv], W=[btmp])
                if hF is not None:
                    P.op("act", lambda e, j=j: e.activation(out=hF[:, j, :n], in_=tmp[:, :n], func=AF.Identity,
                                                           bias=MOD(shm, j, seg)), R=[btmp, bmodv], Wd=[bhT])
                    P.op("pool", lambda e, j=j: e.tensor_copy(out=hT[:, j, :n], in_=hF[:, j, :n]), R=[bhT], Wd=[bhT])
                else:
                    P.op("act", lambda e, j=j: e.activation(out=hT[:, j, :n], in_=tmp[:, :n], func=AF.Identity,
                                                           bias=MOD(shm, j, seg)), R=[btmp, bmodv], Wd=[bhT])

        def norm_tiles(st):
            return (sb(st, "n_xt", [128, 8, 512]), sb(st, "n_sq", [128, 8, 512], BF16), sb(st, "n_rstd", [128, 512]),
                    sb(st, "n_tmp", [128, 512]), ps(st, "n_pss", [128, 512]), Buf(), Buf(), Buf(), Buf(), Buf())

        with phase() as st:
            wi = sb(st, "wi", [128, 8, NPT], BF16)
            bwi = Buf()
            wsrc = w_in.ap()[l].rearrange("(kc p) n -> p kc n", p=128)
            P.dma("pool", wi[:, :, 0:1440], wsrc, Wd=[bwi])
            P.dma("pool", wi[:, :, 1440:1456], wsrc[:, :, 1424:1440], Wd=[bwi])
            P.dma("pool", wi[:, :, 1456:1472], wsrc[:, :, 1408:1424], Wd=[bwi])
            nt = norm_tiles(st)
            hT = sb(st, "hT", [128, 8, 512], BF16)
            bhT = Buf()
            ptsb = sb(st, "ptsb", [128, 12, 512], BF16)
            bptsb = Buf()
            pp = [ps(st, "pp%d" % i, [128, 512]) for i in range(2)]
            bpp = [Buf(), Buf()]
            for (t0, n, seg) in chunks:
                norm_chunk(nt, t0, n, seg, A1, 0, hT, bhT)
                for oc in range(12):
                    r0 = oc * 128
                    nr = min(128, NPT - r0)
                    k = oc % 2
                    P.mm([(pp[k][:nr, :n], wi[:, kc, r0:r0 + nr], hT[:, kc, :n], kc == 0, kc == 7) for kc in range(8)],
                         R=[bwi, bhT], W=[bpp[k]])
                    if k:
                        P.op("act", lambda e, k=k, nr=nr, oc=oc: e.activation(out=ptsb[:nr, oc, :n], in_=pp[k][:nr, :n],
                                                                             func=AF.Copy), R=[bpp[k]], Wd=[bptsb])
                    else:
                        P.op("dve", lambda e, k=k, nr=nr, oc=oc: e.tensor_copy(out=ptsb[:nr, oc, :n], in_=pp[k][:nr, :n]),
                             R=[bpp[k]], Wd=[bptsb])
                c0 = pcol(seg) + (t0 - (0 if seg == 0 else S))
                P.dma("sp", PT.ap()[0:1408, c0:c0 + n].rearrange("(oc p) t -> p oc t", p=128), ptsb[:, 0:11, :n],
                      R=[bptsb], Wd=[bPT])
                P.dma("sp", PT.ap()[1408:1472, c0:c0 + n], ptsb[0:64, 11, :n], R=[bptsb], Wd=[bPT])
                P._wait("act", P._deps([], [bptsb], []))
                P._wait("dve", P._deps([], [bptsb], []))
                bptsb.w = {}

        with phase() as st:
            pw = sb(st, "pw", [128, 2, 128], BF16)
            bpw = Buf()
            P.op("dve", lambda e: e.memset(pw[:], 0.0), W=[bpw])
            for g in range(4):
                p0 = (g % 2) * 64
                P.dma("pool", pw[p0:p0 + 64, g // 2, p0:p0 + 64], pool_w.ap()[l, g], Wd=[bpw])
            u = sb(st, "pl_u", [128, 2, 528], BF16)
            a = sb(st, "pl_a", [128, 528])
            b = sb(st, "pl_b", [128, 528])
            iv = sb(st, "pl_iv", [128, 2, 512])
            dd = sb(st, "pl_d", [128, 512], BF16)
            po = sb(st, "pl_o", [128, 2, 512], BF16)
            pps = ps(st, "pl_ps", [128, 512])
            bu, ba, bb, biv, bdd, bpo, bpps = [Buf() for _ in range(7)]
            for (t0, n, seg) in chunks:
                c0 = pcol(seg) + (t0 - (0 if seg == 0 else S))
                P.dma("sp", u[:, :, :n + 16], PT.ap()[0:256, c0 - 8:c0 + n + 8].rearrange("(cj p) t -> p cj t", p=128),
                      R=[bPT], W=[bu])
                P.dma("sp", iv[:, :, :n], c_invc.ap()[:, :, t0:t0 + n], W=[biv])
                for cj in range(2):
                    for hf in range(2):
                        g = cj * 2 + hf
                        w = POOL_WINDOWS[g]
                        pr = slice(hf * 64, hf * 64 + 64)
                        P.op("dve", lambda e, pr=pr, cj=cj: e.tensor_tensor(out=a[pr, 1:n + 16], in0=u[pr, cj, 0:n + 15],
                                                                          in1=u[pr, cj, 1:n + 16], op=ALU.add),
                             R=[bu], W=[ba])
                        cur, oth, bc_, bo_ = a, b, ba, bb
                        lo, hi = 1, n + 16
                        sh = 1
                        while sh * 2 < w:
                            nlo, nhi = lo + sh, hi - sh
                            P.op("dve", lambda e, pr=pr, cur=cur, oth=oth, nlo=nlo, nhi=nhi, sh=sh: e.tensor_tensor(
                                out=oth[pr, nlo:nhi], in0=cur[pr, nlo - sh:nhi - sh], in1=cur[pr, nlo + sh:nhi + sh],
                                op=ALU.add), R=[bc_], W=[bo_])
                            cur, oth, bc_, bo_ = oth, cur, bo_, bc_
                            lo, hi = nlo, nhi
                            sh *= 2
                        P.op("dve", lambda e, pr=pr, cur=cur, cj=cj: e.tensor_tensor(out=cur[pr, 8:8 + n], in0=cur[pr, 8:8 + n],
                                                                                   in1=iv[pr, cj, :n], op=ALU.mult),
                             R=[biv], W=[bc_])
                        P.op("dve", lambda e, pr=pr, cur=cur, cj=cj: e.tensor_tensor(out=dd[pr, :n], in0=cur[pr, 8:8 + n],
                                                                                   in1=u[pr, cj, 8:8 + n], op=ALU.subtract),
                             R=[bc_, bu], Wd=[bdd])
                    P.mm([(pps[:, :n], pw[:, cj, :], dd[:, :n], True, True)], R=[bpw, bdd], W=[bpps])
                    bdd.w = dict(bdd.w)
                    P.op("act", lambda e, cj=cj: e.activation(out=po[:, cj, :n], in_=pps[:, :n], func=AF.Copy,
                                                             scale=V(l, "psc", cj, 1)), R=[bpps, bconst], Wd=[bpo])
                    P._wait("dve", P._deps([], [bdd], []))
                    bdd.w = {}
                P.dma("sp", MIXT.ap()[0:256, t0:t0 + n].rearrange("(cj p) t -> p cj t", p=128), po[:, :, :n],
                      R=[bpo], Wd=[bMIXT])
                P._wait("act", P._deps([], [bpo], []))
                bpo.w = {}

        with phase() as st:
            fw1 = sb(st, "fw1", [17, 64])
            fw2 = sb(st, "fw2", [64, 64])
            fw3 = sb(st, "fw3", [64, 512])
            fsc = sb(st, "fsc", [64, 4])
            bfw = Buf()
            P.dma("sp", fw1[:], f_w1.ap()[l], Wd=[bfw])
            P.dma("sp", fw2[:], f_w2.ap()[l], Wd=[bfw])
            P.dma("sp", fw3[:], f_w3.ap()[l], Wd=[bfw])
            P.op("dve", lambda e: e.tensor_scalar(out=fsc[:, 0:1], in0=vec[0:64, l, VCOLS["freq"][0]:VCOLS["freq"][0] + 1],
                                                  scalar1=1.0 / 3.0, scalar2=None, op0=ALU.mult), R=[bconst], W=[bfw])
            P.op("dve", lambda e: e.tensor_tensor(out=fsc[:, 1:2], in0=fsc[:, 0:1],
                                                  in1=vec[0:64, l, VCOLS["fb1"][0]:VCOLS["fb1"][0] + 1], op=ALU.mult),
                 R=[bconst], W=[bfw])
            P.op("dve", lambda e: e.tensor_tensor(out=fsc[:, 2:3], in0=fsc[:, 0:1],
                                                  in1=vec[0:64, l, VCOLS["fb2"][0]:VCOLS["fb2"][0] + 1], op=ALU.mult),
                 R=[bconst], W=[bfw])
            zt_ = sb(st, "f_z", [17, 512])
            h1 = sb(st, "f_h1", [64, 512])
            h2 = sb(st, "f_h2", [64, 512])
            s2 = sb(st, "f_s2", [64, 512])
            dfc = sb(st, "f_df", [128, 2, 512])
            dbc = sb(st, "f_db", [128, 2, 512])
            kk = sb(st, "f_k", [128, 2, 512])
            kb = sb(st, "f_kb", [128, 2, 512], BF16)
            p1 = ps(st, "f_p1", [64, 512])
            p3 = [ps(st, "f_p3%d" % i, [128, 512]) for i in range(4)]
            bz_, bh1, bh2, bs2, bdf, bdb, bkk, bkb, bp1 = [Buf() for _ in range(9)]
            bp3 = [Buf() for _ in range(4)]

            def sin3(src_ps, dst, bdst, col):
                P.op("act", lambda e: e.activation(out=dst[:], in_=src_ps[:], func=AF.Sin, scale=fsc[:, 0:1],
                                                   bias=fsc[:, col:col + 1]), R=[bp1, bfw], W=[bdst])
                P.op("dve", lambda e: e.tensor_tensor(out=s2[:], in0=dst[:], in1=dst[:], op=ALU.mult), R=[bdst], W=[bs2])
                P.op("dve", lambda e: e.tensor_scalar(out=s2[:], in0=s2[:], scalar1=-4.0, scalar2=3.0, op0=ALU.mult,
                                                      op1=ALU.add), W=[bs2])
                P.op("dve", lambda e: e.tensor_tensor(out=dst[:], in0=dst[:], in1=s2[:], op=ALU.mult), R=[bs2], W=[bdst])

            for (ztab, dftab, dbtab, KD, bKD, R_, Lseg) in [(c_zl, c_dfl, c_dbl, KDL, bKDL, RL, S),
                                                           (c_zc, c_dfc, c_dbc, KDC, bKDC, RC, CT)]:
                for r0 in range(0, R_, 512):
                    P.dma("sp", zt_[:], ztab.ap()[:, r0:r0 + 512], W=[bz_])
                    P.dma("sp", dfc[:], dftab.ap()[:, r0:r0 + 512].rearrange("(cj p) r -> p cj r", p=128), W=[bdf])
                    P.dma("sp", dbc[:], dbtab.ap()[:, r0:r0 + 512].rearrange("(cj p) r -> p cj r", p=128), W=[bdb])
                    P.mm([(p1[:], fw1[:], zt_[:], True, True)], R=[bfw, bz_], W=[bp1])
                    sin3(p1, h1, bh1, 1)
                    P.mm([(p1[:], fw2[:], h1[:], True, True)], R=[bfw, bh1], W=[bp1])
                    sin3(p1, h2, bh2, 2)
                    for q in range(4):
                        P.mm([(p3[q][:], fw3[:, q * 128:(q + 1) * 128], h2[:], True, True)], R=[bfw, bh2], W=[bp3[q]])
                    for cj in range(2):
                        P.op("dve", lambda e, cj=cj: e.tensor_tensor(out=kk[:, cj, :], in0=p3[cj][:], in1=dfc[:, cj, :],
                                                                    op=ALU.mult), R=[bp3[cj], bdf], Wd=[bkk])
                        P.op("dve", lambda e, cj=cj: e.tensor_tensor(out=dbc[:, cj, :], in0=p3[2 + cj][:], in1=dbc[:, cj, :],
                                                                    op=ALU.mult), R=[bp3[2 + cj]], Wd=[bdb])
                        P.op("pool", lambda e, cj=cj: e.tensor_tensor(out=kk[:, cj, :], in0=kk[:, cj, :], in1=dbc[:, cj, :],
                                                                     op=ALU.add), R=[bdb], W=[bkk])
                        m0 = Lseg - 1
                        if r0 <= m0 < r0 + 512:
                            P.op("pool", lambda e, cj=cj, m0=m0, r0=r0: e.tensor_tensor(
                                out=kk[:, cj, m0 - r0:m0 - r0 + 1], in0=kk[:, cj, m0 - r0:m0 - r0 + 1],
                                in1=V(l, "hyb", cj, 1), op=ALU.add), R=[bconst], W=[bkk])
                        P.op("act", lambda e, cj=cj: e.activation(out=kb[:, cj, :], in_=kk[:, cj, :], func=AF.Copy),
                             R=[bkk], Wd=[bkb])
                    P.dma("sp", KD.ap()[:, r0:r0 + 512].rearrange("(cj p) r -> p cj r", p=128), kb[:], R=[bkb], Wd=[bKD])
                    for en in ("act",):
                        P._wait(en, P._deps([], [bkb], []))
                    bkb.w = {}
                    P._wait("dve", P._deps([], [bkk, bdb], []))
                    bkk.w = {}
                    bdb.w = dict(bdb.w)

        for (seg, Lseg, KD, bKD, tbase) in [(0, S, KDL, bKDL, 0), (1, CT, KDC, bKDC, S)]:
            if seg == 1 and last:
                continue
            NB = Lseg // 128
            with phase() as st:
                U = sb(st, "hy_U", [128, 256, NB], BF16)
                bU = Buf()
                with phase() as st2:
                    hin = sb(st2, "hy_in", [128, 6, 514], BF16)
                    hc = sb(st2, "hy_c", [128, 6, 512])
                    x0b = sb(st2, "hy_x0b", [128, 2, 512], BF16)
                    ub = sb(st2, "hy_ub", [128, 2, 512], BF16)
                    ptp = ps(st2, "hy_ptp", [128, 4, 128], BF16)
                    bhin, bhc, bx0b, bub, bptp = [Buf() for _ in range(5)]
                    for t0 in range(0, Lseg, 512):
                        n = min(512, Lseg - t0)
                        c0 = pcol(seg) + t0
                        P.dma("sp", hin[:, :, :n + 2],
                              PT.ap()[256:1024, c0 - 1:c0 + n + 1].rearrange("(cj p) t -> p cj t", p=128), R=[bPT], W=[bhin])
                        for cj in range(6):
                            eng = "dve" if cj % 2 == 0 else "pool"
                            o = VCOLS["hcw"][0]
                            P.op("dve", lambda e, cj=cj: e.tensor_scalar(out=hc[:, cj, :n], in0=hin[:, cj, 0:n],
                                                                        scalar1=V(l, "hcw", 0 * 6 + cj, 1),
                                                                        scalar2=V(l, "hcb", cj, 1), op0=ALU.mult,
                                                                        op1=ALU.add), R=[bhin, bconst], Wd=[bhc])
                            for tap in (1, 2):
                                P.op("dve", lambda e, cj=cj, tap=tap: e.scalar_tensor_tensor(
                                    out=hc[:, cj, :n], in0=hin[:, cj, tap:tap + n], scalar=V(l, "hcw", tap * 6 + cj, 1),
                                    in1=hc[:, cj, :n], op0=ALU.mult, op1=ALU.add), R=[bhin], Wd=[bhc])
                        for cj in range(2):
                            P.op("act", lambda e, cj=cj: e.activation(out=x0b[:, cj, :n], in_=hc[:, cj, :n], func=AF.Copy),
                                 R=[bhc], Wd=[bx0b])
                            P.op("pool", lambda e, cj=cj: e.tensor_tensor(out=ub[:, cj, :n], in0=hc[:, 2 + cj, :n],
                                                                         in1=hc[:, 4 + cj, :n], op=ALU.mult),
                                 R=[bhc], Wd=[bub])
                        P.dma("sp", X0C.ap()[:, tbase + t0:tbase + t0 + n].rearrange("(cj p) t -> p cj t", p=128),
                              x0b[:, :, :n], R=[bx0b], Wd=[bX0C])
                        for cj in range(2):
                            nb_ = n // 128
                            P._wait("pe", P._deps([bub, bconst], [bptp], []))
                            ins = None
                            for q in range(nb_):
                                ins = nc.tensor.transpose(ptp[:, q, :], ub[:, cj, q * 128:(q + 1) * 128], idb[:])
                            P.cnt["pe"] += 1
                            ins.then_inc(P.sem["pe"], 1)
                            P._commit(("e", "pe"), P.cnt["pe"], [bub, bconst], [bptp], [])
                            P.op("act", lambda e, cj=cj, nb_=nb_, t0=t0: e.activation(
                                out=U[:, cj * 128:(cj + 1) * 128, t0 // 128:t0 // 128 + nb_].rearrange("p c q -> p q c"),
                                in_=ptp[:, 0:nb_, :], func=AF.Copy), R=[bptp], Wd=[bU])
                        P._wait("dve", P._deps([], [bhc], []))
                        P._wait("act", P._deps([], [bx0b], []))
                        P._wait("pool", P._deps([], [bub], []))
                        bhc.w = {}
                        bx0b.w = {}
                        bub.w = {}
                KW = 2 * Lseg
                ksh = [sb(st, "hy_ks%d" % i, [128, KW], BF16) for i in range(2)]
                bks = [Buf(), Buf()]
                ytok = sb(st, "hy_y", [128, NB, 128], BF16)
                bytk = Buf()
                pcv = [ps(st, "hy_pc%d" % i, [128, NB]) for i in range(2)]
                bpcv = [Buf(), Buf()]
                pyt = [ps(st, "hy_pt%d" % i, [128, 512]) for i in range(2)]
                bpyt = [Buf(), Buf()]
                x0l = sb(st, "hy_x0l", [128, 512], BF16)
                yo = sb(st, "hy_yo", [128, 512], BF16)
                bx0l, byo = Buf(), Buf()
                for cj in range(2):
                    for cc in range(128):
                        c = cj * 128 + cc
                        k = c % 2
                        src = bass.AP(KD, c * (KD.shape[1]), [[1, 128], [1, KW]])
                        P.dma("sp", ksh[k][:], src, R=[bKD], W=[bks[k]])
                        items = []
                        dl = [0] + [d for d in range(-(NB - 1), NB) if d != 0]
                        for qi, d in enumerate(dl):
                            i0, i1 = max(0, d), min(NB - 1, NB - 1 + d)
                            ns = Lseg - 128 - 128 * d
                            items.append((pcv[k][:, i0:i1 + 1], ksh[k][:, ns:ns + 128], U[:, c, i0 - d:i1 + 1 - d],
                                          qi == 0, qi == len(dl) - 1))
                        P.mm(items, R=[bks[k], bU], W=[bpcv[k]])
                        if c % 2:
                            P.op("act", lambda e, k=k, cc=cc: e.activation(out=ytok[:, :, cc], in_=pcv[k][:, :], func=AF.Copy),
                                 R=[bpcv[k]], Wd=[bytk])
                        else:
                            P.op("dve", lambda e, k=k, cc=cc: e.tensor_copy(out=ytok[:, :, cc], in_=pcv[k][:, :]),
                                 R=[bpcv[k]], Wd=[bytk])
                    for t0 in range(0, Lseg, 512):
                        n = min(512, Lseg - t0)
                        kq = (t0 // 512) % 2
                        P.dma("sp", x0l[:, :n], X0C.ap()[cj * 128:(cj + 1) * 128, tbase + t0:tbase + t0 + n], R=[bX0C], W=[bx0l])
                        P.mm([(pyt[kq][:, q * 128:(q + 1) * 128], ytok[:, t0 // 128 + q, :], anti[:], True, True)
                              for q in range(n // 128)], R=[bytk, bconst], W=[bpyt[kq]])
                        P.op("dve", lambda e, kq=kq: e.tensor_tensor(out=yo[:, :n], in0=pyt[kq][:, :n], in1=x0l[:, :n],
                                                                    op=ALU.mult), R=[bpyt[kq], bx0l], W=[byo])
                        P.dma("sp", MIXT.ap()[256 + cj * 128:256 + (cj + 1) * 128, tbase + t0:tbase + t0 + n], yo[:, :n],
                              R=[byo], Wd=[bMIXT])
                    P._wait("act", P._deps([], [bytk], []))
                    P._wait("dve", P._deps([], [bytk], []))
                    bytk.w = {}

        with phase() as st:
            wq = sb(st, "a_wq", [128, 2, 8, 128], BF16)
            wqs = sb(st, "a_wqs", [128, 2, 8, 32], BF16)
            wk = sb(st, "a_wk", [128, 8, 128], BF16)
            wv = sb(st, "a_wv", [128, 8, 64], BF16)
            bw = Buf()
            P.op("dve", lambda e: e.memset(wq[:], 0.0), W=[bw])
            P.op("dve", lambda e: e.memset(wk[:], 0.0), W=[bw])
            for kc in range(2):
                srcq = w_uq.ap()[l, kc * 128:(kc + 1) * 128, :].rearrange("p (h d) -> p h d", d=96)
                P.dma("pool", wq[:, kc, :, 0:32], srcq[:, :, 64:96], Wd=[bw])
                P.dma("pool", wq[:, kc, :, 64:128], srcq[:, :, 0:64], Wd=[bw])
                P.dma("pool", wqs[:, kc, :, 0:16], srcq[:, :, 80:96], Wd=[bw])
                P.dma("pool", wqs[:, kc, :, 16:32], srcq[:, :, 64:80], Wd=[bw])
            srck = w_ukv.ap()[l].rearrange("p (h d) -> p h d", d=128)
            P.dma("pool", wk[:, :, 64:128], srck[:, :, 0:64], Wd=[bw])
            P.dma("pool", wv[:, :, :], srck[:, :, 64:128], Wd=[bw])
            gsc = sb(st, "a_gsc", [128, 1])
            P.op("dve", lambda e: e.tensor_scalar(out=gsc[:], in0=V(l, "gqm"), scalar1=ATTN_SCALE, scalar2=None,
                                                  op0=ALU.mult), R=[bconst], W=[bw])
            gscs = sb(st, "a_gscs", [128, 1])
            P.op("dve", lambda e: e.tensor_scalar(out=gscs[:], in0=V(l, "gqs"), scalar1=ATTN_SCALE, scalar2=None,
                                                  op0=ALU.mult), R=[bconst], W=[bw])
            lat = sb(st, "a_lat", [128, 3, 512], BF16)
            sq = sb(st, "a_sq", [128, 3, 512], BF16)
            latn = sb(st, "a_latn", [128, 3, 512], BF16)
            rs = sb(st, "a_rs", [128, 512])
            cs = sb(st, "a_cos", [32, 512])
            sn = sb(st, "a_sin", [32, 512])
            kr = sb(st, "a_kr", [32, 2, 512], BF16)
            hm = sb(st, "a_hm", [128, 512])
            hs = sb(st, "a_hs", [32, 512])
            hsq = sb(st, "a_hsq", [128, 512], BF16)
            hrs = sb(st, "a_hrs", [128, 512])
            ho = sb(st, "a_ho", [128, 8, 512], BF16)
            vo = sb(st, "a_vo", [128, 4, 8, 64], BF16)
            pss = ps(st, "a_pss", [128, 512])
            pm = ps(st, "a_pm", [128, 512])
            psw = ps(st, "a_psw", [32, 512])
            pss2 = ps(st, "a_pss2", [128, 512])
            pv = ps(st, "a_pv", [128, 512])
            blat, bsq, blatn, brs, bcs, bkr, bhm, bhs, bhsq, bhrs, bho, bvo, bpss, bpm, bpsw, bpss2, bpv = [Buf() for _ in range(17)]

            def latent_norm(rows, nchunk, gname, n, c0):
                P.dma("sp", lat[:, 0:nchunk, :n], PT.ap()[rows:rows + nchunk * 128, c0:c0 + n].rearrange("(j p) t -> p j t", p=128),
                      R=[bPT], W=[blat])
                P.op("act", lambda e: e.activation(out=sq[:, 0:nchunk, :n], in_=lat[:, 0:nchunk, :n], func=AF.Square),
                     R=[blat], W=[bsq])
                P.mm([(pss[:, :n], onesb[:], sq[:, j, :n], j == 0, j == nchunk - 1) for j in range(nchunk)],
                     R=[bsq, bconst], W=[bpss])
                P.op("act", lambda e: e.activation(out=rs[:, :n], in_=pss[:, :n], func=AF.Sqrt, scale=1.0 / (128 * nchunk),
                                                   bias=EPS), R=[bpss], W=[brs])
                P.op("dve", lambda e: e.reciprocal(out=rs[:, :n], in_=rs[:, :n]), W=[brs])
                for j in range(nchunk):
                    P.op("dve", lambda e, j=j: e.scalar_tensor_tensor(out=latn[:, j, :n], in0=lat[:, j, :n],
                                                                     scalar=V(l, gname, j, 1), in1=rs[:, :n], op0=ALU.mult,
                                                                     op1=ALU.mult), R=[blat, brs, bconst], Wd=[blatn])

            def head_finish(h, n, t0, gm, gs, sw_from_psum, DST, bDST):
                P.op("act", lambda e: e.activation(out=hsq[:, :n], in_=hm_src[0][:, :n], func=AF.Square), R=[hm_src[1]], W=[bhsq])
                if sw_from_psum:
                    P.op("dve", lambda e: e.memset(hsq[32:64, :n], 0.0), W=[bhsq])
                P.mm([(pss2[:, :n], onesb[:], hsq[:, :n], True, True)], R=[bhsq, bconst], W=[bpss2])
                P.op("act", lambda e: e.activation(out=hrs[:, :n], in_=pss2[:, :n], func=AF.Sqrt, scale=1.0 / 96.0, bias=EPS),
                     R=[bpss2], W=[bhrs])
                P.op("dve", lambda e: e.reciprocal(out=hrs[:, :n], in_=hrs[:, :n]), W=[bhrs])
                P.op("dve", lambda e: e.scalar_tensor_tensor(out=hm[:, :n], in0=hm_src[0][:, :n], scalar=gm, in1=hrs[:, :n],
                                                             op0=ALU.mult, op1=ALU.mult), R=[hm_src[1], bhrs, bw], W=[bhm])
                P.op("dve", lambda e: e.scalar_tensor_tensor(out=hs[:, :n], in0=sw_src[0][0:32, :n], scalar=gs[0:32, :],
                                                             in1=hrs[0:32, :n], op0=ALU.mult, op1=ALU.mult),
                     R=[sw_src[1], bhrs, bw], W=[bhs])
                P.op("dve", lambda e: e.tensor_tensor(out=hm[0:32, :n], in0=hm[0:32, :n], in1=cs[:, :n], op=ALU.mult),
                     R=[bcs], W=[bhm])
                P.op("dve", lambda e: e.tensor_tensor(out=hs[:, :n], in0=hs[:, :n], in1=sn[:, :n], op=ALU.mult),
                     R=[bcs], W=[bhs])
                P.op("dve", lambda e: e.tensor_tensor(out=hm[0:32, :n], in0=hm[0:32, :n], in1=hs[:, :n], op=ALU.add),
                     R=[bhs], W=[bhm])
                P.op("act", lambda e: e.activation(out=ho[:, h, :n], in_=hm[:, :n], func=AF.Copy), R=[bhm], Wd=[bho])

            hm_src = [None, None]
            sw_src = [None, None]
            for (t0, n, seg) in chunks:
                c0 = pcol(seg) + (t0 - (0 if seg == 0 else S))
                P.dma("sp", cs[:, :n], c_cos.ap()[:, t0:t0 + n], W=[bcs])
                P.dma("sp", sn[:, :n], c_sin.ap()[:, t0:t0 + n], Wd=[bcs])
                latent_norm(1280, 1, "kvng", n, c0)
                P.dma("sp", kr[:, :, :n], PT.ap()[1408:1472, c0:c0 + n].rearrange("(a p) t -> p a t", p=32), R=[bPT], W=[bkr])
                P.mm([(pv[:, :].rearrange("p (a f) -> p a f", f=512)[:, 0, :] if False else pv[:, :],
                       latn[:, 0, q * 128:(q + 1) * 128], wv[:, :, :].rearrange("p h d -> p (h d)"), True, True)
                      for q in range(0)], R=[], W=[]) if False else None
                for q in range(n // 128):
                    P.mm([(pv[:, :], latn[:, 0, q * 128:(q + 1) * 128], wv[:, :, :].rearrange("p h d -> p (h d)"), True, True)],
                         R=[blatn, bw], W=[bpv])
                    P.op("act", lambda e, q=q: e.activation(out=vo[:, q, :, :].rearrange("p h d -> p (h d)"), in_=pv[:, :],
                                                           func=AF.Copy), R=[bpv], Wd=[bvo])
                for q in range(n // 128):
                    P.dma("sp", VT.ap()[:, t0 + q * 128:t0 + (q + 1) * 128, :].rearrange("h p d -> p h d"), vo[:, q, :, :],
                          R=[bvo], Wd=[bVT])
                for h in range(8):
                    P.mm([(pm[:, :n], wk[:, h, :], latn[:, 0, :n], True, True)], R=[blatn, bw], W=[bpm])
                    P.op("act", lambda e: e.activation(out=hm[64:128, :n], in_=pm[64:128, :n], func=AF.Copy), R=[bpm], W=[bhm])
                    P.op("pool", lambda e: e.tensor_copy(out=hm[0:32, :n], in_=kr[:, 0, :n]), R=[bkr], W=[bhm])
                    P.op("pool", lambda e: e.memset(hm[32:64, :n], 0.0), W=[bhm])
                    hm_src[0], hm_src[1] = hm, bhm
                    sw_src[0], sw_src[1] = kr[:, 1, :], bkr
                    head_finish(h, n, t0, V(l, "gkm"), V(l, "gks"), False, KT, bKT)
                P.dma("sp", KT.ap()[:, :, t0:t0 + n].rearrange("h p t -> p h t"), ho[:, :, :n], R=[bho], Wd=[bKT])
                P._wait("act", P._deps([], [bho, bvo], []))
                bho.w = {}
                bvo.w = {}
                if seg == 1 and last:
                    continue
                latent_norm(1024, 2, "qng", n, c0)
                for h in range(8):
                    P.mm([(pm[:, :n], wq[:, kc, h, :], latn[:, kc, :n], kc == 0, kc == 1) for kc in range(2)],
                         R=[blatn, bw], W=[bpm])
                    P.mm([(psw[:, :n], wqs[:, kc, h, :], latn[:, kc, :n], kc == 0, kc == 1) for kc in range(2)],
                         R=[blatn, bw], W=[bpsw])
                    hm_src[0], hm_src[1] = pm, bpm
                    sw_src[0], sw_src[1] = psw, bpsw
                    head_finish(h, n, t0, gsc[:, :], gscs, True, QT, bQT)
                P.dma("sp", QT.ap()[:, :, t0:t0 + n].rearrange("h p t -> p h t"), ho[:, :, :n], R=[bho], Wd=[bQT])
                P._wait("act", P._deps([], [bho], []))
                bho.w = {}

        with phase() as st:
            NKT = T // 128
            kh = [sb(st, "at_k%d" % i, [128, T], BF16) for i in range(2)]
            vh = [sb(st, "at_v%d" % i, [128, NKT, 64], BF16) for i in range(2)]
            bkh = [Buf(), Buf()]
            qc = [sb(st, "at_q%d" % i, [128, 512], BF16) for i in range(2)]
            bqc = [Buf(), Buf()]
            et = [sb(st, "at_e%d" % i, [128, 512], BF16) for i in range(3)]
            bet = [Buf() for _ in range(3)]
            pS = [ps(st, "at_ps%d" % i, [128, 512]) for i in range(3)]
            bpS = [Buf() for _ in range(3)]
            pO = [ps(st, "at_po%d" % i, [64, 512]) for i in range(2)]
            pD = [ps(st, "at_pd%d" % i, [64, 512]) for i in range(2)]
            bpO = [Buf(), Buf()]
            rc = sb(st, "at_rc", [64, 512])
            oo = sb(st, "at_oo", [64, 512], BF16)
            brc, boo = Buf(), Buf()
            cnt = 0
            qi = 0
            for h in range(8):
                hk = h % 2
                P.dma("sp", kh[hk][:], KT.ap()[h], R=[bKT], W=[bkh[hk]])
                P.dma("sp", vh[hk][:], VT.ap()[h].rearrange("(q p) d -> p q d", p=128), R=[bVT], Wd=[bkh[hk]])
                for (t0, n, seg) in chunks:
                    if seg == 1 and last:
                        continue
                    kts = list(range(S // 128, NKT)) + (list(range(0, S // 128)) if seg == 0 else [])
                    q_ = qi % 2
                    qi += 1
                    P.dma("sp", qc[q_][:, :n], QT.ap()[h, :, t0:t0 + n], R=[bQT], W=[bqc[q_]])
                    for ki, kt in enumerate(kts):
                        s_ = cnt % 3
                        cnt += 1
                        P.mm([(pS[s_][:, :n], kh[hk][:, kt * 128:(kt + 1) * 128], qc[q_][:, :n], True, True)],
                             R=[bkh[hk], bqc[q_]], W=[bpS[s_]])
                        P.op("act", lambda e, s_=s_: e.activation(out=et[s_][:, :n], in_=pS[s_][:, :n], func=AF.Exp),
                             R=[bpS[s_]], W=[bet[s_]])
                        P.mm([(pO[q_][:, :n], vh[hk][:, kt, :], et[s_][:, :n], ki == 0, ki == len(kts) - 1),
                              (pD[q_][:, :n], onesb[:, 0:64], et[s_][:, :n], ki == 0, ki == len(kts) - 1)],
                             R=[bet[s_], bkh[hk], bconst], Wd=[bpO[q_]] if ki else (), W=[bpO[q_]] if ki == 0 else ())
                    P.op("dve", lambda e, q_=q_: e.reciprocal(out=rc[:, :n], in_=pD[q_][:, :n]), R=[bpO[q_]], W=[brc])
                    P.op("dve", lambda e, q_=q_: e.tensor_tensor(out=oo[:, :n], in0=pO[q_][:, :n], in1=rc[:, :n], op=ALU.mult),
                         R=[bpO[q_], brc], W=[boo])
                    P.dma("sp", MIXT.ap()[512 + h * 64:512 + (h + 1) * 64, t0:t0 + n], oo[:, :n], R=[boo], Wd=[bMIXT])

        with phase() as st:
            wo = sb(st, "o_w", [128, 8, D], BF16)
            rw = sb(st, "o_rw", [128, 8, NE])
            rb = sb(st, "o_rb", [1, NE])
            bwo = Buf()
            P.dma("pool", wo[:], w_out.ap()[l].rearrange("(kc p) n -> p kc n", p=128), Wd=[bwo])
            P.dma("sp", rw[:], r_w.ap()[l].rearrange("(kc p) n -> p kc n", p=128), Wd=[bwo])
            P.dma("sp", rb[:], r_b.ap()[l], Wd=[bwo])
            mx = sb(st, "o_mx", [128, 8, 512], BF16)
            xt = sb(st, "o_xt", [128, 8, 512])
            bmx, bxt = Buf(), Buf()
            po_ = [ps(st, "o_ps%d" % i, [128, 512]) for i in range(2)]
            bpo_ = [Buf(), Buf()]
            nt = norm_tiles(st)
            h2 = sb(st, "o_h2", [128, 8, 512], BF16)
            h2f = sb(st, "o_h2f", [128, 8, 512])
            bh2 = Buf()
            plg = ps(st, "o_plg", [128, NE])
            pgt = ps(st, "o_pgt", [NE, 512])
            lg = sb(st, "o_lg", [128, NE])
            m8 = sb(st, "o_m8", [128, 8])
            msk = sb(st, "o_msk", [128, NE])
            ex = sb(st, "o_ex", [128, NE])
            ssum = sb(st, "o_ss", [128, 2])
            gts = sb(st, "o_gts", [NE, 512])
            bplg, bpgt, blg, bm8, bmsk, bex, bss, bgts = [Buf() for _ in range(8)]
            for (t0, n, seg) in chunks:
                if seg == 1 and last:
                    continue
                P.dma("sp", mx[:, :, :n], MIXT.ap()[:, t0:t0 + n].rearrange("(kc p) t -> p kc t", p=128), R=[bMIXT], W=[bmx])
                P.dma("sp", xt[:, :, :n], XTv[:, :, t0:t0 + n], R=[bXT], W=[bxt])
                for j in range(8):
                    k = j % 2
                    P.mm([(po_[k][:, :n], wo[:, kc, j * 128:(j + 1) * 128], mx[:, kc, :n], kc == 0, kc == 7) for kc in range(8)],
                         R=[bwo, bmx], W=[bpo_[k]])
                    P.op("dve", lambda e, j=j, k=k: e.scalar_tensor_tensor(out=xt[:, j, :n], in0=po_[k][:, :n],
                                                                          scalar=MOD(2, j, seg), in1=xt[:, j, :n],
                                                                          op0=ALU.mult, op1=ALU.add),
                         R=[bpo_[k], bmodv], W=[bxt])
                P.dma("sp", XTv[:, :, t0:t0 + n], xt[:, :, :n], R=[bxt], Wd=[bXT])
                norm_chunk(nt, t0, n, seg, A2, 3, h2, bh2, hF=h2f)
                P.dma("sp", H2T.ap()[:, t0:t0 + n].rearrange("(kc p) t -> p kc t", p=128), h2[:, :, :n], R=[bh2], Wd=[bH2T])
                for q in range(n // 128):
                    items = [(plg[:, :], h2f[:, kc, q * 128:(q + 1) * 128], rw[:, kc, :], kc == 0, False) for kc in range(8)]
                    items.append((plg[:, :], onesf[0:1, :], rb[0:1, :], False, True))
                    P.mm(items, R=[bh2, bwo, bconst], W=[bplg])
                    P.op("dve", lambda e: e.tensor_copy(out=lg[:], in_=plg[:]), R=[bplg], W=[blg])
                    P.op("dve", lambda e: e.max(out=m8[:], in_=lg[:]), R=[blg], W=[bm8])
                    P.op("dve", lambda e: e.tensor_scalar(out=msk[:], in0=lg[:], scalar1=m8[:, 3:4], scalar2=None,
                                                          op0=ALU.is_ge), R=[blg, bm8], W=[bmsk])
                    P.op("dve", lambda e: e.tensor_scalar(out=ssum[:, 1:2], in0=m8[:, 0:1], scalar1=-1.0, scalar2=None,
                                                          op0=ALU.mult), R=[bm8], W=[bss])
                    P.op("act", lambda e: e.activation(out=ex[:], in_=lg[:], func=AF.Exp, bias=ssum[:, 1:2]),
                         R=[blg, bss], W=[bex])
                    P.op("dve", lambda e: e.tensor_tensor(out=ex[:], in0=ex[:], in1=msk[:], op=ALU.mult), R=[bmsk], W=[bex])
                    P.op("dve", lambda e: e.reduce_sum(out=ssum[:, 0:1], in_=ex[:], axis=AX.X), R=[bex], W=[bss])
                    P.op("dve", lambda e: e.reciprocal(out=ssum[:, 0:1], in_=ssum[:, 0:1]), W=[bss])
                    P.op("dve", lambda e: e.tensor_scalar(out=ex[:], in0=ex[:], scalar1=ssum[:, 0:1], scalar2=None,
                                                          op0=ALU.mult), R=[bss], W=[bex])
                    P._wait("pe", P._deps([bex, bconst], [], [bpgt]))
                    ins = nc.tensor.transpose(pgt[:, q * 128:(q + 1) * 128], ex[:], idf[:])
                    P.cnt["pe"] += 1
                    ins.then_inc(P.sem["pe"], 1)
                    P._commit(("e", "pe"), P.cnt["pe"], [bex, bconst], [], [bpgt])
                P.op("act", lambda e: e.activation(out=gts[:, :n], in_=pgt[:, :n], func=AF.Copy), R=[bpgt], W=[bgts])
                P.dma("sp", GT.ap()[:, t0:t0 + n], gts[:, :n], R=[bgts], Wd=[bGT])
                P._wait("pe", P._deps([], [bpgt], []))
                bpgt.w = {}
                P._wait("act", P._deps([], [bh2], []))
                P._wait("pool", P._deps([], [bh2], []))
                bh2.w = {}

        with phase() as st:
            TG = 1024
            NSL = 5
            wsl = [sb(st, "m_w%d" % i, [128, 8, 1024], BF16) for i in range(NSL)]
            bws = [Buf() for _ in range(NSL)]
            b2 = sb(st, "m_b2", [NE, D], BF16)
            sel = sb(st, "m_sel", [NE, NE, 128], BF16)
            bb2 = Buf()
            P.dma("pool", b2[:], m_b2.ap()[l], Wd=[bb2])
            P.op("dve", lambda e: e.memset(sel[:], 0.0), W=[bb2])
            for e_ in range(NE):
                pass
            P.op("dve", lambda e: e.tensor_tensor(out=sel[:], in0=sel[:],
                                                  in1=idf[0:NE, 0:NE].unsqueeze(2).to_broadcast([NE, NE, 128]), op=ALU.add),
                 R=[bconst], W=[bb2])
            hh = sb(st, "m_h", [128, 8, TG], BF16)
            acc = sb(st, "m_acc", [128, 8, TG])
            at = sb(st, "m_at", [128, 8, TG], BF16)
            gt = sb(st, "m_gt", [NE, TG], BF16)
            gtf = sb(st, "m_gtf", [NE, TG])
            xt = sb(st, "m_xt", [128, 512])
            bhh, bacc, bat, bgt, bxt = [Buf() for _ in range(5)]
            glu = [sb(st, "m_glu%d" % i, [128, 512]) for i in range(2)]
            sg = [sb(st, "m_sg%d" % i, [128, 512]) for i in range(2)]
            ln = [sb(st, "m_ln%d" % i, [128, 512]) for i in range(2)]
            bglu = [Buf(), Buf()]
            bsg = [Buf(), Buf()]
            bln = [Buf(), Buf()]
            pg = [ps(st, "m_pg%d" % i, [128, 512]) for i in range(2)]
            pl = [ps(st, "m_pl%d" % i, [128, 512]) for i in range(2)]
            py = [ps(st, "m_py%d" % i, [128, 512]) for i in range(2)]
            pbc = [ps(st, "m_pbc%d" % i, [128, 512]) for i in range(2)]
            bpg, bpl, bpy, bpbc = [[Buf(), Buf()] for _ in range(4)]
            Tm = T if not last else S
            slot = 0
            ycnt = 0
            tcnt = 0
            for g0 in range(0, Tm, TG):
                ng = min(TG, Tm - g0)
                halves = [(hs_, min(512, ng - hs_)) for hs_ in range(0, ng, 512)]
                P.dma("sp", hh[:, :, :ng], H2T.ap()[:, g0:g0 + ng].rearrange("(kc p) t -> p kc t", p=128), R=[bH2T], W=[bhh])
                P.dma("sp", gtf[:, :ng], GT.ap()[:, g0:g0 + ng], R=[bGT], W=[bgt])
                P.op("pool", lambda e: e.tensor_copy(out=gt[:, :ng], in_=gtf[:, :ng]), W=[bgt])
                for ex_ in range(NE):
                    s1, s2_, s3 = slot % NSL, (slot + 1) % NSL, (slot + 2) % NSL
                    slot += 3
                    w1src = W1B.ap()[ex_].rearrange("(kc p) n -> p kc n", p=128)
                    P.dma("sp", wsl[s1][:], w1src[:, :, 0:1024], R=[bW1B], W=[bws[s1]])
                    P.dma("sp", wsl[s2_][:], w1src[:, :, 1024:2048], R=[bW1B], W=[bws[s2_]])
                    P.dma("sp", wsl[s3][:], W2B.ap()[ex_].rearrange("(kc p) n -> p kc n", p=128), R=[bW2B], W=[bws[s3]])
                    b1o = VCOLS["b1"][0] + ex_ * 16
                    for (hs_, hn) in halves:
                        hsl = slice(hs_, hs_ + hn)
                        kb_ = tcnt % 2
                        P.mm([(pbc[kb_][:, :hn], sel[:, ex_, :], gt[:, hsl], True, True)], R=[bb2, bgt], W=[bpbc[kb_]])
                        for j in range(8):
                            k = tcnt % 2
                            tcnt += 1
                            P.mm([(pg[k][:, :hn], wsl[s1][:, kc, j * 128:(j + 1) * 128], hh[:, kc, hsl], kc == 0, kc == 7)
                                  for kc in range(8)], R=[bws[s1], bhh], W=[bpg[k]])
                            P.mm([(pl[k][:, :hn], wsl[s2_][:, kc, j * 128:(j + 1) * 128], hh[:, kc, hsl], kc == 0, kc == 7)
                                  for kc in range(8)], R=[bws[s2_], bhh], W=[bpl[k]])
                            bg_ = vec[:, l, b1o + j:b1o + j + 1]
                            bl_ = vec[:, l, b1o + 8 + j:b1o + 8 + j + 1]
                            P.op("dve", lambda e, k=k, bg_=bg_: e.tensor_scalar(out=glu[k][:, :hn], in0=pg[k][:, :hn], scalar1=bg_,
                                                                               scalar2=7.0, op0=ALU.add, op1=ALU.min),
                                 R=[bpg[k], bconst], W=[bglu[k]])
                            P.op("act", lambda e, k=k: e.activation(out=sg[k][:, :hn], in_=glu[k][:, :hn], func=AF.Sigmoid,
                                                                   scale=1.702), R=[bglu[k]], W=[bsg[k]])
                            P.op("dve", lambda e, k=k, bl_=bl_: e.tensor_scalar(out=ln[k][:, :hn], in0=pl[k][:, :hn], scalar1=bl_,
                                                                               scalar2=7.0, op0=ALU.add, op1=ALU.min),
                                 R=[bpl[k], bconst], W=[bln[k]])
                            P.op("pool", lambda e, k=k: e.tensor_scalar(out=ln[k][:, :hn], in0=ln[k][:, :hn], scalar1=-7.0,
                                                                       scalar2=1.0, op0=ALU.max, op1=ALU.add), W=[bln[k]])
                            P.op("pool", lambda e, k=k: e.tensor_tensor(out=glu[k][:, :hn], in0=glu[k][:, :hn], in1=sg[k][:, :hn],
                                                                       op=ALU.mult), R=[bsg[k]], W=[bglu[k]])
                            P.op("pool", lambda e, k=k: e.tensor_tensor(out=glu[k][:, :hn], in0=glu[k][:, :hn], in1=ln[k][:, :hn],
                                                                       op=ALU.mult), R=[bln[k]], W=[bglu[k]])
                            P.op("dve", lambda e, k=k, j=j, kb_=kb_: e.tensor_tensor(out=at[:, j, hsl], in0=glu[k][:, :hn],
                                                                                    in1=pbc[kb_][:, :hn], op=ALU.mult),
                                 R=[bglu[k], bpbc[kb_]], Wd=[bat])
                        for j in range(8):
                            k = ycnt % 2
                            ycnt += 1
                            items = [(py[k][:, :hn], wsl[s3][:, kc, j * 128:(j + 1) * 128], at[:, kc, hsl], kc == 0, False)
                                     for kc in range(8)]
                            if ex_ == 0:
                                items.append((py[k][:, :hn], b2[:, j * 128:(j + 1) * 128], gt[:, hsl], False, True))
                            else:
                                o_, l_, r_, st_, _ = items[-1]
                                items[-1] = (o_, l_, r_, st_, True)
                            P.mm(items, R=[bws[s3], bat, bb2, bgt], W=[bpy[k]])
                            if ex_ == 0:
                                P.op("act", lambda e, k=k, j=j: e.activation(out=acc[:, j, hsl], in_=py[k][:, :hn], func=AF.Copy),
                                     R=[bpy[k]], Wd=[bacc])
                            else:
                                P.op("dve", lambda e, k=k, j=j: e.tensor_tensor(out=acc[:, j, hsl], in0=acc[:, j, hsl],
                                                                               in1=py[k][:, :hn], op=ALU.add),
                                     R=[bpy[k]], Wd=[bacc])
                        P._wait("dve", P._deps([], [bat], []))
                        bat.w = {}
                for (hs_, hn) in halves:
                    t0 = g0 + hs_
                    seg = 0 if t0 < S else 1
                    for j in range(8):
                        P.dma("sp", xt[:, :hn], XT.ap()[j, :, t0:t0 + hn], R=[bXT], W=[bxt])
                        P.op("dve", lambda e, j=j, hs_=hs_, hn=hn, seg=seg: e.scalar_tensor_tensor(
                            out=xt[:, :hn], in0=acc[:, j, hs_:hs_ + hn], scalar=MOD(5, j, seg), in1=xt[:, :hn],
                            op0=ALU.mult, op1=ALU.add), R=[bacc, bmodv], W=[bxt])
                        P.dma("sp", XT.ap()[j, :, t0:t0 + hn], xt[:, :hn], R=[bxt], Wd=[bXT])
                P._wait("act", P._deps([], [bacc], []))
                P._wait("dve", P._deps([], [bacc], []))
                bacc.w = {}
        modst.close()

    with phase() as st:
        xl = [sb(st, "f_xl%d" % i, [128, 8, 128]) for i in range(2)]
        yo_ = [sb(st, "f_yo%d" % i, [128, D]) for i in range(2)]
        pf = [ps(st, "f_ps%d" % i, [128, D]) for i in range(2)]
        bxl, byo_, bpf = [Buf(), Buf()], [Buf(), Buf()], [Buf(), Buf()]
        for ti in range(S // 128):
            k = ti % 2
            P.dma("sp", xl[k][:], XTv[:, :, ti * 128:(ti + 1) * 128], R=[bXT], W=[bxl[k]])
            P._wait("pe", P._deps([bxl[k], bconst], [bpf[k]], []))
            ins = None
            for j in range(8):
                ins = nc.tensor.transpose(pf[k][:, j * 128:(j + 1) * 128], xl[k][:, j, :], idf[:])
            P.cnt["pe"] += 1
            ins.then_inc(P.sem["pe"], 1)
            P._commit(("e", "pe"), P.cnt["pe"], [bxl[k], bconst], [bpf[k]], [])
            P.op("act", lambda e, k=k: e.activation(out=yo_[k][:], in_=pf[k][:], func=AF.Copy), R=[bpf[k]], W=[byo_[k]])
            P.dma("sp", y_out.ap()[ti * 128:(ti + 1) * 128, :], yo_[k][:], R=[byo_[k]])
    P.finish()
    es.close()
    return nc


def _tables(S):
    T = S + CT
    n_rows = S // GRID_W
    row = np.repeat(np.arange(n_rows, dtype=np.float32), GRID_W)
    col = np.tile(np.arange(GRID_W, dtype=np.float32), n_rows)
    inv = (10000.0 ** (-np.arange(8, dtype=np.float32) / 8)).astype(np.float32)
    ang = np.concatenate([row[:, None] * inv, col[:, None] * inv], axis=-1).astype(np.float32)
    cos = np.ones((32, T), np.float32)
    sin = np.zeros((32, T), np.float32)
    cos[0:16, :S] = np.cos(ang).T
    cos[16:32, :S] = np.cos(ang).T
    sin[0:16, :S] = -np.sin(ang).T
    sin[16:32, :S] = np.sin(ang).T
    invc = np.zeros((128, 2, T), np.float32)
    for g, w in enumerate(POOL_WINDOWS):
        for (L, off) in [(S, 0), (CT, S)]:
            t = np.arange(L)
            lo = np.clip(t - w // 2, 0, L)
            hi = np.clip(t + w // 2, 0, L)
            invc[(g % 2) * 64:(g % 2) * 64 + 64, g // 2, off:off + L] = (1.0 / (hi - lo).astype(np.float32))[None, :]
    deltas = np.abs(np.linspace(HY_MIN_DECAY, HY_MAX_DECAY, 256, dtype=np.float32))

    def filt(L):
        R = 2 * L + 512
        z = np.zeros((17, R), np.float32)
        df = np.zeros((256, R), np.float32)
        db = np.zeros((256, R), np.float32)
        m = np.arange(R)
        lag = (L - 1) - m
        valid = np.abs(lag) <= L - 1
        idx = np.abs(lag)[valid]
        tt = np.linspace(0.0, 1.0, L, dtype=np.float32)[idx]
        wpos = ((2.0 * math.pi / L) * np.arange(L, dtype=np.float32))[idx]
        f = np.linspace(1e-4, 7, 8, dtype=np.float32)
        zz = np.concatenate([tt[None, :], np.cos(f[:, None] * wpos[None, :]), -np.sin(f[:, None] * wpos[None, :])], axis=0)
        z[:, valid] = zz.astype(np.float32)
        dec = np.exp(-tt[None, :] * deltas[:, None]).astype(np.float32)
        lv = lag[valid]
        dfv = np.where(lv[None, :] >= 0, dec, 0.0)
        dbv = np.where(lv[None, :] < 0, dec, 0.0)
        df[:, valid] = dfv
        db[:, valid] = dbv
        return z, df, db

    zl, dfl, dbl = filt(S)
    zc, dfc, dbc = filt(CT)
    idf = np.eye(128, dtype=np.float32)
    return {"c_idf": idf, "c_idb": idf.astype(ml_dtypes.bfloat16), "c_anti": idf[::-1].copy().astype(ml_dtypes.bfloat16),
            "c_cos": cos, "c_sin": sin, "c_invc": invc, "c_zl": zl, "c_dfl": dfl, "c_dbl": dbl,
            "c_zc": zc, "c_dfc": dfc, "c_dbc": dbc}


def _pack_vecs(inp, b, NL):
    v = np.zeros((NL, 128, NV), np.float32)

    def put(l, name, arr, j0=0):
        o, w = VCOLS[name]
        arr = np.asarray(arr, np.float32).reshape(-1, 128)
        v[l, :, o + j0:o + j0 + arr.shape[0]] = arr.T

    for l in range(NL):
        put(l, "n1g", inp["norm1_g"][l])
        put(l, "n2g", inp["norm2_g"][l])
        put(l, "bmod", inp["b_mod"][l])
        put(l, "c", inp["c"][b])
        put(l, "cctx", inp["c_ctx"])
        put(l, "psc", inp["pool_scale"][l])
        for tap in range(3):
            put(l, "hcw", inp["hy_conv_w"][l, tap], tap * 6)
        put(l, "hcb", inp["hy_conv_b"][l])
        put(l, "hyb", inp["hy_bias"][l])
        put(l, "qng", inp["mla_q_norm_g"][l])
        put(l, "kvng", inp["mla_kv_norm_g"][l])
        for nm, g in (("q", inp["qk_norm_q"][l]), ("k", inp["qk_norm_k"][l])):
            main = np.zeros(128, np.float32)
            main[0:32] = g[64:96]
            main[64:128] = g[0:64]
            sw = np.zeros(128, np.float32)
            sw[0:16] = g[80:96]
            sw[16:32] = g[64:80]
            put(l, "g%sm" % nm, main)
            put(l, "g%ss" % nm, sw)
        for nm, src in (("fb1", "hy_f_b1"), ("fb2", "hy_f_b2"), ("freq", "hy_freq")):
            a = np.zeros(128, np.float32)
            a[0:64] = inp[src][l]
            put(l, nm, a)
        b1 = np.asarray(inp["moe_b1"][l], np.float32).reshape(NE, 16, 128)
        o, w = VCOLS["b1"]
        v[l, :, o:o + w] = b1.transpose(2, 0, 1).reshape(128, NE * 16)
    return v


_NC_CACHE = {}


def run(inputs, S, NL, batches, dbg=False):
    inp = {k: np.asarray(v) for k, v in inputs.items()}
    key = (S, NL, dbg)
    if key not in _NC_CACHE:
        _NC_CACHE[key] = build(S, NL, dbg)
    nc = _NC_CACHE[key]
    tabs = _tables(S)
    shared = dict(tabs)
    for nm in ["w_mod", "w_in", "pool_w", "hy_f_w1", "hy_f_w2", "hy_f_w3", "mla_w_uq", "mla_w_ukv", "w_out", "router_w",
               "moe_w1", "moe_w2", "moe_b2"]:
        shared[nm] = np.ascontiguousarray(inp[nm][:NL], dtype=np.float32)
    shared["router_b"] = np.ascontiguousarray(inp["router_b"][:NL, None, :], dtype=np.float32)
    in_maps = []
    for b in batches:
        m = dict(shared)
        m["x"] = np.ascontiguousarray(inp["x"][b, :S], dtype=np.float32)
        m["ctx"] = np.ascontiguousarray(inp["ctx"][b], dtype=np.float32)
        m["vecs"] = _pack_vecs(inp, b, NL)
        in_maps.append(m)
    res = run_bass_kernel_spmd(nc, in_maps, core_ids=list(range(len(batches))))
    return res


def kernel(**inputs):
    B, S, _ = inputs["x"].shape
    res = run(inputs, S, 2, list(range(B)))
    return np.stack([np.asarray(r["y"], dtype=np.float32) for r in res.results], axis=0)
```

```python
import math
from contextlib import ExitStack
import numpy as np
import ml_dtypes
import concourse.bass as bass
import concourse.mybir as mybir
from concourse.bass_utils import run_bass_kernel_spmd

F32 = mybir.dt.float32
BF16 = mybir.dt.bfloat16
ALU = mybir.AluOpType
AF = mybir.ActivationFunctionType
AX = mybir.AxisListType

D = 1024
CT = 256
NE = 32
NPT = 1472
EPS = 1e-6
GRID_W = 64
POOL_WINDOWS = (2, 4, 8, 16)
HY_MIN_DECAY = math.log(1e-2) / 1.5
HY_MAX_DECAY = math.log(1e-2) / 0.3
ATTN_SCALE = 96 ** -0.5
SIG_CLAMP = float(1.0 / (1.0 + math.exp(-1.702 * 7.0)))

VCOLS = {}
_o = 0
for _n, _w in [("n1g", 8), ("n2g", 8), ("bmod", 48), ("c", 8), ("cctx", 8), ("psc", 2), ("hcw", 18), ("hcb", 6),
               ("hyb", 2), ("qng", 2), ("kvng", 1), ("gqm", 1), ("gqs", 1), ("gkm", 1), ("gks", 1),
               ("fb1", 1), ("fb2", 1), ("freq", 1), ("b1", 512)]:
    VCOLS[_n] = (_o, _w)
    _o += _w
NV = _o


class Buf:
    __slots__ = ("w", "r")

    def __init__(self):
        self.w = {}
        self.r = {}


class Prog:
    NS = 32

    def __init__(self, nc, es):
        self.nc = nc
        self.E = {"pe": nc.tensor, "act": nc.scalar, "dve": nc.vector, "pool": nc.gpsimd, "sp": nc.sync}
        self.sem = {k: es.enter_context(nc.semaphore("s_" + k)) for k in self.E}
        self.cnt = {k: 0 for k in self.E}
        self.dsem = [es.enter_context(nc.semaphore("d%d" % i)) for i in range(self.NS)]
        self.dtot = [0] * self.NS
        self.dn = 0
        self.NX = 8
        self.xsem = [es.enter_context(nc.semaphore("x%d" % i)) for i in range(self.NX)]
        self.xtot = [0] * self.NX
        self.xn = 0
        self.seen = {k: {} for k in self.E}

    def _wait(self, e, deps):
        for key, n in deps.items():
            if n <= 0 or (key == ("e", "pe") and e == "pe"):
                continue
            if self.seen[e].get(key, 0) >= n:
                continue
            sem = self.sem[key[1]] if key[0] == "e" else (self.dsem[key[1]] if key[0] == "d" else self.xsem[key[1]])
            self.E[e].wait_ge(sem, n)
            self.seen[e][key] = n

    @staticmethod
    def _deps(R, W, Wd):
        d = {}
        for b in R:
            for k, n in b.w.items():
                d[k] = max(d.get(k, 0), n)
        for b in W:
            for k, n in b.w.items():
                d[k] = max(d.get(k, 0), n)
            for k, n in b.r.items():
                d[k] = max(d.get(k, 0), n)
        for b in Wd:
            for k, n in b.r.items():
                d[k] = max(d.get(k, 0), n)
        return d

    @staticmethod
    def _commit(key, n, R, W, Wd):
        for b in R:
            b.r[key] = max(b.r.get(key, 0), n)
        for b in W:
            b.w = {key: n}
            b.r = {}
        for b in Wd:
            b.w[key] = max(b.w.get(key, 0), n)

    def op(self, e, fn, R=(), W=(), Wd=()):
        self._wait(e, self._deps(R, W, Wd))
        ins = fn(self.E[e])
        self.cnt[e] += 1
        ins.then_inc(self.sem[e], 1)
        self._commit(("e", e), self.cnt[e], R, W, Wd)

    def mm(self, items, R=(), W=(), Wd=()):
        self._wait("pe", self._deps(R, W, Wd))
        ins = None
        for (o, l, r, st, sp) in items:
            ins = self.nc.tensor.matmul(o, l, r, start=st, stop=sp)
        self.cnt["pe"] += 1
        ins.then_inc(self.sem["pe"], 1)
        self._commit(("e", "pe"), self.cnt["pe"], R, W, Wd)

    def dma(self, e, out, in_, R=(), W=(), Wd=(), **kw):
        i = self.dn
        self.dn = (i + 1) % self.NS
        d = self._deps(R, W, Wd)
        if self.dtot[i]:
            d[("d", i)] = max(d.get(("d", i), 0), self.dtot[i])
        self._wait(e, d)
        ins = self.E[e].dma_start(out=out, in_=in_, **kw)
        self.dtot[i] += 16
        ins.then_inc(self.dsem[i], 16)
        self._commit(("d", i), self.dtot[i], R, W, Wd)

    def barrier(self):
        for e in self.E:
            d = {}
            for k in self.E:
                if k != e and self.cnt[k]:
                    d[("e", k)] = self.cnt[k]
            for i in range(self.NS):
                if self.dtot[i]:
                    d[("d", i)] = self.dtot[i]
            self._wait(e, d)

    def bgdma(self, e, out, in_, R=(), W=(), Wd=(), **kw):
        i = self.xn
        self.xn = (i + 1) % self.NX
        d = self._deps(R, W, Wd)
        if self.xtot[i]:
            d[("x", i)] = max(d.get(("x", i), 0), self.xtot[i])
        self._wait(e, d)
        ins = self.E[e].dma_start(out=out, in_=in_, **kw)
        self.xtot[i] += 16
        ins.then_inc(self.xsem[i], 16)
        self._commit(("x", i), self.xtot[i], R, W, Wd)

    def finish(self):
        for i in range(self.NX):
            if self.xtot[i]:
                self.nc.sync.wait_ge(self.xsem[i], self.xtot[i])
        for i in range(self.NS):
            if self.dtot[i]:
                self.nc.sync.wait_ge(self.dsem[i], self.dtot[i])
        for k in self.E:
            if k != "sp" and self.cnt[k]:
                self.nc.sync.wait_ge(self.sem[k], self.cnt[k])


def build(S, NL, dbg=False):
    T = S + CT
    NLC = S // 512
    chunks = [(i * 512, 512, 0) for i in range(NLC)] + [(S, CT, 1)]
    PTW = S + 24 + CT + 8
    RL = 2 * S + 512
    RC = 2 * CT + 512
    nc = bass.Bass("TRN2", target_bir_lowering=False)
    es = ExitStack()
    P = Prog(nc, es)

    def din(name, shape, dt=F32):
        return nc.dram_tensor(name, list(shape), dt, kind="ExternalInput")

    def dscr(name, shape, dt):
        return nc.dram_tensor(name, list(shape), dt, kind="ExternalOutput" if dbg else "Internal")

    x_in = din("x", [S, D])
    ctx_in = din("ctx", [CT, D])
    vecs = din("vecs", [NL, 128, NV])
    w_mod = din("w_mod", [NL, D, 6 * D])
    w_in = din("w_in", [NL, D, 1440])
    pool_w = din("pool_w", [NL, 4, 64, 64])
    f_w1 = din("hy_f_w1", [NL, 17, 64])
    f_w2 = din("hy_f_w2", [NL, 64, 64])
    f_w3 = din("hy_f_w3", [NL, 64, 512])
    w_uq = din("mla_w_uq", [NL, 256, 768])
    w_ukv = din("mla_w_ukv", [NL, 128, 1024])
    w_out = din("w_out", [NL, D, D])
    r_w = din("router_w", [NL, D, NE])
    r_b = din("router_b", [NL, 1, NE])
    m_w1 = din("moe_w1", [NL, NE, D, 2 * D])
    m_w2 = din("moe_w2", [NL, NE, D, D])
    m_b2 = din("moe_b2", [NL, NE, D])
    c_idf = din("c_idf", [128, 128])
    c_idb = din("c_idb", [128, 128], BF16)
    c_anti = din("c_anti", [128, 128], BF16)
    c_cos = din("c_cos", [32, T])
    c_sin = din("c_sin", [32, T])
    c_invc = din("c_invc", [128, 2, T])
    c_zl = din("c_zl", [17, RL])
    c_dfl = din("c_dfl", [256, RL])
    c_dbl = din("c_dbl", [256, RL])
    c_zc = din("c_zc", [17, RC])
    c_dfc = din("c_dfc", [256, RC])
    c_dbc = din("c_dbc", [256, RC])
    y_out = nc.dram_tensor("y", [S, D], F32, kind="ExternalOutput")

    XT = dscr("XT", [8, 128, T], F32)
    PT = dscr("PT", [NPT, PTW], BF16)
    X0C = dscr("X0C", [256, T], BF16)
    KDL = dscr("KDL", [256, RL], BF16)
    KDC = dscr("KDC", [256, RC], BF16)
    KT = dscr("KT", [8, 128, T], BF16)
    QT = dscr("QT", [8, 128, T], BF16)
    VT = dscr("VT", [8, T, 64], BF16)
    MIXT = dscr("MIXT", [D, T], BF16)
    H2T = dscr("H2T", [D, T], BF16)
    GT = dscr("GT", [NE, T], F32)
    W1B = dscr("W1B", [NE, D, 2 * D], BF16)
    W2B = dscr("W2B", [NE, D, D], BF16)
    bW1B, bW2B = Buf(), Buf()
    bXT, bPT, bX0C, bKDL, bKDC, bKT, bQT, bVT, bMIXT, bH2T, bGT = [Buf() for _ in range(11)]

    XTv = XT.ap().rearrange("j p t -> p j t")

    uid = [0]

    class phase:
        def __init__(self_, name="ph"):
            self_.name = name

        def __enter__(self_):
            self_.st = ExitStack()
            self_.st.enter_context(nc.named_scope(self_.name))
            return self_.st

        def __exit__(self_, *a):
            if a[0] is None:
                P.barrier()
            self_.st.close()
            return False

    def sb(st, name, shape, dt=F32):
        uid[0] += 1
        return st.enter_context(nc.sbuf_tensor("%s_%d" % (name, uid[0]), list(shape), dt))

    def ps(st, name, shape, dt=F32):
        uid[0] += 1
        return st.enter_context(nc.psum_tensor("%s_%d" % (name, uid[0]), list(shape), dt))

    def pcol(seg):
        return 8 if seg == 0 else S + 24

    idf = sb(es, "idf", [128, 128])
    idb = sb(es, "idb", [128, 128], BF16)
    anti = sb(es, "anti", [128, 128], BF16)
    onesb = sb(es, "onesb", [128, 128], BF16)
    onesf = sb(es, "onesf", [128, 128])
    vec = sb(es, "vec", [128, NL, NV])
    bconst = Buf()
    P.dma("sp", idf[:], c_idf.ap(), W=[bconst])
    P.dma("sp", idb[:], c_idb.ap(), Wd=[bconst])
    P.dma("sp", anti[:], c_anti.ap(), Wd=[bconst])
    P.dma("sp", vec[:], vecs.ap().rearrange("l p v -> p l v"), Wd=[bconst])
    P.op("dve", lambda e: e.memset(onesb[:], 1.0), Wd=[bconst])
    P.op("dve", lambda e: e.memset(onesf[:], 1.0), Wd=[bconst])

    def V(l, name, j=0, n=1):
        o, w = VCOLS[name]
        return vec[:, l, o + j:o + j + n]

    with phase("setup") as st:
        zt = sb(st, "zt", [128, 16], BF16)
        bz = Buf()
        P.op("dve", lambda e: e.memset(zt[:], 0.0), W=[bz])
        for r0 in range(0, NPT, 128):
            nr = min(128, NPT - r0)
            for (c0, w) in [(0, 8), (8 + S, 16), (S + 24 + CT, 8)]:
                P.dma("sp", PT.ap()[r0:r0 + nr, c0:c0 + w], zt[0:nr, 0:w], R=[bz], Wd=[bPT])
        xin = [sb(st, "xin%d" % i, [128, D]) for i in range(2)]
        xtt = [sb(st, "xtt%d" % i, [128, 8, 128]) for i in range(2)]
        pst = [ps(st, "pst%d" % i, [128, 8, 128]) for i in range(2)]
        bxin = [Buf(), Buf()]
        bxtt = [Buf(), Buf()]
        bpst = [Buf(), Buf()]
        for ti in range(T // 128):
            k = ti % 2
            src = x_in.ap()[ti * 128:(ti + 1) * 128, :] if ti * 128 < S else ctx_in.ap()[ti * 128 - S:(ti + 1) * 128 - S, :]
            P.dma("sp", xin[k][:], src, W=[bxin[k]])
            P._wait("pe", P._deps([bxin[k], bconst], [bpst[k]], []))
            ins = None
            for j in range(8):
                ins = nc.tensor.transpose(pst[k][:, j, :], xin[k][:, j * 128:(j + 1) * 128], idf[:])
            P.cnt["pe"] += 1
            ins.then_inc(P.sem["pe"], 1)
            P._commit(("e", "pe"), P.cnt["pe"], [bxin[k], bconst], [bpst[k]], [])
            P.op("act" if ti % 2 else "dve",
                 (lambda e, k=k: e.activation(out=xtt[k][:], in_=pst[k][:], func=AF.Copy)) if ti % 2 else
                 (lambda e, k=k: e.tensor_copy(out=xtt[k][:], in_=pst[k][:])), R=[bpst[k]], W=[bxtt[k]])
            P.dma("sp", XTv[:, :, ti * 128:(ti + 1) * 128], xtt[k][:], R=[bxtt[k]], Wd=[bXT])

    for l in range(NL):
        last = (l == NL - 1)
        for e_ in range(NE):
            for r0 in range(0, D, 256):
                P.bgdma("pool", W1B.ap()[e_, r0:r0 + 256, :], m_w1.ap()[l, e_, r0:r0 + 256, :], Wd=[bW1B])
            for r0 in range(0, D, 512):
                P.bgdma("pool", W2B.ap()[e_, r0:r0 + 512, :], m_w2.ap()[l, e_, r0:r0 + 512, :], Wd=[bW2B])
        modst = ExitStack()
        sT = sb(modst, "sT", [128, 8, 2])
        modT = sb(modst, "modT", [128, 48, 2])
        A1 = sb(modst, "A1", [128, 8, 2])
        A2 = sb(modst, "A2", [128, 8, 2])
        bmodv = Buf()
        with phase("mod") as st:
            wm = [sb(st, "wm%d" % i, [128, 8, 768]) for i in range(2)]
            bwm = [Buf(), Buf()]
            psm = ps(st, "psm", [128, 48, 2])
            bpsm = Buf()
            bsT = Buf()
            P.op("act", lambda e: e.activation(out=sT[:, :, 0], in_=V(l, "c", 0, 8), func=AF.Silu), R=[bconst], W=[bsT])
            P.op("act", lambda e: e.activation(out=sT[:, :, 1], in_=V(l, "cctx", 0, 8), func=AF.Silu), R=[bconst], W=[bsT])
            for s in range(8):
                k = s % 2
                P.dma("sp", wm[k][:], w_mod.ap()[l, :, s * 768:(s + 1) * 768].rearrange("(kc p) n -> p kc n", p=128),
                      W=[bwm[k]])
                items = []
                for oc in range(6):
                    for kc in range(8):
                        items.append((psm[:, s * 6 + oc, :], wm[k][:, kc, oc * 128:(oc + 1) * 128], sT[:, kc, :],
                                      kc == 0, kc == 7))
                P.mm(items, R=[bwm[k], bsT], Wd=[bpsm])
            for i in range(2):
                P.op("dve", lambda e, i=i: e.tensor_tensor(out=modT[:, :, i], in0=psm[:, :, i], in1=V(l, "bmod", 0, 48),
                                                          op=ALU.add), R=[bpsm, bconst], W=[bmodv])
                P.op("dve", lambda e, i=i: e.scalar_tensor_tensor(out=A1[:, :, i], in0=modT[:, 8:16, i], scalar=1.0,
                                                                 in1=V(l, "n1g", 0, 8), op0=ALU.add, op1=ALU.mult),
                     R=[bmodv], W=[bmodv])
                P.op("dve", lambda e, i=i: e.scalar_tensor_tensor(out=A2[:, :, i], in0=modT[:, 32:40, i], scalar=1.0,
                                                                 in1=V(l, "n2g", 0, 8), op0=ALU.add, op1=ALU.mult),
                     R=[bmodv], W=[bmodv])

        def MOD(m, j, i):
            return modT[:, m * 8 + j, i:i + 1]

        def norm_chunk(st_tiles, t0, n, seg, Asel, shm, hT, bhT, hF=None):
            xt, sq, rstd, tmp, pss, bxt, bsq, bpss, brs, btmp = st_tiles
            P.dma("sp", xt[:, :, :n], XTv[:, :, t0:t0 + n], R=[bXT], W=[bxt])
            P.op("act", lambda e: e.activation(out=sq[:, :, :n], in_=xt[:, :, :n], func=AF.Square), R=[bxt], W=[bsq])
            P.mm([(pss[:, :n], onesb[:], sq[:, j, :n], j == 0, j == 7) for j in range(8)], R=[bsq, bconst], W=[bpss])
            P.op("act", lambda e: e.activation(out=rstd[:, :n], in_=pss[:, :n], func=AF.Sqrt, scale=1.0 / D, bias=EPS),
                 R=[bpss], W=[brs])
            P.op("dve", lambda e: e.reciprocal(out=rstd[:, :n], in_=rstd[:, :n]), W=[brs])
            for j in range(8):
                P.op("dve", lambda e, j=j: e.scalar_tensor_tensor(out=tmp[:, :n], in0=xt[:, j, :n],
                                                                 scalar=Asel[:, j, seg:seg + 1], in1=rstd[:, :n],
                                                                 op0=ALU.mult, op1=ALU.mult),
                     R=[bxt, brs, bmodv], W=[btmp])
                if hF is not None:
                    P.op("act", lambda e, j=j: e.activation(out=hF[:, j, :n], in_=tmp[:, :n], func=AF.Identity,
                                                           bias=MOD(shm, j, seg)), R=[btmp, bmodv], Wd=[bhT])
                    P.op("pool", lambda e, j=j: e.tensor_copy(out=hT[:, j, :n], in_=hF[:, j, :n]), R=[bhT], Wd=[bhT])
                else:
                    P.op("act", lambda e, j=j: e.activation(out=hT[:, j, :n], in_=tmp[:, :n], func=AF.Identity,
                                                           bias=MOD(shm, j, seg)), R=[btmp, bmodv], Wd=[bhT])

        def norm_tiles(st):
            return (sb(st, "n_xt", [128, 8, 512]), sb(st, "n_sq", [128, 8, 512], BF16), sb(st, "n_rstd", [128, 512]),
                    sb(st, "n_tmp", [128, 512]), ps(st, "n_pss", [128, 512]), Buf(), Buf(), Buf(), Buf(), Buf())

        with phase("inproj") as st:
            wi = sb(st, "wi", [128, 8, NPT], BF16)
            bwi = Buf()
            wsrc = w_in.ap()[l].rearrange("(kc p) n -> p kc n", p=128)
            P.dma("pool", wi[:, :, 0:1440], wsrc, Wd=[bwi])
            P.dma("pool", wi[:, :, 1440:1456], wsrc[:, :, 1424:1440], Wd=[bwi])
            P.dma("pool", wi[:, :, 1456:1472], wsrc[:, :, 1408:1424], Wd=[bwi])
            nt = norm_tiles(st)
            hT = sb(st, "hT", [128, 8, 512], BF16)
            bhT = Buf()
            ptsb = sb(st, "ptsb", [128, 12, 512], BF16)
            bptsb = Buf()
            pp = [ps(st, "pp%d" % i, [128, 512]) for i in range(2)]
            bpp = [Buf(), Buf()]
            for (t0, n, seg) in chunks:
                norm_chunk(nt, t0, n, seg, A1, 0, hT, bhT)
                for oc in range(12):
                    r0 = oc * 128
                    nr = min(128, NPT - r0)
                    k = oc % 2
                    P.mm([(pp[k][:nr, :n], wi[:, kc, r0:r0 + nr], hT[:, kc, :n], kc == 0, kc == 7) for kc in range(8)],
                         R=[bwi, bhT], W=[bpp[k]])
                    if k:
                        P.op("act", lambda e, k=k, nr=nr, oc=oc: e.activation(out=ptsb[:nr, oc, :n], in_=pp[k][:nr, :n],
                                                                             func=AF.Copy), R=[bpp[k]], Wd=[bptsb])
                    else:
                        P.op("dve", lambda e, k=k, nr=nr, oc=oc: e.tensor_copy(out=ptsb[:nr, oc, :n], in_=pp[k][:nr, :n]),
                             R=[bpp[k]], Wd=[bptsb])
                c0 = pcol(seg) + (t0 - (0 if seg == 0 else S))
                P.dma("sp", PT.ap()[0:1408, c0:c0 + n].rearrange("(oc p) t -> p oc t", p=128), ptsb[:, 0:11, :n],
                      R=[bptsb], Wd=[bPT])
                P.dma("sp", PT.ap()[1408:1472, c0:c0 + n], ptsb[0:64, 11, :n], R=[bptsb], Wd=[bPT])
                P._wait("act", P._deps([], [bptsb], []))
                P._wait("dve", P._deps([], [bptsb], []))
                bptsb.w = {}

        with phase("pool") as st:
            pw = sb(st, "pw", [128, 2, 128], BF16)
            bpw = Buf()
            P.op("dve", lambda e: e.memset(pw[:], 0.0), W=[bpw])
            for g in range(4):
                p0 = (g % 2) * 64
                P.dma("pool", pw[p0:p0 + 64, g // 2, p0:p0 + 64], pool_w.ap()[l, g], Wd=[bpw])
            u = sb(st, "pl_u", [128, 2, 528], BF16)
            a = sb(st, "pl_a", [128, 528])
            b = sb(st, "pl_b", [128, 528])
            iv = sb(st, "pl_iv", [128, 2, 512])
            dd = sb(st, "pl_d", [128, 512], BF16)
            po = sb(st, "pl_o", [128, 2, 512], BF16)
            pps = ps(st, "pl_ps", [128, 512])
            bu, ba, bb, biv, bdd, bpo, bpps = [Buf() for _ in range(7)]
            for (t0, n, seg) in chunks:
                c0 = pcol(seg) + (t0 - (0 if seg == 0 else S))
                P.dma("sp", u[:, :, :n + 16], PT.ap()[0:256, c0 - 8:c0 + n + 8].rearrange("(cj p) t -> p cj t", p=128),
                      R=[bPT], W=[bu])
                P.dma("sp", iv[:, :, :n], c_invc.ap()[:, :, t0:t0 + n], W=[biv])
                for cj in range(2):
                    for hf in range(2):
                        g = cj * 2 + hf
                        w = POOL_WINDOWS[g]
                        pr = slice(hf * 64, hf * 64 + 64)
                        P.op("dve", lambda e, pr=pr, cj=cj: e.tensor_tensor(out=a[pr, 1:n + 16], in0=u[pr, cj, 0:n + 15],
                                                                          in1=u[pr, cj, 1:n + 16], op=ALU.add),
                             R=[bu], W=[ba])
                        cur, oth, bc_, bo_ = a, b, ba, bb
                        lo, hi = 1, n + 16
                        sh = 1
                        while sh * 2 < w:
                            nlo, nhi = lo + sh, hi - sh
                            P.op("dve", lambda e, pr=pr, cur=cur, oth=oth, nlo=nlo, nhi=nhi, sh=sh: e.tensor_tensor(
                                out=oth[pr, nlo:nhi], in0=cur[pr, nlo - sh:nhi - sh], in1=cur[pr, nlo + sh:nhi + sh],
                                op=ALU.add), R=[bc_], W=[bo_])
                            cur, oth, bc_, bo_ = oth, cur, bo_, bc_
                            lo, hi = nlo, nhi
                            sh *= 2
                        P.op("dve", lambda e, pr=pr, cur=cur, cj=cj: e.tensor_tensor(out=cur[pr, 8:8 + n], in0=cur[pr, 8:8 + n],
                                                                                   in1=iv[pr, cj, :n], op=ALU.mult),
                             R=[biv], W=[bc_])
                        P.op("dve", lambda e, pr=pr, cur=cur, cj=cj: e.tensor_tensor(out=dd[pr, :n], in0=cur[pr, 8:8 + n],
                                                                                   in1=u[pr, cj, 8:8 + n], op=ALU.subtract),
                             R=[bc_, bu], Wd=[bdd])
                    P.mm([(pps[:, :n], pw[:, cj, :], dd[:, :n], True, True)], R=[bpw, bdd], W=[bpps])
                    bdd.w = dict(bdd.w)
                    P.op("act", lambda e, cj=cj: e.activation(out=po[:, cj, :n], in_=pps[:, :n], func=AF.Copy,
                                                             scale=V(l, "psc", cj, 1)), R=[bpps, bconst], Wd=[bpo])
                    P._wait("dve", P._deps([], [bdd], []))
                    bdd.w = {}
                P.dma("sp", MIXT.ap()[0:256, t0:t0 + n].rearrange("(cj p) t -> p cj t", p=128), po[:, :, :n],
                      R=[bpo], Wd=[bMIXT])
                P._wait("act", P._deps([], [bpo], []))
                bpo.w = {}

        with phase("hyfilt") as st:
            fw1 = sb(st, "fw1", [17, 64])
            fw2 = sb(st, "fw2", [64, 64])
            fw3 = sb(st, "fw3", [64, 512])
            fsc = sb(st, "fsc", [64, 4])
            bfw = Buf()
            P.dma("sp", fw1[:], f_w1.ap()[l], Wd=[bfw])
            P.dma("sp", fw2[:], f_w2.ap()[l], Wd=[bfw])
            P.dma("sp", fw3[:], f_w3.ap()[l], Wd=[bfw])
            P.op("dve", lambda e: e.tensor_scalar(out=fsc[:, 0:1], in0=vec[0:64, l, VCOLS["freq"][0]:VCOLS["freq"][0] + 1],
                                                  scalar1=1.0 / 3.0, scalar2=None, op0=ALU.mult), R=[bconst], W=[bfw])
            P.op("dve", lambda e: e.tensor_tensor(out=fsc[:, 1:2], in0=fsc[:, 0:1],
                                                  in1=vec[0:64, l, VCOLS["fb1"][0]:VCOLS["fb1"][0] + 1], op=ALU.mult),
                 R=[bconst], W=[bfw])
            P.op("dve", lambda e: e.tensor_tensor(out=fsc[:, 2:3], in0=fsc[:, 0:1],
                                                  in1=vec[0:64, l, VCOLS["fb2"][0]:VCOLS["fb2"][0] + 1], op=ALU.mult),
                 R=[bconst], W=[bfw])
            zt_ = sb(st, "f_z", [17, 512])
            h1 = sb(st, "f_h1", [64, 512])
            h2 = sb(st, "f_h2", [64, 512])
            s2 = sb(st, "f_s2", [64, 512])
            dfc = sb(st, "f_df", [128, 2, 512])
            dbc = sb(st, "f_db", [128, 2, 512])
            kk = sb(st, "f_k", [128, 2, 512])
            kb = sb(st, "f_kb", [128, 2, 512], BF16)
            p1 = ps(st, "f_p1", [64, 512])
            p3 = [ps(st, "f_p3%d" % i, [128, 512]) for i in range(4)]
            bz_, bh1, bh2, bs2, bdf, bdb, bkk, bkb, bp1 = [Buf() for _ in range(9)]
            bp3 = [Buf() for _ in range(4)]

            def sin3(src_ps, dst, bdst, col):
                P.op("act", lambda e: e.activation(out=dst[:], in_=src_ps[:], func=AF.Sin, scale=fsc[:, 0:1],
                                                   bias=fsc[:, col:col + 1]), R=[bp1, bfw], W=[bdst])
                P.op("dve", lambda e: e.tensor_tensor(out=s2[:], in0=dst[:], in1=dst[:], op=ALU.mult), R=[bdst], W=[bs2])
                P.op("dve", lambda e: e.tensor_scalar(out=s2[:], in0=s2[:], scalar1=-4.0, scalar2=3.0, op0=ALU.mult,
                                                      op1=ALU.add), W=[bs2])
                P.op("dve", lambda e: e.tensor_tensor(out=dst[:], in0=dst[:], in1=s2[:], op=ALU.mult), R=[bs2], W=[bdst])

            for (ztab, dftab, dbtab, KD, bKD, R_, Lseg) in [(c_zl, c_dfl, c_dbl, KDL, bKDL, RL, S),
                                                           (c_zc, c_dfc, c_dbc, KDC, bKDC, RC, CT)]:
                for r0 in range(0, R_, 512):
                    P.dma("sp", zt_[:], ztab.ap()[:, r0:r0 + 512], W=[bz_])
                    P.dma("sp", dfc[:], dftab.ap()[:, r0:r0 + 512].rearrange("(cj p) r -> p cj r", p=128), W=[bdf])
                    P.dma("sp", dbc[:], dbtab.ap()[:, r0:r0 + 512].rearrange("(cj p) r -> p cj r", p=128), W=[bdb])
                    P.mm([(p1[:], fw1[:], zt_[:], True, True)], R=[bfw, bz_], W=[bp1])
                    sin3(p1, h1, bh1, 1)
                    P.mm([(p1[:], fw2[:], h1[:], True, True)], R=[bfw, bh1], W=[bp1])
                    sin3(p1, h2, bh2, 2)
                    for q in range(4):
                        P.mm([(p3[q][:], fw3[:, q * 128:(q + 1) * 128], h2[:], True, True)], R=[bfw, bh2], W=[bp3[q]])
                    for cj in range(2):
                        P.op("dve", lambda e, cj=cj: e.tensor_tensor(out=kk[:, cj, :], in0=p3[cj][:], in1=dfc[:, cj, :],
                                                                    op=ALU.mult), R=[bp3[cj], bdf], Wd=[bkk])
                        P.op("dve", lambda e, cj=cj: e.tensor_tensor(out=dbc[:, cj, :], in0=p3[2 + cj][:], in1=dbc[:, cj, :],
                                                                    op=ALU.mult), R=[bp3[2 + cj]], Wd=[bdb])
                        P.op("pool", lambda e, cj=cj: e.tensor_tensor(out=kk[:, cj, :], in0=kk[:, cj, :], in1=dbc[:, cj, :],
                                                                     op=ALU.add), R=[bdb], W=[bkk])
                        m0 = Lseg - 1
                        if r0 <= m0 < r0 + 512:
                            P.op("pool", lambda e, cj=cj, m0=m0, r0=r0: e.tensor_tensor(
                                out=kk[:, cj, m0 - r0:m0 - r0 + 1], in0=kk[:, cj, m0 - r0:m0 - r0 + 1],
                                in1=V(l, "hyb", cj, 1), op=ALU.add), R=[bconst], W=[bkk])
                        P.op("act", lambda e, cj=cj: e.activation(out=kb[:, cj, :], in_=kk[:, cj, :], func=AF.Copy),
                             R=[bkk], Wd=[bkb])
                    P.dma("sp", KD.ap()[:, r0:r0 + 512].rearrange("(cj p) r -> p cj r", p=128), kb[:], R=[bkb], Wd=[bKD])
                    for en in ("act",):
                        P._wait(en, P._deps([], [bkb], []))
                    bkb.w = {}
                    P._wait("dve", P._deps([], [bkk, bdb], []))
                    bkk.w = {}
                    bdb.w = dict(bdb.w)

        for (seg, Lseg, KD, bKD, tbase) in [(0, S, KDL, bKDL, 0), (1, CT, KDC, bKDC, S)]:
            if seg == 1 and last:
                continue
            NB = Lseg // 128
            with phase("hyconv") as st:
                U = sb(st, "hy_U", [128, 256, NB], BF16)
                bU = Buf()
                with phase("hyfront") as st2:
                    hin = sb(st2, "hy_in", [128, 6, 514], BF16)
                    hc = sb(st2, "hy_c", [128, 6, 512])
                    x0b = sb(st2, "hy_x0b", [128, 2, 512], BF16)
                    ub = sb(st2, "hy_ub", [128, 2, 512], BF16)
                    ptp = ps(st2, "hy_ptp", [128, 4, 128], BF16)
                    bhin, bhc, bx0b, bub, bptp = [Buf() for _ in range(5)]
                    for t0 in range(0, Lseg, 512):
                        n = min(512, Lseg - t0)
                        c0 = pcol(seg) + t0
                        P.dma("sp", hin[:, :, :n + 2],
                              PT.ap()[256:1024, c0 - 1:c0 + n + 1].rearrange("(cj p) t -> p cj t", p=128), R=[bPT], W=[bhin])
                        for cj in range(6):
                            eng = "dve" if cj % 2 == 0 else "pool"
                            o = VCOLS["hcw"][0]
                            P.op("dve", lambda e, cj=cj: e.tensor_scalar(out=hc[:, cj, :n], in0=hin[:, cj, 0:n],
                                                                        scalar1=V(l, "hcw", 0 * 6 + cj, 1),
                                                                        scalar2=V(l, "hcb", cj, 1), op0=ALU.mult,
                                                                        op1=ALU.add), R=[bhin, bconst], Wd=[bhc])
                            for tap in (1, 2):
                                P.op("dve", lambda e, cj=cj, tap=tap: e.scalar_tensor_tensor(
                                    out=hc[:, cj, :n], in0=hin[:, cj, tap:tap + n], scalar=V(l, "hcw", tap * 6 + cj, 1),
                                    in1=hc[:, cj, :n], op0=ALU.mult, op1=ALU.add), R=[bhin], Wd=[bhc])
                        for cj in range(2):
                            P.op("act", lambda e, cj=cj: e.activation(out=x0b[:, cj, :n], in_=hc[:, cj, :n], func=AF.Copy),
                                 R=[bhc], Wd=[bx0b])
                            P.op("pool", lambda e, cj=cj: e.tensor_tensor(out=ub[:, cj, :n], in0=hc[:, 2 + cj, :n],
                                                                         in1=hc[:, 4 + cj, :n], op=ALU.mult),
                                 R=[bhc], Wd=[bub])
                        P.dma("sp", X0C.ap()[:, tbase + t0:tbase + t0 + n].rearrange("(cj p) t -> p cj t", p=128),
                              x0b[:, :, :n], R=[bx0b], Wd=[bX0C])
                        for cj in range(2):
                            nb_ = n // 128
                            P._wait("pe", P._deps([bub, bconst], [bptp], []))
                            ins = None
                            for q in range(nb_):
                                ins = nc.tensor.transpose(ptp[:, q, :], ub[:, cj, q * 128:(q + 1) * 128], idb[:])
                            P.cnt["pe"] += 1
                            ins.then_inc(P.sem["pe"], 1)
                            P._commit(("e", "pe"), P.cnt["pe"], [bub, bconst], [bptp], [])
                            P.op("act", lambda e, cj=cj, nb_=nb_, t0=t0: e.activation(
                                out=U[:, cj * 128:(cj + 1) * 128, t0 // 128:t0 // 128 + nb_].rearrange("p c q -> p q c"),
                                in_=ptp[:, 0:nb_, :], func=AF.Copy), R=[bptp], Wd=[bU])
                        P._wait("dve", P._deps([], [bhc], []))
                        P._wait("act", P._deps([], [bx0b], []))
                        P._wait("pool", P._deps([], [bub], []))
                        bhc.w = {}
                        bx0b.w = {}
                        bub.w = {}
                KW = 2 * Lseg
                ksh = [sb(st, "hy_ks%d" % i, [128, KW], BF16) for i in range(2)]
                bks = [Buf(), Buf()]
                ytok = sb(st, "hy_y", [128, NB, 128], BF16)
                bytk = Buf()
                pcv = [ps(st, "hy_pc%d" % i, [128, NB]) for i in range(2)]
                bpcv = [Buf(), Buf()]
                pyt = [ps(st, "hy_pt%d" % i, [128, 512]) for i in range(2)]
                bpyt = [Buf(), Buf()]
                x0l = sb(st, "hy_x0l", [128, 512], BF16)
                yo = sb(st, "hy_yo", [128, 512], BF16)
                bx0l, byo = Buf(), Buf()
                for cj in range(2):
                    for cc in range(128):
                        c = cj * 128 + cc
                        k = c % 2
                        src = bass.AP(KD, c * (KD.shape[1]), [[1, 128], [1, KW]])
                        P.dma("sp", ksh[k][:], src, R=[bKD], W=[bks[k]])
                        items = []
                        dl = [0] + [d for d in range(-(NB - 1), NB) if d != 0]
                        for qi, d in enumerate(dl):
                            i0, i1 = max(0, d), min(NB - 1, NB - 1 + d)
                            ns = Lseg - 128 - 128 * d
                            items.append((pcv[k][:, i0:i1 + 1], ksh[k][:, ns:ns + 128], U[:, c, i0 - d:i1 + 1 - d],
                                          qi == 0, qi == len(dl) - 1))
                        P.mm(items, R=[bks[k], bU], W=[bpcv[k]])
                        if c % 2:
                            P.op("act", lambda e, k=k, cc=cc: e.activation(out=ytok[:, :, cc], in_=pcv[k][:, :], func=AF.Copy),
                                 R=[bpcv[k]], Wd=[bytk])
                        else:
                            P.op("dve", lambda e, k=k, cc=cc: e.tensor_copy(out=ytok[:, :, cc], in_=pcv[k][:, :]),
                                 R=[bpcv[k]], Wd=[bytk])
                    for t0 in range(0, Lseg, 512):
                        n = min(512, Lseg - t0)
                        kq = (t0 // 512) % 2
                        P.dma("sp", x0l[:, :n], X0C.ap()[cj * 128:(cj + 1) * 128, tbase + t0:tbase + t0 + n], R=[bX0C], W=[bx0l])
                        P.mm([(pyt[kq][:, q * 128:(q + 1) * 128], ytok[:, t0 // 128 + q, :], anti[:], True, True)
                              for q in range(n // 128)], R=[bytk, bconst], W=[bpyt[kq]])
                        P.op("dve", lambda e, kq=kq: e.tensor_tensor(out=yo[:, :n], in0=pyt[kq][:, :n], in1=x0l[:, :n],
                                                                    op=ALU.mult), R=[bpyt[kq], bx0l], W=[byo])
                        P.dma("sp", MIXT.ap()[256 + cj * 128:256 + (cj + 1) * 128, tbase + t0:tbase + t0 + n], yo[:, :n],
                              R=[byo], Wd=[bMIXT])
                    P._wait("act", P._deps([], [bytk], []))
                    P._wait("dve", P._deps([], [bytk], []))
                    bytk.w = {}

        with phase("attnprep") as st:
            wq = sb(st, "a_wq", [128, 2, 8, 128], BF16)
            wqs = sb(st, "a_wqs", [128, 2, 8, 32], BF16)
            wk = sb(st, "a_wk", [128, 8, 128], BF16)
            wv = sb(st, "a_wv", [128, 8, 64], BF16)
            bw = Buf()
            P.op("dve", lambda e: e.memset(wq[:], 0.0), W=[bw])
            P.op("dve", lambda e: e.memset(wk[:], 0.0), W=[bw])
            for kc in range(2):
                srcq = w_uq.ap()[l, kc * 128:(kc + 1) * 128, :].rearrange("p (h d) -> p h d", d=96)
                P.dma("pool", wq[:, kc, :, 0:32], srcq[:, :, 64:96], Wd=[bw])
                P.dma("pool", wq[:, kc, :, 64:128], srcq[:, :, 0:64], Wd=[bw])
                P.dma("pool", wqs[:, kc, :, 0:16], srcq[:, :, 80:96], Wd=[bw])
                P.dma("pool", wqs[:, kc, :, 16:32], srcq[:, :, 64:80], Wd=[bw])
            srck = w_ukv.ap()[l].rearrange("p (h d) -> p h d", d=128)
            P.dma("pool", wk[:, :, 64:128], srck[:, :, 0:64], Wd=[bw])
            P.dma("pool", wv[:, :, :], srck[:, :, 64:128], Wd=[bw])
            gsc = sb(st, "a_gsc", [128, 1])
            P.op("dve", lambda e: e.tensor_scalar(out=gsc[:], in0=V(l, "gqm"), scalar1=ATTN_SCALE, scalar2=None,
                                                  op0=ALU.mult), R=[bconst], W=[bw])
            gscs = sb(st, "a_gscs", [128, 1])
            P.op("dve", lambda e: e.tensor_scalar(out=gscs[:], in0=V(l, "gqs"), scalar1=ATTN_SCALE, scalar2=None,
                                                  op0=ALU.mult), R=[bconst], W=[bw])
            lat = sb(st, "a_lat", [128, 3, 512], BF16)
            sq = sb(st, "a_sq", [128, 3, 512], BF16)
            latn = sb(st, "a_latn", [128, 3, 512], BF16)
            rs = sb(st, "a_rs", [128, 512])
            cs = sb(st, "a_cos", [32, 512])
            sn = sb(st, "a_sin", [32, 512])
            kr = sb(st, "a_kr", [32, 2, 512], BF16)
            hm = sb(st, "a_hm", [128, 512])
            hs = sb(st, "a_hs", [32, 512])
            hsq = sb(st, "a_hsq", [128, 512], BF16)
            hrs = sb(st, "a_hrs", [128, 512])
            ho = sb(st, "a_ho", [128, 8, 512], BF16)
            vo = sb(st, "a_vo", [128, 4, 8, 64], BF16)
            pss = ps(st, "a_pss", [128, 512])
            pm = ps(st, "a_pm", [128, 512])
            psw = ps(st, "a_psw", [32, 512])
            pss2 = ps(st, "a_pss2", [128, 512])
            pv = ps(st, "a_pv", [128, 512])
            blat, bsq, blatn, brs, bcs, bkr, bhm, bhs, bhsq, bhrs, bho, bvo, bpss, bpm, bpsw, bpss2, bpv = [Buf() for _ in range(17)]

            def latent_norm(rows, nchunk, gname, n, c0):
                P.dma("sp", lat[:, 0:nchunk, :n], PT.ap()[rows:rows + nchunk * 128, c0:c0 + n].rearrange("(j p) t -> p j t", p=128),
                      R=[bPT], W=[blat])
                P.op("act", lambda e: e.activation(out=sq[:, 0:nchunk, :n], in_=lat[:, 0:nchunk, :n], func=AF.Square),
                     R=[blat], W=[bsq])
                P.mm([(pss[:, :n], onesb[:], sq[:, j, :n], j == 0, j == nchunk - 1) for j in range(nchunk)],
                     R=[bsq, bconst], W=[bpss])
                P.op("act", lambda e: e.activation(out=rs[:, :n], in_=pss[:, :n], func=AF.Sqrt, scale=1.0 / (128 * nchunk),
                                                   bias=EPS), R=[bpss], W=[brs])
                P.op("dve", lambda e: e.reciprocal(out=rs[:, :n], in_=rs[:, :n]), W=[brs])
                for j in range(nchunk):
                    P.op("dve", lambda e, j=j: e.scalar_tensor_tensor(out=latn[:, j, :n], in0=lat[:, j, :n],
                                                                     scalar=V(l, gname, j, 1), in1=rs[:, :n], op0=ALU.mult,
                                                                     op1=ALU.mult), R=[blat, brs, bconst], Wd=[blatn])

            def head_finish(h, n, t0, gm, gs, sw_from_psum, DST, bDST):
                P.op("act", lambda e: e.activation(out=hsq[:, :n], in_=hm_src[0][:, :n], func=AF.Square), R=[hm_src[1]], W=[bhsq])
                if sw_from_psum:
                    P.op("dve", lambda e: e.memset(hsq[32:64, :n], 0.0), W=[bhsq])
                P.mm([(pss2[:, :n], onesb[:], hsq[:, :n], True, True)], R=[bhsq, bconst], W=[bpss2])
                P.op("act", lambda e: e.activation(out=hrs[:, :n], in_=pss2[:, :n], func=AF.Sqrt, scale=1.0 / 96.0, bias=EPS),
                     R=[bpss2], W=[bhrs])
                P.op("dve", lambda e: e.reciprocal(out=hrs[:, :n], in_=hrs[:, :n]), W=[bhrs])
                P.op("dve", lambda e: e.scalar_tensor_tensor(out=hm[:, :n], in0=hm_src[0][:, :n], scalar=gm, in1=hrs[:, :n],
                                                             op0=ALU.mult, op1=ALU.mult), R=[hm_src[1], bhrs, bw], W=[bhm])
                P.op("dve", lambda e: e.scalar_tensor_tensor(out=hs[:, :n], in0=sw_src[0][0:32, :n], scalar=gs[0:32, :],
                                                             in1=hrs[0:32, :n], op0=ALU.mult, op1=ALU.mult),
                     R=[sw_src[1], bhrs, bw], W=[bhs])
                P.op("dve", lambda e: e.tensor_tensor(out=hm[0:32, :n], in0=hm[0:32, :n], in1=cs[:, :n], op=ALU.mult),
                     R=[bcs], W=[bhm])
                P.op("dve", lambda e: e.tensor_tensor(out=hs[:, :n], in0=hs[:, :n], in1=sn[:, :n], op=ALU.mult),
                     R=[bcs], W=[bhs])
                P.op("dve", lambda e: e.tensor_tensor(out=hm[0:32, :n], in0=hm[0:32, :n], in1=hs[:, :n], op=ALU.add),
                     R=[bhs], W=[bhm])
                P.op("act", lambda e: e.activation(out=ho[:, h, :n], in_=hm[:, :n], func=AF.Copy), R=[bhm], Wd=[bho])

            hm_src = [None, None]
            sw_src = [None, None]
            for (t0, n, seg) in chunks:
                c0 = pcol(seg) + (t0 - (0 if seg == 0 else S))
                P.dma("sp", cs[:, :n], c_cos.ap()[:, t0:t0 + n], W=[bcs])
                P.dma("sp", sn[:, :n], c_sin.ap()[:, t0:t0 + n], Wd=[bcs])
                latent_norm(1280, 1, "kvng", n, c0)
                P.dma("sp", kr[:, :, :n], PT.ap()[1408:1472, c0:c0 + n].rearrange("(a p) t -> p a t", p=32), R=[bPT], W=[bkr])
                P.mm([(pv[:, :].rearrange("p (a f) -> p a f", f=512)[:, 0, :] if False else pv[:, :],
                       latn[:, 0, q * 128:(q + 1) * 128], wv[:, :, :].rearrange("p h d -> p (h d)"), True, True)
                      for q in range(0)], R=[], W=[]) if False else None
                for q in range(n // 128):
                    P.mm([(pv[:, :], latn[:, 0, q * 128:(q + 1) * 128], wv[:, :, :].rearrange("p h d -> p (h d)"), True, True)],
                         R=[blatn, bw], W=[bpv])
                    P.op("act", lambda e, q=q: e.activation(out=vo[:, q, :, :].rearrange("p h d -> p (h d)"), in_=pv[:, :],
                                                           func=AF.Copy), R=[bpv], Wd=[bvo])
                for q in range(n // 128):
                    P.dma("sp", VT.ap()[:, t0 + q * 128:t0 + (q + 1) * 128, :].rearrange("h p d -> p h d"), vo[:, q, :, :],
                          R=[bvo], Wd=[bVT])
                for h in range(8):
                    P.mm([(pm[:, :n], wk[:, h, :], latn[:, 0, :n], True, True)], R=[blatn, bw], W=[bpm])
                    P.op("act", lambda e: e.activation(out=hm[64:128, :n], in_=pm[64:128, :n], func=AF.Copy), R=[bpm], W=[bhm])
                    P.op("pool", lambda e: e.tensor_copy(out=hm[0:32, :n], in_=kr[:, 0, :n]), R=[bkr], W=[bhm])
                    P.op("pool", lambda e: e.memset(hm[32:64, :n], 0.0), W=[bhm])
                    hm_src[0], hm_src[1] = hm, bhm
                    sw_src[0], sw_src[1] = kr[:, 1, :], bkr
                    head_finish(h, n, t0, V(l, "gkm"), V(l, "gks"), False, KT, bKT)
                P.dma("sp", KT.ap()[:, :, t0:t0 + n].rearrange("h p t -> p h t"), ho[:, :, :n], R=[bho], Wd=[bKT])
                P._wait("act", P._deps([], [bho, bvo], []))
                bho.w = {}
                bvo.w = {}
                if seg == 1 and last:
                    continue
                latent_norm(1024, 2, "qng", n, c0)
                for h in range(8):
                    P.mm([(pm[:, :n], wq[:, kc, h, :], latn[:, kc, :n], kc == 0, kc == 1) for kc in range(2)],
                         R=[blatn, bw], W=[bpm])
                    P.mm([(psw[:, :n], wqs[:, kc, h, :], latn[:, kc, :n], kc == 0, kc == 1) for kc in range(2)],
                         R=[blatn, bw], W=[bpsw])
                    hm_src[0], hm_src[1] = pm, bpm
                    sw_src[0], sw_src[1] = psw, bpsw
                    head_finish(h, n, t0, gsc[:, :], gscs, True, QT, bQT)
                P.dma("sp", QT.ap()[:, :, t0:t0 + n].rearrange("h p t -> p h t"), ho[:, :, :n], R=[bho], Wd=[bQT])
                P._wait("act", P._deps([], [bho], []))
                bho.w = {}

        with phase("attn") as st:
            NKT = T // 128
            kh = [sb(st, "at_k%d" % i, [128, T], BF16) for i in range(2)]
            vh = [sb(st, "at_v%d" % i, [128, NKT, 64], BF16) for i in range(2)]
            bkh = [Buf(), Buf()]
            qc = [sb(st, "at_q%d" % i, [128, 512], BF16) for i in range(2)]
            bqc = [Buf(), Buf()]
            et = [sb(st, "at_e%d" % i, [128, 512], BF16) for i in range(3)]
            bet = [Buf() for _ in range(3)]
            pS = [ps(st, "at_ps%d" % i, [128, 512]) for i in range(3)]
            bpS = [Buf() for _ in range(3)]
            pO = [ps(st, "at_po%d" % i, [64, 512]) for i in range(2)]
            pD = [ps(st, "at_pd%d" % i, [64, 512]) for i in range(2)]
            bpO = [Buf(), Buf()]
            rc = sb(st, "at_rc", [64, 512])
            oo = sb(st, "at_oo", [64, 512], BF16)
            brc, boo = Buf(), Buf()
            cnt = 0
            qi = 0
            for h in range(8):
                hk = h % 2
                P.dma("sp", kh[hk][:], KT.ap()[h], R=[bKT], W=[bkh[hk]])
                P.dma("sp", vh[hk][:], VT.ap()[h].rearrange("(q p) d -> p q d", p=128), R=[bVT], Wd=[bkh[hk]])
                for (t0, n, seg) in chunks:
                    if seg == 1 and last:
                        continue
                    kts = list(range(S // 128, NKT)) + (list(range(0, S // 128)) if seg == 0 else [])
                    q_ = qi % 2
                    qi += 1
                    P.dma("sp", qc[q_][:, :n], QT.ap()[h, :, t0:t0 + n], R=[bQT], W=[bqc[q_]])
                    for ki, kt in enumerate(kts):
                        s_ = cnt % 3
                        cnt += 1
                        P.mm([(pS[s_][:, :n], kh[hk][:, kt * 128:(kt + 1) * 128], qc[q_][:, :n], True, True)],
                             R=[bkh[hk], bqc[q_]], W=[bpS[s_]])
                        P.op("act", lambda e, s_=s_: e.activation(out=et[s_][:, :n], in_=pS[s_][:, :n], func=AF.Exp),
                             R=[bpS[s_]], W=[bet[s_]])
                        P.mm([(pO[q_][:, :n], vh[hk][:, kt, :], et[s_][:, :n], ki == 0, ki == len(kts) - 1),
                              (pD[q_][:, :n], onesb[:, 0:64], et[s_][:, :n], ki == 0, ki == len(kts) - 1)],
                             R=[bet[s_], bkh[hk], bconst], Wd=[bpO[q_]] if ki else (), W=[bpO[q_]] if ki == 0 else ())
                    P.op("dve", lambda e, q_=q_: e.reciprocal(out=rc[:, :n], in_=pD[q_][:, :n]), R=[bpO[q_]], W=[brc])
                    P.op("dve", lambda e, q_=q_: e.tensor_tensor(out=oo[:, :n], in0=pO[q_][:, :n], in1=rc[:, :n], op=ALU.mult),
                         R=[bpO[q_], brc], W=[boo])
                    P.dma("sp", MIXT.ap()[512 + h * 64:512 + (h + 1) * 64, t0:t0 + n], oo[:, :n], R=[boo], Wd=[bMIXT])

        with phase("wout") as st:
            wo = sb(st, "o_w", [128, 8, D], BF16)
            rw = sb(st, "o_rw", [128, 8, NE])
            rb = sb(st, "o_rb", [1, NE])
            bwo = Buf()
            P.dma("pool", wo[:], w_out.ap()[l].rearrange("(kc p) n -> p kc n", p=128), Wd=[bwo])
            P.dma("sp", rw[:], r_w.ap()[l].rearrange("(kc p) n -> p kc n", p=128), Wd=[bwo])
            P.dma("sp", rb[:], r_b.ap()[l], Wd=[bwo])
            mx = sb(st, "o_mx", [128, 8, 512], BF16)
            xt = sb(st, "o_xt", [128, 8, 512])
            bmx, bxt = Buf(), Buf()
            po_ = [ps(st, "o_ps%d" % i, [128, 512]) for i in range(2)]
            bpo_ = [Buf(), Buf()]
            nt = norm_tiles(st)
            h2 = sb(st, "o_h2", [128, 8, 512], BF16)
            h2f = sb(st, "o_h2f", [128, 8, 512])
            bh2 = Buf()
            plg = ps(st, "o_plg", [128, NE])
            pgt = ps(st, "o_pgt", [NE, 512])
            lg = sb(st, "o_lg", [128, NE])
            m8 = sb(st, "o_m8", [128, 8])
            msk = sb(st, "o_msk", [128, NE])
            ex = sb(st, "o_ex", [128, NE])
            ssum = sb(st, "o_ss", [128, 2])
            gts = sb(st, "o_gts", [NE, 512])
            bplg, bpgt, blg, bm8, bmsk, bex, bss, bgts = [Buf() for _ in range(8)]
            for (t0, n, seg) in chunks:
                if seg == 1 and last:
                    continue
                P.dma("sp", mx[:, :, :n], MIXT.ap()[:, t0:t0 + n].rearrange("(kc p) t -> p kc t", p=128), R=[bMIXT], W=[bmx])
                P.dma("sp", xt[:, :, :n], XTv[:, :, t0:t0 + n], R=[bXT], W=[bxt])
                for j in range(8):
                    k = j % 2
                    P.mm([(po_[k][:, :n], wo[:, kc, j * 128:(j + 1) * 128], mx[:, kc, :n], kc == 0, kc == 7) for kc in range(8)],
                         R=[bwo, bmx], W=[bpo_[k]])
                    P.op("dve", lambda e, j=j, k=k: e.scalar_tensor_tensor(out=xt[:, j, :n], in0=po_[k][:, :n],
                                                                          scalar=MOD(2, j, seg), in1=xt[:, j, :n],
                                                                          op0=ALU.mult, op1=ALU.add),
                         R=[bpo_[k], bmodv], W=[bxt])
                P.dma("sp", XTv[:, :, t0:t0 + n], xt[:, :, :n], R=[bxt], Wd=[bXT])
                norm_chunk(nt, t0, n, seg, A2, 3, h2, bh2, hF=h2f)
                P.dma("sp", H2T.ap()[:, t0:t0 + n].rearrange("(kc p) t -> p kc t", p=128), h2[:, :, :n], R=[bh2], Wd=[bH2T])
                for q in range(n // 128):
                    items = [(plg[:, :], h2f[:, kc, q * 128:(q + 1) * 128], rw[:, kc, :], kc == 0, False) for kc in range(8)]
                    items.append((plg[:, :], onesf[0:1, :], rb[0:1, :], False, True))
                    P.mm(items, R=[bh2, bwo, bconst], W=[bplg])
                    P.op("dve", lambda e: e.tensor_copy(out=lg[:], in_=plg[:]), R=[bplg], W=[blg])
                    P.op("dve", lambda e: e.max(out=m8[:], in_=lg[:]), R=[blg], W=[bm8])
                    P.op("dve", lambda e: e.tensor_scalar(out=msk[:], in0=lg[:], scalar1=m8[:, 3:4], scalar2=None,
                                                          op0=ALU.is_ge), R=[blg, bm8], W=[bmsk])
                    P.op("dve", lambda e: e.tensor_scalar(out=ssum[:, 1:2], in0=m8[:, 0:1], scalar1=-1.0, scalar2=None,
                                                          op0=ALU.mult), R=[bm8], W=[bss])
                    P.op("act", lambda e: e.activation(out=ex[:], in_=lg[:], func=AF.Exp, bias=ssum[:, 1:2]),
                         R=[blg, bss], W=[bex])
                    P.op("dve", lambda e: e.tensor_tensor(out=ex[:], in0=ex[:], in1=msk[:], op=ALU.mult), R=[bmsk], W=[bex])
                    P.op("dve", lambda e: e.reduce_sum(out=ssum[:, 0:1], in_=ex[:], axis=AX.X), R=[bex], W=[bss])
                    P.op("dve", lambda e: e.reciprocal(out=ssum[:, 0:1], in_=ssum[:, 0:1]), W=[bss])
                    P.op("dve", lambda e: e.tensor_scalar(out=ex[:], in0=ex[:], scalar1=ssum[:, 0:1], scalar2=None,
                                                          op0=ALU.mult), R=[bss], W=[bex])
                    P._wait("pe", P._deps([bex, bconst], [], [bpgt]))
                    ins = nc.tensor.transpose(pgt[:, q * 128:(q + 1) * 128], ex[:], idf[:])
                    P.cnt["pe"] += 1
                    ins.then_inc(P.sem["pe"], 1)
                    P._commit(("e", "pe"), P.cnt["pe"], [bex, bconst], [], [bpgt])
                P.op("act", lambda e: e.activation(out=gts[:, :n], in_=pgt[:, :n], func=AF.Copy), R=[bpgt], W=[bgts])
                P.dma("sp", GT.ap()[:, t0:t0 + n], gts[:, :n], R=[bgts], Wd=[bGT])
                P._wait("pe", P._deps([], [bpgt], []))
                bpgt.w = {}
                P._wait("act", P._deps([], [bh2], []))
                P._wait("pool", P._deps([], [bh2], []))
                bh2.w = {}

        with phase("moe") as st:
            TG = 1024
            NSL = 4
            NR = 2
            wsl = [sb(st, "m_w%d" % i, [128, 8, 1024], BF16) for i in range(NSL)]
            bws = [Buf() for _ in range(NSL)]
            b2 = sb(st, "m_b2", [NE, D], BF16)
            sel = sb(st, "m_sel", [NE, NE, 128], BF16)
            bb2 = Buf()
            P.dma("pool", b2[:], m_b2.ap()[l], Wd=[bb2])
            P.op("dve", lambda e: e.memset(sel[:], 0.0), W=[bb2])
            P.op("dve", lambda e: e.tensor_tensor(out=sel[:], in0=sel[:],
                                                  in1=idf[0:NE, 0:NE].unsqueeze(2).to_broadcast([NE, NE, 128]), op=ALU.add),
                 R=[bconst], W=[bb2])
            b1s = sb(st, "m_b1s", [128, 512])
            b1p = sb(st, "m_b1p", [128, 512])
            bb1 = Buf()
            o1 = VCOLS["b1"][0]
            P.op("dve", lambda e: e.tensor_scalar(out=b1s[:], in0=vec[:, l, o1:o1 + 512], scalar1=1.702, scalar2=None,
                                                  op0=ALU.mult), R=[bconst], W=[bb1])
            P.op("dve", lambda e: e.tensor_scalar(out=b1p[:], in0=vec[:, l, o1:o1 + 512], scalar1=1.0, scalar2=None,
                                                  op0=ALU.add), R=[bconst], Wd=[bb1])
            hh = sb(st, "m_h", [128, 8, TG], BF16)
            acc = sb(st, "m_acc", [128, 8, TG])
            at = sb(st, "m_at", [128, 8, TG], BF16)
            gt = sb(st, "m_gt", [NE, TG], BF16)
            gtf = sb(st, "m_gtf", [NE, TG])
            xt = sb(st, "m_xt", [128, 512])
            bhh, bacc, bgt, bxt = [Buf() for _ in range(4)]
            bat = [Buf(), Buf()]
            glu = [sb(st, "m_glu%d" % i, [128, 512]) for i in range(NR)]
            sg = [sb(st, "m_sg%d" % i, [128, 512]) for i in range(NR)]
            ln = [sb(st, "m_ln%d" % i, [128, 512]) for i in range(NR)]
            bglu = [Buf() for _ in range(NR)]
            bsg = [Buf() for _ in range(NR)]
            bln = [Buf() for _ in range(NR)]
            gbc = [sb(st, "m_gbc%d" % i, [128, 512]) for i in range(2)]
            bgbc = [Buf(), Buf()]
            pg = [ps(st, "m_pg%d" % i, [128, 512]) for i in range(2)]
            pl = [ps(st, "m_pl%d" % i, [128, 512]) for i in range(2)]
            py = [ps(st, "m_py%d" % i, [128, 512]) for i in range(2)]
            pbc = ps(st, "m_pbc", [128, 512])
            bpbc = Buf()
            bpg, bpl, bpy = [[Buf(), Buf()] for _ in range(3)]
            Tm = T if not last else S
            slot = 0
            ycnt = 0
            tcnt = 0
            gcnt = 0
            for g0 in range(0, Tm, TG):
                ng = min(TG, Tm - g0)
                halves = [(hs_, min(512, ng - hs_)) for hs_ in range(0, ng, 512)]
                P.dma("sp", hh[:, :, :ng], H2T.ap()[:, g0:g0 + ng].rearrange("(kc p) t -> p kc t", p=128), R=[bH2T], W=[bhh])
                P.dma("sp", gtf[:, :ng], GT.ap()[:, g0:g0 + ng], R=[bGT], W=[bgt])
                P.op("pool", lambda e: e.tensor_copy(out=gt[:, :ng], in_=gtf[:, :ng]), W=[bgt])
                for ex_ in range(NE):
                    s1, s2_, s3 = slot % NSL, (slot + 1) % NSL, (slot + 2) % NSL
                    slot += 3
                    w1src = W1B.ap()[ex_].rearrange("(kc p) n -> p kc n", p=128)
                    P.dma("sp", wsl[s1][:], w1src[:, :, 0:1024], R=[bW1B], W=[bws[s1]])
                    P.dma("sp", wsl[s2_][:], w1src[:, :, 1024:2048], R=[bW1B], W=[bws[s2_]])
                    P.dma("sp", wsl[s3][:], W2B.ap()[ex_].rearrange("(kc p) n -> p kc n", p=128), R=[bW2B], W=[bws[s3]])
                    b1o = ex_ * 16
                    for hi, (hs_, hn) in enumerate(halves):
                        hsl = slice(hs_, hs_ + hn)
                        kb_ = gcnt % 2
                        gcnt += 1
                        P.mm([(pbc[:, :hn], sel[:, ex_, :], gt[:, hsl], True, True)], R=[bb2, bgt], W=[bpbc])
                        P.op("act", lambda e, kb_=kb_: e.activation(out=gbc[kb_][:, :hn], in_=pbc[:, :hn], func=AF.Copy),
                             R=[bpbc], W=[bgbc[kb_]])
                        for j in range(8):
                            k = tcnt % 2
                            r = tcnt % NR
                            tcnt += 1
                            P.mm([(pg[k][:, :hn], wsl[s1][:, kc, j * 128:(j + 1) * 128], hh[:, kc, hsl], kc == 0, kc == 7)
                                  for kc in range(8)], R=[bws[s1], bhh], W=[bpg[k]])
                            P.mm([(pl[k][:, :hn], wsl[s2_][:, kc, j * 128:(j + 1) * 128], hh[:, kc, hsl], kc == 0, kc == 7)
                                  for kc in range(8)], R=[bws[s2_], bhh], W=[bpl[k]])
                            cg = b1o + j
                            cl = b1o + 8 + j
                            P.op("dve", lambda e, k=k, r=r, cg=cg: e.tensor_scalar(out=glu[r][:, :hn], in0=pg[k][:, :hn],
                                                                                  scalar1=vec[:, l, o1 + cg:o1 + cg + 1],
                                                                                  scalar2=7.0, op0=ALU.add, op1=ALU.min),
                                 R=[bpg[k], bconst], W=[bglu[r]])
                            P.op("act", lambda e, k=k, r=r, cg=cg: e.activation(out=sg[r][:, :hn], in_=glu[r][:, :hn],
                                                                               func=AF.Sigmoid, scale=1.702),
                                 R=[bglu[r]], W=[bsg[r]])
                            P.op("dve", lambda e, k=k, r=r, cl=cl: e.tensor_scalar(out=ln[r][:, :hn], in0=pl[k][:, :hn],
                                                                                  scalar1=b1p[:, cl:cl + 1], scalar2=8.0,
                                                                                  op0=ALU.add, op1=ALU.min),
                                 R=[bpl[k], bb1], W=[bln[r]])
                            P.op("dve", lambda e, r=r: e.scalar_tensor_tensor(out=glu[r][:, :hn], in0=sg[r][:, :hn],
                                                                             scalar=SIG_CLAMP, in1=glu[r][:, :hn],
                                                                             op0=ALU.min, op1=ALU.mult),
                                 R=[bsg[r]], W=[bglu[r]])
                            P.op("dve", lambda e, r=r: e.scalar_tensor_tensor(out=ln[r][:, :hn], in0=ln[r][:, :hn],
                                                                             scalar=-6.0, in1=glu[r][:, :hn],
                                                                             op0=ALU.max, op1=ALU.mult),
                                 R=[bglu[r]], W=[bln[r]])
                            P.op("pool", lambda e, r=r, j=j, kb_=kb_, hsl=hsl: e.tensor_tensor(
                                out=at[:, j, hsl], in0=ln[r][:, :hn], in1=gbc[kb_][:, :hn], op=ALU.mult),
                                R=[bln[r], bgbc[kb_]], Wd=[bat[hi]])
                    for hi, (hs_, hn) in enumerate(halves):
                        hsl = slice(hs_, hs_ + hn)
                        for j in range(8):
                            k = ycnt % 2
                            ycnt += 1
                            items = [(py[k][:, :hn], wsl[s3][:, kc, j * 128:(j + 1) * 128], at[:, kc, hsl], kc == 0, kc == 7)
                                     for kc in range(8)]
                            if ex_ == 0:
                                o_, l_, r_, st_, _ = items[-1]
                                items[-1] = (o_, l_, r_, st_, False)
                                items.append((py[k][:, :hn], b2[:, j * 128:(j + 1) * 128], gt[:, hsl], False, True))
                            P.mm(items, R=[bws[s3], bat[hi], bb2, bgt], W=[bpy[k]])
                            if ex_ == 0:
                                P.op("act", lambda e, k=k, j=j, hsl=hsl, hn=hn: e.activation(out=acc[:, j, hsl], in_=py[k][:, :hn],
                                                                                            func=AF.Copy),
                                     R=[bpy[k]], Wd=[bacc])
                            else:
                                P.op("dve", lambda e, k=k, j=j, hsl=hsl, hn=hn: e.tensor_tensor(out=acc[:, j, hsl], in0=acc[:, j, hsl],
                                                                                               in1=py[k][:, :hn], op=ALU.add),
                                     R=[bpy[k]], Wd=[bacc])
                for (hs_, hn) in halves:
                    t0 = g0 + hs_
                    seg = 0 if t0 < S else 1
                    for j in range(8):
                        P.dma("sp", xt[:, :hn], XT.ap()[j, :, t0:t0 + hn], R=[bXT], W=[bxt])
                        P.op("dve", lambda e, j=j, hs_=hs_, hn=hn, seg=seg: e.scalar_tensor_tensor(
                            out=xt[:, :hn], in0=acc[:, j, hs_:hs_ + hn], scalar=MOD(5, j, seg), in1=xt[:, :hn],
                            op0=ALU.mult, op1=ALU.add), R=[bacc, bmodv], W=[bxt])
                        P.dma("sp", XT.ap()[j, :, t0:t0 + hn], xt[:, :hn], R=[bxt], Wd=[bXT])
                P._wait("act", P._deps([], [bacc], []))
                P._wait("dve", P._deps([], [bacc], []))
                bacc.w = {}
        modst.close()

    with phase("final") as st:
        xl = [sb(st, "f_xl%d" % i, [128, 8, 128]) for i in range(2)]
        yo_ = [sb(st, "f_yo%d" % i, [128, D]) for i in range(2)]
        pf = [ps(st, "f_ps%d" % i, [128, D]) for i in range(2)]
        bxl, byo_, bpf = [Buf(), Buf()], [Buf(), Buf()], [Buf(), Buf()]
        for ti in range(S // 128):
            k = ti % 2
            P.dma("sp", xl[k][:], XTv[:, :, ti * 128:(ti + 1) * 128], R=[bXT], W=[bxl[k]])
            P._wait("pe", P._deps([bxl[k], bconst], [bpf[k]], []))
            ins = None
            for j in range(8):
                ins = nc.tensor.transpose(pf[k][:, j * 128:(j + 1) * 128], xl[k][:, j, :], idf[:])
            P.cnt["pe"] += 1
            ins.then_inc(P.sem["pe"], 1)
            P._commit(("e", "pe"), P.cnt["pe"], [bxl[k], bconst], [bpf[k]], [])
            P.op("act", lambda e, k=k: e.activation(out=yo_[k][:], in_=pf[k][:], func=AF.Copy), R=[bpf[k]], W=[byo_[k]])
            P.dma("sp", y_out.ap()[ti * 128:(ti + 1) * 128, :], yo_[k][:], R=[byo_[k]])
    P.finish()
    es.close()
    return nc


def _tables(S):
    T = S + CT
    n_rows = S // GRID_W
    row = np.repeat(np.arange(n_rows, dtype=np.float32), GRID_W)
    col = np.tile(np.arange(GRID_W, dtype=np.float32), n_rows)
    inv = (10000.0 ** (-np.arange(8, dtype=np.float32) / 8)).astype(np.float32)
    ang = np.concatenate([row[:, None] * inv, col[:, None] * inv], axis=-1).astype(np.float32)
    cos = np.ones((32, T), np.float32)
    sin = np.zeros((32, T), np.float32)
    cos[0:16, :S] = np.cos(ang).T
    cos[16:32, :S] = np.cos(ang).T
    sin[0:16, :S] = -np.sin(ang).T
    sin[16:32, :S] = np.sin(ang).T
    invc = np.zeros((128, 2, T), np.float32)
    for g, w in enumerate(POOL_WINDOWS):
        for (L, off) in [(S, 0), (CT, S)]:
            t = np.arange(L)
            lo = np.clip(t - w // 2, 0, L)
            hi = np.clip(t + w // 2, 0, L)
            invc[(g % 2) * 64:(g % 2) * 64 + 64, g // 2, off:off + L] = (1.0 / (hi - lo).astype(np.float32))[None, :]
    deltas = np.abs(np.linspace(HY_MIN_DECAY, HY_MAX_DECAY, 256, dtype=np.float32))

    def filt(L):
        R = 2 * L + 512
        z = np.zeros((17, R), np.float32)
        df = np.zeros((256, R), np.float32)
        db = np.zeros((256, R), np.float32)
        m = np.arange(R)
        lag = (L - 1) - m
        valid = np.abs(lag) <= L - 1
        idx = np.abs(lag)[valid]
        tt = np.linspace(0.0, 1.0, L, dtype=np.float32)[idx]
        wpos = ((2.0 * math.pi / L) * np.arange(L, dtype=np.float32))[idx]
        f = np.linspace(1e-4, 7, 8, dtype=np.float32)
        zz = np.concatenate([tt[None, :], np.cos(f[:, None] * wpos[None, :]), -np.sin(f[:, None] * wpos[None, :])], axis=0)
        z[:, valid] = zz.astype(np.float32)
        dec = np.exp(-tt[None, :] * deltas[:, None]).astype(np.float32)
        lv = lag[valid]
        dfv = np.where(lv[None, :] >= 0, dec, 0.0)
        dbv = np.where(lv[None, :] < 0, dec, 0.0)
        df[:, valid] = dfv
        db[:, valid] = dbv
        return z, df, db

    zl, dfl, dbl = filt(S)
    zc, dfc, dbc = filt(CT)
    idf = np.eye(128, dtype=np.float32)
    return {"c_idf": idf, "c_idb": idf.astype(ml_dtypes.bfloat16), "c_anti": idf[::-1].copy().astype(ml_dtypes.bfloat16),
            "c_cos": cos, "c_sin": sin, "c_invc": invc, "c_zl": zl, "c_dfl": dfl, "c_dbl": dbl,
            "c_zc": zc, "c_dfc": dfc, "c_dbc": dbc}


def _pack_vecs(inp, b, NL):
    v = np.zeros((NL, 128, NV), np.float32)

    def put(l, name, arr, j0=0):
        o, w = VCOLS[name]
        arr = np.asarray(arr, np.float32).reshape(-1, 128)
        v[l, :, o + j0:o + j0 + arr.shape[0]] = arr.T

    for l in range(NL):
        put(l, "n1g", inp["norm1_g"][l])
        put(l, "n2g", inp["norm2_g"][l])
        put(l, "bmod", inp["b_mod"][l])
        put(l, "c", inp["c"][b])
        put(l, "cctx", inp["c_ctx"])
        put(l, "psc", inp["pool_scale"][l])
        for tap in range(3):
            put(l, "hcw", inp["hy_conv_w"][l, tap], tap * 6)
        put(l, "hcb", inp["hy_conv_b"][l])
        put(l, "hyb", inp["hy_bias"][l])
        put(l, "qng", inp["mla_q_norm_g"][l])
        put(l, "kvng", inp["mla_kv_norm_g"][l])
        for nm, g in (("q", inp["qk_norm_q"][l]), ("k", inp["qk_norm_k"][l])):
            main = np.zeros(128, np.float32)
            main[0:32] = g[64:96]
            main[64:128] = g[0:64]
            sw = np.zeros(128, np.float32)
            sw[0:16] = g[80:96]
            sw[16:32] = g[64:80]
            put(l, "g%sm" % nm, main)
            put(l, "g%ss" % nm, sw)
        for nm, src in (("fb1", "hy_f_b1"), ("fb2", "hy_f_b2"), ("freq", "hy_freq")):
            a = np.zeros(128, np.float32)
            a[0:64] = inp[src][l]
            put(l, nm, a)
        b1 = np.asarray(inp["moe_b1"][l], np.float32).reshape(NE, 16, 128)
        o, w = VCOLS["b1"]
        v[l, :, o:o + w] = b1.transpose(2, 0, 1).reshape(128, NE * 16)
    return v


_NC_CACHE = {}


def run(inputs, S, NL, batches, dbg=False):
    inp = {k: np.asarray(v) for k, v in inputs.items()}
    key = (S, NL, dbg)
    if key not in _NC_CACHE:
        _NC_CACHE[key] = build(S, NL, dbg)
    nc = _NC_CACHE[key]
    tabs = _tables(S)
    shared = dict(tabs)
    for nm in ["w_mod", "w_in", "pool_w", "hy_f_w1", "hy_f_w2", "hy_f_w3", "mla_w_uq", "mla_w_ukv", "w_out", "router_w",
               "moe_w1", "moe_w2", "moe_b2"]:
        shared[nm] = np.ascontiguousarray(inp[nm][:NL], dtype=np.float32)
    shared["router_b"] = np.ascontiguousarray(inp["router_b"][:NL, None, :], dtype=np.float32)
    in_maps = []
    for b in batches:
        m = dict(shared)
        m["x"] = np.ascontiguousarray(inp["x"][b, :S], dtype=np.float32)
        m["ctx"] = np.ascontiguousarray(inp["ctx"][b], dtype=np.float32)
        m["vecs"] = _pack_vecs(inp, b, NL)
        in_maps.append(m)
    res = run_bass_kernel_spmd(nc, in_maps, core_ids=list(range(len(batches))))
    return res


def kernel(**inputs):
    B, S, _ = inputs["x"].shape
    res = run(inputs, S, 2, list(range(B)))
    return np.stack([np.asarray(r["y"], dtype=np.float32) for r in res.results], axis=0)
```

```python
import math
from contextlib import ExitStack
import numpy as np
import ml_dtypes
import concourse.bass as bass
import concourse.mybir as mybir
from concourse.bass_utils import run_bass_kernel_spmd

F32 = mybir.dt.float32
BF16 = mybir.dt.bfloat16
ALU = mybir.AluOpType
AF = mybir.ActivationFunctionType
AX = mybir.AxisListType

D = 1024
CT = 256
NE = 32
NPT = 1472
EPS = 1e-6
GRID_W = 64
POOL_WINDOWS = (2, 4, 8, 16)
HY_MIN_DECAY = math.log(1e-2) / 1.5
HY_MAX_DECAY = math.log(1e-2) / 0.3
ATTN_SCALE = 96 ** -0.5
SIG_CLAMP = float(1.0 / (1.0 + math.exp(-1.702 * 7.0)))

VCOLS = {}
_o = 0
for _n, _w in [("n1g", 8), ("n2g", 8), ("bmod", 48), ("c", 8), ("cctx", 8), ("psc", 2), ("hcw", 18), ("hcb", 6),
               ("hyb", 2), ("qng", 2), ("kvng", 1), ("gqm", 1), ("gqs", 1), ("gkm", 1), ("gks", 1),
               ("fb1", 1), ("fb2", 1), ("freq", 1), ("b1", 512)]:
    VCOLS[_n] = (_o, _w)
    _o += _w
NV = _o


class Buf:
    __slots__ = ("w", "r")

    def __init__(self):
        self.w = {}
        self.r = {}


class Prog:
    NS = 32

    def __init__(self, nc, es):
        self.nc = nc
        self.E = {"pe": nc.tensor, "act": nc.scalar, "dve": nc.vector, "pool": nc.gpsimd, "sp": nc.sync}
        self.sem = {k: es.enter_context(nc.semaphore("s_" + k)) for k in self.E}
        self.cnt = {k: 0 for k in self.E}
        self.dsem = [es.enter_context(nc.semaphore("d%d" % i)) for i in range(self.NS)]
        self.dtot = [0] * self.NS
        self.dn = 0
        self.NX = 8
        self.xsem = [es.enter_context(nc.semaphore("x%d" % i)) for i in range(self.NX)]
        self.xtot = [0] * self.NX
        self.xn = 0
        self.seen = {k: {} for k in self.E}

    def _wait(self, e, deps):
        for key, n in deps.items():
            if n <= 0 or (key == ("e", "pe") and e == "pe"):
                continue
            if self.seen[e].get(key, 0) >= n:
                continue
            sem = self.sem[key[1]] if key[0] == "e" else (self.dsem[key[1]] if key[0] == "d" else self.xsem[key[1]])
            self.E[e].wait_ge(sem, n)
            self.seen[e][key] = n

    @staticmethod
    def _deps(R, W, Wd):
        d = {}
        for b in R:
            for k, n in b.w.items():
                d[k] = max(d.get(k, 0), n)
        for b in W:
            for k, n in b.w.items():
                d[k] = max(d.get(k, 0), n)
            for k, n in b.r.items():
                d[k] = max(d.get(k, 0), n)
        for b in Wd:
            for k, n in b.r.items():
                d[k] = max(d.get(k, 0), n)
        return d

    @staticmethod
    def _commit(key, n, R, W, Wd):
        for b in R:
            b.r[key] = max(b.r.get(key, 0), n)
        for b in W:
            b.w = {key: n}
            b.r = {}
        for b in Wd:
            b.w[key] = max(b.w.get(key, 0), n)

    def op(self, e, fn, R=(), W=(), Wd=()):
        self._wait(e, self._deps(R, W, Wd))
        ins = fn(self.E[e])
        self.cnt[e] += 1
        ins.then_inc(self.sem[e], 1)
        self._commit(("e", e), self.cnt[e], R, W, Wd)

    def mm(self, items, R=(), W=(), Wd=()):
        self._wait("pe", self._deps(R, W, Wd))
        ins = None
        for (o, l, r, st, sp) in items:
            ins = self.nc.tensor.matmul(o, l, r, start=st, stop=sp)
        self.cnt["pe"] += 1
        ins.then_inc(self.sem["pe"], 1)
        self._commit(("e", "pe"), self.cnt["pe"], R, W, Wd)

    def dma(self, e, out, in_, R=(), W=(), Wd=(), **kw):
        i = self.dn
        self.dn = (i + 1) % self.NS
        d = self._deps(R, W, Wd)
        if self.dtot[i]:
            d[("d", i)] = max(d.get(("d", i), 0), self.dtot[i])
        self._wait(e, d)
        ins = self.E[e].dma_start(out=out, in_=in_, **kw)
        self.dtot[i] += 16
        ins.then_inc(self.dsem[i], 16)
        self._commit(("d", i), self.dtot[i], R, W, Wd)

    def barrier(self):
        for e in self.E:
            d = {}
            for k in self.E:
                if k != e and self.cnt[k]:
                    d[("e", k)] = self.cnt[k]
            for i in range(self.NS):
                if self.dtot[i]:
                    d[("d", i)] = self.dtot[i]
            self._wait(e, d)

    def bgdma(self, e, out, in_, R=(), W=(), Wd=(), **kw):
        i = self.xn
        self.xn = (i + 1) % self.NX
        d = self._deps(R, W, Wd)
        if self.xtot[i]:
            d[("x", i)] = max(d.get(("x", i), 0), self.xtot[i])
        self._wait(e, d)
        ins = self.E[e].dma_start(out=out, in_=in_, **kw)
        self.xtot[i] += 16
        ins.then_inc(self.xsem[i], 16)
        self._commit(("x", i), self.xtot[i], R, W, Wd)

    def finish(self):
        for i in range(self.NX):
            if self.xtot[i]:
                self.nc.sync.wait_ge(self.xsem[i], self.xtot[i])
        for i in range(self.NS):
            if self.dtot[i]:
                self.nc.sync.wait_ge(self.dsem[i], self.dtot[i])
        for k in self.E:
            if k != "sp" and self.cnt[k]:
                self.nc.sync.wait_ge(self.sem[k], self.cnt[k])


def build(S, NL, dbg=False):
    T = S + CT
    NLC = S // 512
    chunks = [(i * 512, 512, 0) for i in range(NLC)] + [(S, CT, 1)]
    PTW = S + 24 + CT + 8
    RL = 2 * S + 512
    RC = 2 * CT + 512
    nc = bass.Bass("TRN2", target_bir_lowering=False)
    es = ExitStack()
    P = Prog(nc, es)

    def din(name, shape, dt=F32):
        return nc.dram_tensor(name, list(shape), dt, kind="ExternalInput")

    def dscr(name, shape, dt):
        return nc.dram_tensor(name, list(shape), dt, kind="ExternalOutput" if dbg else "Internal")

    x_in = din("x", [S, D])
    ctx_in = din("ctx", [CT, D])
    vecs = din("vecs", [NL, 128, NV])
    w_mod = din("w_mod", [NL, D, 6 * D])
    w_in = din("w_in", [NL, D, 1440])
    pool_w = din("pool_w", [NL, 4, 64, 64])
    f_w1 = din("hy_f_w1", [NL, 17, 64])
    f_w2 = din("hy_f_w2", [NL, 64, 64])
    f_w3 = din("hy_f_w3", [NL, 64, 512])
    w_uq = din("mla_w_uq", [NL, 256, 768])
    w_ukv = din("mla_w_ukv", [NL, 128, 1024])
    w_out = din("w_out", [NL, D, D])
    r_w = din("router_w", [NL, D, NE])
    r_b = din("router_b", [NL, 1, NE])
    m_w1 = din("moe_w1", [NL, NE, D, 2 * D])
    m_w2 = din("moe_w2", [NL, NE, D, D])
    m_b2 = din("moe_b2", [NL, NE, D])
    c_idf = din("c_idf", [128, 128])
    c_idb = din("c_idb", [128, 128], BF16)
    c_anti = din("c_anti", [128, 128], BF16)
    c_cos = din("c_cos", [32, T])
    c_sin = din("c_sin", [32, T])
    c_invc = din("c_invc", [128, 2, T])
    c_zl = din("c_zl", [17, RL])
    c_dfl = din("c_dfl", [256, RL])
    c_dbl = din("c_dbl", [256, RL])
    c_zc = din("c_zc", [17, RC])
    c_dfc = din("c_dfc", [256, RC])
    c_dbc = din("c_dbc", [256, RC])
    y_out = nc.dram_tensor("y", [S, D], F32, kind="ExternalOutput")

    XT = dscr("XT", [8, 128, T], F32)
    PT = dscr("PT", [NPT, PTW], BF16)
    X0C = dscr("X0C", [256, T], BF16)
    KDL = dscr("KDL", [256, RL], BF16)
    KDC = dscr("KDC", [256, RC], BF16)
    KT = dscr("KT", [8, 128, T], BF16)
    QT = dscr("QT", [8, 128, T], BF16)
    VT = dscr("VT", [8, T, 64], BF16)
    MIXT = dscr("MIXT", [D, T], BF16)
    H2T = dscr("H2T", [D, T], BF16)
    GT = dscr("GT", [NE, T], F32)
    W1B = dscr("W1B", [NE, D, 2 * D], BF16)
    W2B = dscr("W2B", [NE, D, D], BF16)
    bW1B, bW2B = Buf(), Buf()
    bXT, bPT, bX0C, bKDL, bKDC, bKT, bQT, bVT, bMIXT, bH2T, bGT = [Buf() for _ in range(11)]

    XTv = XT.ap().rearrange("j p t -> p j t")

    uid = [0]

    class phase:
        def __init__(self_, name="ph"):
            self_.name = name

        def __enter__(self_):
            self_.st = ExitStack()
            self_.st.enter_context(nc.named_scope(self_.name))
            return self_.st

        def __exit__(self_, *a):
            if a[0] is None:
                P.barrier()
            self_.st.close()
            return False

    def sb(st, name, shape, dt=F32):
        uid[0] += 1
        return st.enter_context(nc.sbuf_tensor("%s_%d" % (name, uid[0]), list(shape), dt))

    def ps(st, name, shape, dt=F32):
        uid[0] += 1
        return st.enter_context(nc.psum_tensor("%s_%d" % (name, uid[0]), list(shape), dt))

    def pcol(seg):
        return 8 if seg == 0 else S + 24

    idf = sb(es, "idf", [128, 128])
    idb = sb(es, "idb", [128, 128], BF16)
    anti = sb(es, "anti", [128, 128], BF16)
    onesb = sb(es, "onesb", [128, 128], BF16)
    onesf = sb(es, "onesf", [128, 128])
    vec = sb(es, "vec", [128, NL, NV])
    bconst = Buf()
    P.dma("sp", idf[:], c_idf.ap(), W=[bconst])
    P.dma("sp", idb[:], c_idb.ap(), Wd=[bconst])
    P.dma("sp", anti[:], c_anti.ap(), Wd=[bconst])
    P.dma("sp", vec[:], vecs.ap().rearrange("l p v -> p l v"), Wd=[bconst])
    P.op("dve", lambda e: e.memset(onesb[:], 1.0), Wd=[bconst])
    P.op("dve", lambda e: e.memset(onesf[:], 1.0), Wd=[bconst])

    def V(l, name, j=0, n=1):
        o, w = VCOLS[name]
        return vec[:, l, o + j:o + j + n]

    with phase("setup") as st:
        zt = sb(st, "zt", [128, 16], BF16)
        bz = Buf()
        P.op("dve", lambda e: e.memset(zt[:], 0.0), W=[bz])
        for r0 in range(0, NPT, 128):
            nr = min(128, NPT - r0)
            for (c0, w) in [(0, 8), (8 + S, 16), (S + 24 + CT, 8)]:
                P.dma("sp", PT.ap()[r0:r0 + nr, c0:c0 + w], zt[0:nr, 0:w], R=[bz], Wd=[bPT])
        xin = [sb(st, "xin%d" % i, [128, D]) for i in range(2)]
        xtt = [sb(st, "xtt%d" % i, [128, 8, 128]) for i in range(2)]
        pst = [ps(st, "pst%d" % i, [128, 8, 128]) for i in range(2)]
        bxin = [Buf(), Buf()]
        bxtt = [Buf(), Buf()]
        bpst = [Buf(), Buf()]
        for ti in range(T // 128):
            k = ti % 2
            src = x_in.ap()[ti * 128:(ti + 1) * 128, :] if ti * 128 < S else ctx_in.ap()[ti * 128 - S:(ti + 1) * 128 - S, :]
            P.dma("sp", xin[k][:], src, W=[bxin[k]])
            P._wait("pe", P._deps([bxin[k], bconst], [bpst[k]], []))
            ins = None
            for j in range(8):
                ins = nc.tensor.transpose(pst[k][:, j, :], xin[k][:, j * 128:(j + 1) * 128], idf[:])
            P.cnt["pe"] += 1
            ins.then_inc(P.sem["pe"], 1)
            P._commit(("e", "pe"), P.cnt["pe"], [bxin[k], bconst], [bpst[k]], [])
            P.op("act" if ti % 2 else "dve",
                 (lambda e, k=k: e.activation(out=xtt[k][:], in_=pst[k][:], func=AF.Copy)) if ti % 2 else
                 (lambda e, k=k: e.tensor_copy(out=xtt[k][:], in_=pst[k][:])), R=[bpst[k]], W=[bxtt[k]])
            P.dma("sp", XTv[:, :, ti * 128:(ti + 1) * 128], xtt[k][:], R=[bxtt[k]], Wd=[bXT])

    for l in range(NL):
        last = (l == NL - 1)
        for e_ in range(NE):
            for r0 in range(0, D, 256):
                P.bgdma("pool", W1B.ap()[e_, r0:r0 + 256, :], m_w1.ap()[l, e_, r0:r0 + 256, :], Wd=[bW1B])
            for r0 in range(0, D, 512):
                P.bgdma("pool", W2B.ap()[e_, r0:r0 + 512, :], m_w2.ap()[l, e_, r0:r0 + 512, :], Wd=[bW2B])
        modst = ExitStack()
        sT = sb(modst, "sT", [128, 8, 2])
        modT = sb(modst, "modT", [128, 48, 2])
        A1 = sb(modst, "A1", [128, 8, 2])
        A2 = sb(modst, "A2", [128, 8, 2])
        bmodv = Buf()
        with phase("mod") as st:
            wm = [sb(st, "wm%d" % i, [128, 8, 768]) for i in range(2)]
            bwm = [Buf(), Buf()]
            psm = ps(st, "psm", [128, 48, 2])
            bpsm = Buf()
            bsT = Buf()
            P.op("act", lambda e: e.activation(out=sT[:, :, 0], in_=V(l, "c", 0, 8), func=AF.Silu), R=[bconst], W=[bsT])
            P.op("act", lambda e: e.activation(out=sT[:, :, 1], in_=V(l, "cctx", 0, 8), func=AF.Silu), R=[bconst], W=[bsT])
            for s in range(8):
                k = s % 2
                P.dma("sp", wm[k][:], w_mod.ap()[l, :, s * 768:(s + 1) * 768].rearrange("(kc p) n -> p kc n", p=128),
                      W=[bwm[k]])
                items = []
                for oc in range(6):
                    for kc in range(8):
                        items.append((psm[:, s * 6 + oc, :], wm[k][:, kc, oc * 128:(oc + 1) * 128], sT[:, kc, :],
                                      kc == 0, kc == 7))
                P.mm(items, R=[bwm[k], bsT], Wd=[bpsm])
            for i in range(2):
                P.op("dve", lambda e, i=i: e.tensor_tensor(out=modT[:, :, i], in0=psm[:, :, i], in1=V(l, "bmod", 0, 48),
                                                          op=ALU.add), R=[bpsm, bconst], W=[bmodv])
                P.op("dve", lambda e, i=i: e.scalar_tensor_tensor(out=A1[:, :, i], in0=modT[:, 8:16, i], scalar=1.0,
                                                                 in1=V(l, "n1g", 0, 8), op0=ALU.add, op1=ALU.mult),
                     R=[bmodv], W=[bmodv])
                P.op("dve", lambda e, i=i: e.scalar_tensor_tensor(out=A2[:, :, i], in0=modT[:, 32:40, i], scalar=1.0,
                                                                 in1=V(l, "n2g", 0, 8), op0=ALU.add, op1=ALU.mult),
                     R=[bmodv], W=[bmodv])

        def MOD(m, j, i):
            return modT[:, m * 8 + j, i:i + 1]

        def norm_chunk(st_tiles, t0, n, seg, Asel, shm, hT, bhT, hF=None):
            xt, sq, rstd, tmp, pss, bxt, bsq, bpss, brs, btmp = st_tiles
            P.dma("sp", xt[:, :, :n], XTv[:, :, t0:t0 + n], R=[bXT], W=[bxt])
            P.op("act", lambda e: e.activation(out=sq[:, :, :n], in_=xt[:, :, :n], func=AF.Square), R=[bxt], W=[bsq])
            P.mm([(pss[:, :n], onesb[:], sq[:, j, :n], j == 0, j == 7) for j in range(8)], R=[bsq, bconst], W=[bpss])
            P.op("act", lambda e: e.activation(out=rstd[:, :n], in_=pss[:, :n], func=AF.Sqrt, scale=1.0 / D, bias=EPS),
                 R=[bpss], W=[brs])
            P.op("dve", lambda e: e.reciprocal(out=rstd[:, :n], in_=rstd[:, :n]), W=[brs])
            for j in range(8):
                P.op("dve", lambda e, j=j: e.scalar_tensor_tensor(out=tmp[:, :n], in0=xt[:, j, :n],
                                                                 scalar=Asel[:, j, seg:seg + 1], in1=rstd[:, :n],
                                                                 op0=ALU.mult, op1=ALU.mult),
                     R=[bxt, brs, bmodv], W=[btmp])
                if hF is not None:
                    P.op("act", lambda e, j=j: e.activation(out=hF[:, j, :n], in_=tmp[:, :n], func=AF.Identity,
                                                           bias=MOD(shm, j, seg)), R=[btmp, bmodv], Wd=[bhT])
                    P.op("pool", lambda e, j=j: e.tensor_copy(out=hT[:, j, :n], in_=hF[:, j, :n]), R=[bhT], Wd=[bhT])
                else:
                    P.op("act", lambda e, j=j: e.activation(out=hT[:, j, :n], in_=tmp[:, :n], func=AF.Identity,
                                                           bias=MOD(shm, j, seg)), R=[btmp, bmodv], Wd=[bhT])

        def norm_tiles(st):
            return (sb(st, "n_xt", [128, 8, 512]), sb(st, "n_sq", [128, 8, 512], BF16), sb(st, "n_rstd", [128, 512]),
                    sb(st, "n_tmp", [128, 512]), ps(st, "n_pss", [128, 512]), Buf(), Buf(), Buf(), Buf(), Buf())

        with phase("inproj") as st:
            wi = sb(st, "wi", [128, 8, NPT], BF16)
            bwi = Buf()
            wsrc = w_in.ap()[l].rearrange("(kc p) n -> p kc n", p=128)
            P.dma("pool", wi[:, :, 0:1440], wsrc, Wd=[bwi])
            P.dma("pool", wi[:, :, 1440:1456], wsrc[:, :, 1424:1440], Wd=[bwi])
            P.dma("pool", wi[:, :, 1456:1472], wsrc[:, :, 1408:1424], Wd=[bwi])
            nt = norm_tiles(st)
            hT = sb(st, "hT", [128, 8, 512], BF16)
            bhT = Buf()
            ptsb = sb(st, "ptsb", [128, 12, 512], BF16)
            bptsb = Buf()
            pp = [ps(st, "pp%d" % i, [128, 512]) for i in range(2)]
            bpp = [Buf(), Buf()]
            for (t0, n, seg) in chunks:
                norm_chunk(nt, t0, n, seg, A1, 0, hT, bhT)
                for oc in range(12):
                    r0 = oc * 128
                    nr = min(128, NPT - r0)
                    k = oc % 2
                    P.mm([(pp[k][:nr, :n], wi[:, kc, r0:r0 + nr], hT[:, kc, :n], kc == 0, kc == 7) for kc in range(8)],
                         R=[bwi, bhT], W=[bpp[k]])
                    if k:
                        P.op("act", lambda e, k=k, nr=nr, oc=oc: e.activation(out=ptsb[:nr, oc, :n], in_=pp[k][:nr, :n],
                                                                             func=AF.Copy), R=[bpp[k]], Wd=[bptsb])
                    else:
                        P.op("dve", lambda e, k=k, nr=nr, oc=oc: e.tensor_copy(out=ptsb[:nr, oc, :n], in_=pp[k][:nr, :n]),
                             R=[bpp[k]], Wd=[bptsb])
                c0 = pcol(seg) + (t0 - (0 if seg == 0 else S))
                P.dma("sp", PT.ap()[0:1408, c0:c0 + n].rearrange("(oc p) t -> p oc t", p=128), ptsb[:, 0:11, :n],
                      R=[bptsb], Wd=[bPT])
                P.dma("sp", PT.ap()[1408:1472, c0:c0 + n], ptsb[0:64, 11, :n], R=[bptsb], Wd=[bPT])
                P._wait("act", P._deps([], [bptsb], []))
                P._wait("dve", P._deps([], [bptsb], []))
                bptsb.w = {}

        with phase("pool") as st:
            pw = sb(st, "pw", [128, 2, 128], BF16)
            bpw = Buf()
            P.op("dve", lambda e: e.memset(pw[:], 0.0), W=[bpw])
            for g in range(4):
                p0 = (g % 2) * 64
                P.dma("pool", pw[p0:p0 + 64, g // 2, p0:p0 + 64], pool_w.ap()[l, g], Wd=[bpw])
            u = sb(st, "pl_u", [128, 2, 528], BF16)
            a = sb(st, "pl_a", [128, 528])
            b = sb(st, "pl_b", [128, 528])
            iv = sb(st, "pl_iv", [128, 2, 512])
            dd = sb(st, "pl_d", [128, 512], BF16)
            po = sb(st, "pl_o", [128, 2, 512], BF16)
            pps = ps(st, "pl_ps", [128, 512])
            bu, ba, bb, biv, bdd, bpo, bpps = [Buf() for _ in range(7)]
            for (t0, n, seg) in chunks:
                c0 = pcol(seg) + (t0 - (0 if seg == 0 else S))
                P.dma("sp", u[:, :, :n + 16], PT.ap()[0:256, c0 - 8:c0 + n + 8].rearrange("(cj p) t -> p cj t", p=128),
                      R=[bPT], W=[bu])
                P.dma("sp", iv[:, :, :n], c_invc.ap()[:, :, t0:t0 + n], W=[biv])
                for cj in range(2):
                    for hf in range(2):
                        g = cj * 2 + hf
                        w = POOL_WINDOWS[g]
                        pr = slice(hf * 64, hf * 64 + 64)
                        P.op("dve", lambda e, pr=pr, cj=cj: e.tensor_tensor(out=a[pr, 1:n + 16], in0=u[pr, cj, 0:n + 15],
                                                                          in1=u[pr, cj, 1:n + 16], op=ALU.add),
                             R=[bu], W=[ba])
                        cur, oth, bc_, bo_ = a, b, ba, bb
                        lo, hi = 1, n + 16
                        sh = 1
                        while sh * 2 < w:
                            nlo, nhi = lo + sh, hi - sh
                            P.op("dve", lambda e, pr=pr, cur=cur, oth=oth, nlo=nlo, nhi=nhi, sh=sh: e.tensor_tensor(
                                out=oth[pr, nlo:nhi], in0=cur[pr, nlo - sh:nhi - sh], in1=cur[pr, nlo + sh:nhi + sh],
                                op=ALU.add), R=[bc_], W=[bo_])
                            cur, oth, bc_, bo_ = oth, cur, bo_, bc_
                            lo, hi = nlo, nhi
                            sh *= 2
                        P.op("dve", lambda e, pr=pr, cur=cur, cj=cj: e.tensor_tensor(out=cur[pr, 8:8 + n], in0=cur[pr, 8:8 + n],
                                                                                   in1=iv[pr, cj, :n], op=ALU.mult),
                             R=[biv], W=[bc_])
                        P.op("dve", lambda e, pr=pr, cur=cur, cj=cj: e.tensor_tensor(out=dd[pr, :n], in0=cur[pr, 8:8 + n],
                                                                                   in1=u[pr, cj, 8:8 + n], op=ALU.subtract),
                             R=[bc_, bu], Wd=[bdd])
                    P.mm([(pps[:, :n], pw[:, cj, :], dd[:, :n], True, True)], R=[bpw, bdd], W=[bpps])
                    bdd.w = dict(bdd.w)
                    P.op("act", lambda e, cj=cj: e.activation(out=po[:, cj, :n], in_=pps[:, :n], func=AF.Copy,
                                                             scale=V(l, "psc", cj, 1)), R=[bpps, bconst], Wd=[bpo])
                    P._wait("dve", P._deps([], [bdd], []))
                    bdd.w = {}
                P.dma("sp", MIXT.ap()[0:256, t0:t0 + n].rearrange("(cj p) t -> p cj t", p=128), po[:, :, :n],
                      R=[bpo], Wd=[bMIXT])
                P._wait("act", P._deps([], [bpo], []))
                bpo.w = {}

        with phase("hyfilt") as st:
            fw1 = sb(st, "fw1", [17, 64])
            fw2 = sb(st, "fw2", [64, 64])
            fw3 = sb(st, "fw3", [64, 512])
            fsc = sb(st, "fsc", [64, 4])
            bfw = Buf()
            P.dma("sp", fw1[:], f_w1.ap()[l], Wd=[bfw])
            P.dma("sp", fw2[:], f_w2.ap()[l], Wd=[bfw])
            P.dma("sp", fw3[:], f_w3.ap()[l], Wd=[bfw])
            P.op("dve", lambda e: e.tensor_scalar(out=fsc[:, 0:1], in0=vec[0:64, l, VCOLS["freq"][0]:VCOLS["freq"][0] + 1],
                                                  scalar1=1.0 / 3.0, scalar2=None, op0=ALU.mult), R=[bconst], W=[bfw])
            P.op("dve", lambda e: e.tensor_tensor(out=fsc[:, 1:2], in0=fsc[:, 0:1],
                                                  in1=vec[0:64, l, VCOLS["fb1"][0]:VCOLS["fb1"][0] + 1], op=ALU.mult),
                 R=[bconst], W=[bfw])
            P.op("dve", lambda e: e.tensor_tensor(out=fsc[:, 2:3], in0=fsc[:, 0:1],
                                                  in1=vec[0:64, l, VCOLS["fb2"][0]:VCOLS["fb2"][0] + 1], op=ALU.mult),
                 R=[bconst], W=[bfw])
            zt_ = sb(st, "f_z", [17, 512])
            h1 = sb(st, "f_h1", [64, 512])
            h2 = sb(st, "f_h2", [64, 512])
            s2 = sb(st, "f_s2", [64, 512])
            dfc = sb(st, "f_df", [128, 2, 512])
            dbc = sb(st, "f_db", [128, 2, 512])
            kk = sb(st, "f_k", [128, 2, 512])
            kb = sb(st, "f_kb", [128, 2, 512], BF16)
            p1 = ps(st, "f_p1", [64, 512])
            p3 = [ps(st, "f_p3%d" % i, [128, 512]) for i in range(4)]
            bz_, bh1, bh2, bs2, bdf, bdb, bkk, bkb, bp1 = [Buf() for _ in range(9)]
            bp3 = [Buf() for _ in range(4)]

            def sin3(src_ps, dst, bdst, col):
                P.op("act", lambda e: e.activation(out=dst[:], in_=src_ps[:], func=AF.Sin, scale=fsc[:, 0:1],
                                                   bias=fsc[:, col:col + 1]), R=[bp1, bfw], W=[bdst])
                P.op("dve", lambda e: e.tensor_tensor(out=s2[:], in0=dst[:], in1=dst[:], op=ALU.mult), R=[bdst], W=[bs2])
                P.op("dve", lambda e: e.tensor_scalar(out=s2[:], in0=s2[:], scalar1=-4.0, scalar2=3.0, op0=ALU.mult,
                                                      op1=ALU.add), W=[bs2])
                P.op("dve", lambda e: e.tensor_tensor(out=dst[:], in0=dst[:], in1=s2[:], op=ALU.mult), R=[bs2], W=[bdst])

            for (ztab, dftab, dbtab, KD, bKD, R_, Lseg) in [(c_zl, c_dfl, c_dbl, KDL, bKDL, RL, S),
                                                           (c_zc, c_dfc, c_dbc, KDC, bKDC, RC, CT)]:
                for r0 in range(0, R_, 512):
                    P.dma("sp", zt_[:], ztab.ap()[:, r0:r0 + 512], W=[bz_])
                    P.dma("sp", dfc[:], dftab.ap()[:, r0:r0 + 512].rearrange("(cj p) r -> p cj r", p=128), W=[bdf])
                    P.dma("sp", dbc[:], dbtab.ap()[:, r0:r0 + 512].rearrange("(cj p) r -> p cj r", p=128), W=[bdb])
                    P.mm([(p1[:], fw1[:], zt_[:], True, True)], R=[bfw, bz_], W=[bp1])
                    sin3(p1, h1, bh1, 1)
                    P.mm([(p1[:], fw2[:], h1[:], True, True)], R=[bfw, bh1], W=[bp1])
                    sin3(p1, h2, bh2, 2)
                    for q in range(4):
                        P.mm([(p3[q][:], fw3[:, q * 128:(q + 1) * 128], h2[:], True, True)], R=[bfw, bh2], W=[bp3[q]])
                    for cj in range(2):
                        P.op("dve", lambda e, cj=cj: e.tensor_tensor(out=kk[:, cj, :], in0=p3[cj][:], in1=dfc[:, cj, :],
                                                                    op=ALU.mult), R=[bp3[cj], bdf], Wd=[bkk])
                        P.op("dve", lambda e, cj=cj: e.tensor_tensor(out=dbc[:, cj, :], in0=p3[2 + cj][:], in1=dbc[:, cj, :],
                                                                    op=ALU.mult), R=[bp3[2 + cj]], Wd=[bdb])
                        P.op("pool", lambda e, cj=cj: e.tensor_tensor(out=kk[:, cj, :], in0=kk[:, cj, :], in1=dbc[:, cj, :],
                                                                     op=ALU.add), R=[bdb], W=[bkk])
                        m0 = Lseg - 1
                        if r0 <= m0 < r0 + 512:
                            P.op("pool", lambda e, cj=cj, m0=m0, r0=r0: e.tensor_tensor(
                                out=kk[:, cj, m0 - r0:m0 - r0 + 1], in0=kk[:, cj, m0 - r0:m0 - r0 + 1],
                                in1=V(l, "hyb", cj, 1), op=ALU.add), R=[bconst], W=[bkk])
                        P.op("act", lambda e, cj=cj: e.activation(out=kb[:, cj, :], in_=kk[:, cj, :], func=AF.Copy),
                             R=[bkk], Wd=[bkb])
                    P.dma("sp", KD.ap()[:, r0:r0 + 512].rearrange("(cj p) r -> p cj r", p=128), kb[:], R=[bkb], Wd=[bKD])
                    for en in ("act",):
                        P._wait(en, P._deps([], [bkb], []))
                    bkb.w = {}
                    P._wait("dve", P._deps([], [bkk, bdb], []))
                    bkk.w = {}
                    bdb.w = dict(bdb.w)

        for (seg, Lseg, KD, bKD, tbase) in [(0, S, KDL, bKDL, 0), (1, CT, KDC, bKDC, S)]:
            if seg == 1 and last:
                continue
            NB = Lseg // 128
            with phase("hyconv") as st:
                U = sb(st, "hy_U", [128, 256, NB], BF16)
                bU = Buf()
                with phase("hyfront") as st2:
                    hin = sb(st2, "hy_in", [128, 6, 514], BF16)
                    hc = sb(st2, "hy_c", [128, 6, 512])
                    x0b = sb(st2, "hy_x0b", [128, 2, 512], BF16)
                    ub = sb(st2, "hy_ub", [128, 2, 512], BF16)
                    ptp = ps(st2, "hy_ptp", [128, 4, 128], BF16)
                    bhin, bhc, bx0b, bub, bptp = [Buf() for _ in range(5)]
                    for t0 in range(0, Lseg, 512):
                        n = min(512, Lseg - t0)
                        c0 = pcol(seg) + t0
                        P.dma("sp", hin[:, :, :n + 2],
                              PT.ap()[256:1024, c0 - 1:c0 + n + 1].rearrange("(cj p) t -> p cj t", p=128), R=[bPT], W=[bhin])
                        for cj in range(6):
                            eng = "dve" if cj % 2 == 0 else "pool"
                            o = VCOLS["hcw"][0]
                            P.op("dve", lambda e, cj=cj: e.tensor_scalar(out=hc[:, cj, :n], in0=hin[:, cj, 0:n],
                                                                        scalar1=V(l, "hcw", 0 * 6 + cj, 1),
                                                                        scalar2=V(l, "hcb", cj, 1), op0=ALU.mult,
                                                                        op1=ALU.add), R=[bhin, bconst], Wd=[bhc])
                            for tap in (1, 2):
                                P.op("dve", lambda e, cj=cj, tap=tap: e.scalar_tensor_tensor(
                                    out=hc[:, cj, :n], in0=hin[:, cj, tap:tap + n], scalar=V(l, "hcw", tap * 6 + cj, 1),
                                    in1=hc[:, cj, :n], op0=ALU.mult, op1=ALU.add), R=[bhin], Wd=[bhc])
                        for cj in range(2):
                            P.op("act", lambda e, cj=cj: e.activation(out=x0b[:, cj, :n], in_=hc[:, cj, :n], func=AF.Copy),
                                 R=[bhc], Wd=[bx0b])
                            P.op("pool", lambda e, cj=cj: e.tensor_tensor(out=ub[:, cj, :n], in0=hc[:, 2 + cj, :n],
                                                                         in1=hc[:, 4 + cj, :n], op=ALU.mult),
                                 R=[bhc], Wd=[bub])
                        P.dma("sp", X0C.ap()[:, tbase + t0:tbase + t0 + n].rearrange("(cj p) t -> p cj t", p=128),
                              x0b[:, :, :n], R=[bx0b], Wd=[bX0C])
                        for cj in range(2):
                            nb_ = n // 128
                            P._wait("pe", P._deps([bub, bconst], [bptp], []))
                            ins = None
                            for q in range(nb_):
                                ins = nc.tensor.transpose(ptp[:, q, :], ub[:, cj, q * 128:(q + 1) * 128], idb[:])
                            P.cnt["pe"] += 1
                            ins.then_inc(P.sem["pe"], 1)
                            P._commit(("e", "pe"), P.cnt["pe"], [bub, bconst], [bptp], [])
                            P.op("act", lambda e, cj=cj, nb_=nb_, t0=t0: e.activation(
                                out=U[:, cj * 128:(cj + 1) * 128, t0 // 128:t0 // 128 + nb_].rearrange("p c q -> p q c"),
                                in_=ptp[:, 0:nb_, :], func=AF.Copy), R=[bptp], Wd=[bU])
                        P._wait("dve", P._deps([], [bhc], []))
                        P._wait("act", P._deps([], [bx0b], []))
                        P._wait("pool", P._deps([], [bub], []))
                        bhc.w = {}
                        bx0b.w = {}
                        bub.w = {}
                KW = 2 * Lseg
                ksh = [sb(st, "hy_ks%d" % i, [128, KW], BF16) for i in range(2)]
                bks = [Buf(), Buf()]
                ytok = sb(st, "hy_y", [128, NB, 128], BF16)
                bytk = Buf()
                pcv = [ps(st, "hy_pc%d" % i, [128, NB]) for i in range(2)]
                bpcv = [Buf(), Buf()]
                pyt = [ps(st, "hy_pt%d" % i, [128, 512]) for i in range(2)]
                bpyt = [Buf(), Buf()]
                x0l = sb(st, "hy_x0l", [128, 512], BF16)
                yo = sb(st, "hy_yo", [128, 512], BF16)
                bx0l, byo = Buf(), Buf()
                for cj in range(2):
                    for cc in range(128):
                        c = cj * 128 + cc
                        k = c % 2
                        src = bass.AP(KD, c * (KD.shape[1]), [[1, 128], [1, KW]])
                        P.dma("sp", ksh[k][:], src, R=[bKD], W=[bks[k]])
                        items = []
                        dl = [0] + [d for d in range(-(NB - 1), NB) if d != 0]
                        for qi, d in enumerate(dl):
                            i0, i1 = max(0, d), min(NB - 1, NB - 1 + d)
                            ns = Lseg - 128 - 128 * d
                            items.append((pcv[k][:, i0:i1 + 1], ksh[k][:, ns:ns + 128], U[:, c, i0 - d:i1 + 1 - d],
                                          qi == 0, qi == len(dl) - 1))
                        P.mm(items, R=[bks[k], bU], W=[bpcv[k]])
                        if c % 2:
                            P.op("act", lambda e, k=k, cc=cc: e.activation(out=ytok[:, :, cc], in_=pcv[k][:, :], func=AF.Copy),
                                 R=[bpcv[k]], Wd=[bytk])
                        else:
                            P.op("dve", lambda e, k=k, cc=cc: e.tensor_copy(out=ytok[:, :, cc], in_=pcv[k][:, :]),
                                 R=[bpcv[k]], Wd=[bytk])
                    for t0 in range(0, Lseg, 512):
                        n = min(512, Lseg - t0)
                        kq = (t0 // 512) % 2
                        P.dma("sp", x0l[:, :n], X0C.ap()[cj * 128:(cj + 1) * 128, tbase + t0:tbase + t0 + n], R=[bX0C], W=[bx0l])
                        P.mm([(pyt[kq][:, q * 128:(q + 1) * 128], ytok[:, t0 // 128 + q, :], anti[:], True, True)
                              for q in range(n // 128)], R=[bytk, bconst], W=[bpyt[kq]])
                        P.op("dve", lambda e, kq=kq: e.tensor_tensor(out=yo[:, :n], in0=pyt[kq][:, :n], in1=x0l[:, :n],
                                                                    op=ALU.mult), R=[bpyt[kq], bx0l], W=[byo])
                        P.dma("sp", MIXT.ap()[256 + cj * 128:256 + (cj + 1) * 128, tbase + t0:tbase + t0 + n], yo[:, :n],
                              R=[byo], Wd=[bMIXT])
                    P._wait("act", P._deps([], [bytk], []))
                    P._wait("dve", P._deps([], [bytk], []))
                    bytk.w = {}

        with phase("attnprep") as st:
            wq = sb(st, "a_wq", [128, 2, 8, 128], BF16)
            wqs = sb(st, "a_wqs", [128, 2, 8, 32], BF16)
            wk = sb(st, "a_wk", [128, 8, 128], BF16)
            wv = sb(st, "a_wv", [128, 8, 64], BF16)
            bw = Buf()
            P.op("dve", lambda e: e.memset(wq[:], 0.0), W=[bw])
            P.op("dve", lambda e: e.memset(wk[:], 0.0), W=[bw])
            for kc in range(2):
                srcq = w_uq.ap()[l, kc * 128:(kc + 1) * 128, :].rearrange("p (h d) -> p h d", d=96)
                P.dma("pool", wq[:, kc, :, 0:32], srcq[:, :, 64:96], Wd=[bw])
                P.dma("pool", wq[:, kc, :, 64:128], srcq[:, :, 0:64], Wd=[bw])
                P.dma("pool", wqs[:, kc, :, 0:16], srcq[:, :, 80:96], Wd=[bw])
                P.dma("pool", wqs[:, kc, :, 16:32], srcq[:, :, 64:80], Wd=[bw])
            srck = w_ukv.ap()[l].rearrange("p (h d) -> p h d", d=128)
            P.dma("pool", wk[:, :, 64:128], srck[:, :, 0:64], Wd=[bw])
            P.dma("pool", wv[:, :, :], srck[:, :, 64:128], Wd=[bw])
            gsc = sb(st, "a_gsc", [128, 1])
            P.op("dve", lambda e: e.tensor_scalar(out=gsc[:], in0=V(l, "gqm"), scalar1=ATTN_SCALE, scalar2=None,
                                                  op0=ALU.mult), R=[bconst], W=[bw])
            gscs = sb(st, "a_gscs", [128, 1])
            P.op("dve", lambda e: e.tensor_scalar(out=gscs[:], in0=V(l, "gqs"), scalar1=ATTN_SCALE, scalar2=None,
                                                  op0=ALU.mult), R=[bconst], W=[bw])
            lat = sb(st, "a_lat", [128, 3, 512], BF16)
            sq = sb(st, "a_sq", [128, 3, 512], BF16)
            latn = sb(st, "a_latn", [128, 3, 512], BF16)
            rs = sb(st, "a_rs", [128, 512])
            cs = sb(st, "a_cos", [32, 512])
            sn = sb(st, "a_sin", [32, 512])
            kr = sb(st, "a_kr", [32, 2, 512], BF16)
            hm = sb(st, "a_hm", [128, 512])
            hs = sb(st, "a_hs", [32, 512])
            hsq = sb(st, "a_hsq", [128, 512], BF16)
            hrs = sb(st, "a_hrs", [128, 512])
            ho = sb(st, "a_ho", [128, 8, 512], BF16)
            vo = sb(st, "a_vo", [128, 4, 8, 64], BF16)
            pss = ps(st, "a_pss", [128, 512])
            pm = ps(st, "a_pm", [128, 512])
            psw = ps(st, "a_psw", [32, 512])
            pss2 = ps(st, "a_pss2", [128, 512])
            pv = ps(st, "a_pv", [128, 512])
            blat, bsq, blatn, brs, bcs, bkr, bhm, bhs, bhsq, bhrs, bho, bvo, bpss, bpm, bpsw, bpss2, bpv = [Buf() for _ in range(17)]

            def latent_norm(rows, nchunk, gname, n, c0):
                P.dma("sp", lat[:, 0:nchunk, :n], PT.ap()[rows:rows + nchunk * 128, c0:c0 + n].rearrange("(j p) t -> p j t", p=128),
                      R=[bPT], W=[blat])
                P.op("act", lambda e: e.activation(out=sq[:, 0:nchunk, :n], in_=lat[:, 0:nchunk, :n], func=AF.Square),
                     R=[blat], W=[bsq])
                P.mm([(pss[:, :n], onesb[:], sq[:, j, :n], j == 0, j == nchunk - 1) for j in range(nchunk)],
                     R=[bsq, bconst], W=[bpss])
                P.op("act", lambda e: e.activation(out=rs[:, :n], in_=pss[:, :n], func=AF.Sqrt, scale=1.0 / (128 * nchunk),
                                                   bias=EPS), R=[bpss], W=[brs])
                P.op("dve", lambda e: e.reciprocal(out=rs[:, :n], in_=rs[:, :n]), W=[brs])
                for j in range(nchunk):
                    P.op("dve", lambda e, j=j: e.scalar_tensor_tensor(out=latn[:, j, :n], in0=lat[:, j, :n],
                                                                     scalar=V(l, gname, j, 1), in1=rs[:, :n], op0=ALU.mult,
                                                                     op1=ALU.mult), R=[blat, brs, bconst], Wd=[blatn])

            def head_finish(h, n, t0, gm, gs, sw_from_psum, DST, bDST):
                P.op("act", lambda e: e.activation(out=hsq[:, :n], in_=hm_src[0][:, :n], func=AF.Square), R=[hm_src[1]], W=[bhsq])
                if sw_from_psum:
                    P.op("dve", lambda e: e.memset(hsq[32:64, :n], 0.0), W=[bhsq])
                P.mm([(pss2[:, :n], onesb[:], hsq[:, :n], True, True)], R=[bhsq, bconst], W=[bpss2])
                P.op("act", lambda e: e.activation(out=hrs[:, :n], in_=pss2[:, :n], func=AF.Sqrt, scale=1.0 / 96.0, bias=EPS),
                     R=[bpss2], W=[bhrs])
                P.op("dve", lambda e: e.reciprocal(out=hrs[:, :n], in_=hrs[:, :n]), W=[bhrs])
                P.op("dve", lambda e: e.scalar_tensor_tensor(out=hm[:, :n], in0=hm_src[0][:, :n], scalar=gm, in1=hrs[:, :n],
                                                             op0=ALU.mult, op1=ALU.mult), R=[hm_src[1], bhrs, bw], W=[bhm])
                P.op("dve", lambda e: e.scalar_tensor_tensor(out=hs[:, :n], in0=sw_src[0][0:32, :n], scalar=gs[0:32, :],
                                                             in1=hrs[0:32, :n], op0=ALU.mult, op1=ALU.mult),
                     R=[sw_src[1], bhrs, bw], W=[bhs])
                P.op("dve", lambda e: e.tensor_tensor(out=hm[0:32, :n], in0=hm[0:32, :n], in1=cs[:, :n], op=ALU.mult),
                     R=[bcs], W=[bhm])
                P.op("dve", lambda e: e.tensor_tensor(out=hs[:, :n], in0=hs[:, :n], in1=sn[:, :n], op=ALU.mult),
                     R=[bcs], W=[bhs])
                P.op("dve", lambda e: e.tensor_tensor(out=hm[0:32, :n], in0=hm[0:32, :n], in1=hs[:, :n], op=ALU.add),
                     R=[bhs], W=[bhm])
                P.op("act", lambda e: e.activation(out=ho[:, h, :n], in_=hm[:, :n], func=AF.Copy), R=[bhm], Wd=[bho])

            hm_src = [None, None]
            sw_src = [None, None]
            for (t0, n, seg) in chunks:
                c0 = pcol(seg) + (t0 - (0 if seg == 0 else S))
                P.dma("sp", cs[:, :n], c_cos.ap()[:, t0:t0 + n], W=[bcs])
                P.dma("sp", sn[:, :n], c_sin.ap()[:, t0:t0 + n], Wd=[bcs])
                latent_norm(1280, 1, "kvng", n, c0)
                P.dma("sp", kr[:, :, :n], PT.ap()[1408:1472, c0:c0 + n].rearrange("(a p) t -> p a t", p=32), R=[bPT], W=[bkr])
                P.mm([(pv[:, :].rearrange("p (a f) -> p a f", f=512)[:, 0, :] if False else pv[:, :],
                       latn[:, 0, q * 128:(q + 1) * 128], wv[:, :, :].rearrange("p h d -> p (h d)"), True, True)
                      for q in range(0)], R=[], W=[]) if False else None
                for q in range(n // 128):
                    P.mm([(pv[:, :], latn[:, 0, q * 128:(q + 1) * 128], wv[:, :, :].rearrange("p h d -> p (h d)"), True, True)],
                         R=[blatn, bw], W=[bpv])
                    P.op("act", lambda e, q=q: e.activation(out=vo[:, q, :, :].rearrange("p h d -> p (h d)"), in_=pv[:, :],
                                                           func=AF.Copy), R=[bpv], Wd=[bvo])
                for q in range(n // 128):
                    P.dma("sp", VT.ap()[:, t0 + q * 128:t0 + (q + 1) * 128, :].rearrange("h p d -> p h d"), vo[:, q, :, :],
                          R=[bvo], Wd=[bVT])
                for h in range(8):
                    P.mm([(pm[:, :n], wk[:, h, :], latn[:, 0, :n], True, True)], R=[blatn, bw], W=[bpm])
                    P.op("act", lambda e: e.activation(out=hm[64:128, :n], in_=pm[64:128, :n], func=AF.Copy), R=[bpm], W=[bhm])
                    P.op("pool", lambda e: e.tensor_copy(out=hm[0:32, :n], in_=kr[:, 0, :n]), R=[bkr], W=[bhm])
                    P.op("pool", lambda e: e.memset(hm[32:64, :n], 0.0), W=[bhm])
                    hm_src[0], hm_src[1] = hm, bhm
                    sw_src[0], sw_src[1] = kr[:, 1, :], bkr
                    head_finish(h, n, t0, V(l, "gkm"), V(l, "gks"), False, KT, bKT)
                P.dma("sp", KT.ap()[:, :, t0:t0 + n].rearrange("h p t -> p h t"), ho[:, :, :n], R=[bho], Wd=[bKT])
                P._wait("act", P._deps([], [bho, bvo], []))
                bho.w = {}
                bvo.w = {}
                if seg == 1 and last:
                    continue
                latent_norm(1024, 2, "qng", n, c0)
                for h in range(8):
                    P.mm([(pm[:, :n], wq[:, kc, h, :], latn[:, kc, :n], kc == 0, kc == 1) for kc in range(2)],
                         R=[blatn, bw], W=[bpm])
                    P.mm([(psw[:, :n], wqs[:, kc, h, :], latn[:, kc, :n], kc == 0, kc == 1) for kc in range(2)],
                         R=[blatn, bw], W=[bpsw])
                    hm_src[0], hm_src[1] = pm, bpm
                    sw_src[0], sw_src[1] = psw, bpsw
                    head_finish(h, n, t0, gsc[:, :], gscs, True, QT, bQT)
                P.dma("sp", QT.ap()[:, :, t0:t0 + n].rearrange("h p t -> p h t"), ho[:, :, :n], R=[bho], Wd=[bQT])
                P._wait("act", P._deps([], [bho], []))
                bho.w = {}

        with phase("attn") as st:
            NKT = T // 128
            kh = [sb(st, "at_k%d" % i, [128, T], BF16) for i in range(2)]
            vh = [sb(st, "at_v%d" % i, [128, NKT, 128], BF16) for i in range(2)]
            bvones = Buf()
            for i in range(2):
                P.op("pool", lambda e, i=i: e.memset(vh[i][:, :, 64:128], 1.0), Wd=[bvones])
            bkh = [Buf(), Buf()]
            qc = [sb(st, "at_q%d" % i, [128, 512], BF16) for i in range(2)]
            bqc = [Buf(), Buf()]
            et = [sb(st, "at_e%d" % i, [128, 512], BF16) for i in range(3)]
            bet = [Buf() for _ in range(3)]
            pS = [ps(st, "at_ps%d" % i, [128, 512]) for i in range(3)]
            bpS = [Buf() for _ in range(3)]
            pO = [ps(st, "at_po%d" % i, [128, 512]) for i in range(2)]
            bpO = [Buf(), Buf()]
            rc = sb(st, "at_rc", [64, 512])
            oo = sb(st, "at_oo", [64, 512], BF16)
            brc, boo = Buf(), Buf()
            cnt = 0
            qi = 0
            for h in range(8):
                hk = h % 2
                P.dma("sp", kh[hk][:], KT.ap()[h], R=[bKT], W=[bkh[hk]])
                P.dma("sp", vh[hk][:, :, 0:64], VT.ap()[h].rearrange("(q p) d -> p q d", p=128), R=[bVT], Wd=[bkh[hk]])
                for (t0, n, seg) in chunks:
                    if seg == 1 and last:
                        continue
                    kts = list(range(S // 128, NKT)) + (list(range(0, S // 128)) if seg == 0 else [])
                    q_ = qi % 2
                    qi += 1
                    P.dma("sp", qc[q_][:, :n], QT.ap()[h, :, t0:t0 + n], R=[bQT], W=[bqc[q_]])
                    for ki, kt in enumerate(kts):
                        s_ = cnt % 3
                        cnt += 1
                        P.mm([(pS[s_][:, :n], kh[hk][:, kt * 128:(kt + 1) * 128], qc[q_][:, :n], True, True)],
                             R=[bkh[hk], bqc[q_]], W=[bpS[s_]])
                        P.op("act", lambda e, s_=s_: e.activation(out=et[s_][:, :n], in_=pS[s_][:, :n], func=AF.Exp),
                             R=[bpS[s_]], W=[bet[s_]])
                        P.mm([(pO[q_][:, :n], vh[hk][:, kt, :], et[s_][:, :n], ki == 0, ki == len(kts) - 1)],
                             R=[bet[s_], bkh[hk], bvones], Wd=[bpO[q_]] if ki else (), W=[bpO[q_]] if ki == 0 else ())
                    P.op("dve", lambda e, q_=q_: e.reciprocal(out=rc[:, :n], in_=pO[q_][64:128, :n]), R=[bpO[q_]], W=[brc])
                    P.op("dve", lambda e, q_=q_: e.tensor_tensor(out=oo[:, :n], in0=pO[q_][0:64, :n], in1=rc[:, :n], op=ALU.mult),
                         R=[bpO[q_], brc], W=[boo])
                    P.dma("sp", MIXT.ap()[512 + h * 64:512 + (h + 1) * 64, t0:t0 + n], oo[:, :n], R=[boo], Wd=[bMIXT])

        with phase("wout") as st:
            wo = sb(st, "o_w", [128, 8, D], BF16)
            rw = sb(st, "o_rw", [128, 8, NE])
            rb = sb(st, "o_rb", [1, NE])
            bwo = Buf()
            P.dma("pool", wo[:], w_out.ap()[l].rearrange("(kc p) n -> p kc n", p=128), Wd=[bwo])
            P.dma("sp", rw[:], r_w.ap()[l].rearrange("(kc p) n -> p kc n", p=128), Wd=[bwo])
            P.dma("sp", rb[:], r_b.ap()[l], Wd=[bwo])
            mx = sb(st, "o_mx", [128, 8, 512], BF16)
            xt = sb(st, "o_xt", [128, 8, 512])
            bmx, bxt = Buf(), Buf()
            po_ = [ps(st, "o_ps%d" % i, [128, 512]) for i in range(2)]
            bpo_ = [Buf(), Buf()]
            nt = norm_tiles(st)
            h2 = sb(st, "o_h2", [128, 8, 512], BF16)
            h2f = sb(st, "o_h2f", [128, 8, 512])
            bh2 = Buf()
            plg = ps(st, "o_plg", [128, NE])
            pgt = ps(st, "o_pgt", [NE, 512])
            lg = sb(st, "o_lg", [128, NE])
            m8 = sb(st, "o_m8", [128, 8])
            msk = sb(st, "o_msk", [128, NE])
            ex = sb(st, "o_ex", [128, NE])
            ssum = sb(st, "o_ss", [128, 2])
            gts = sb(st, "o_gts", [NE, 512])
            bplg, bpgt, blg, bm8, bmsk, bex, bss, bgts = [Buf() for _ in range(8)]
            for (t0, n, seg) in chunks:
                if seg == 1 and last:
                    continue
                P.dma("sp", mx[:, :, :n], MIXT.ap()[:, t0:t0 + n].rearrange("(kc p) t -> p kc t", p=128), R=[bMIXT], W=[bmx])
                P.dma("sp", xt[:, :, :n], XTv[:, :, t0:t0 + n], R=[bXT], W=[bxt])
                for j in range(8):
                    k = j % 2
                    P.mm([(po_[k][:, :n], wo[:, kc, j * 128:(j + 1) * 128], mx[:, kc, :n], kc == 0, kc == 7) for kc in range(8)],
                         R=[bwo, bmx], W=[bpo_[k]])
                    P.op("dve", lambda e, j=j, k=k: e.scalar_tensor_tensor(out=xt[:, j, :n], in0=po_[k][:, :n],
                                                                          scalar=MOD(2, j, seg), in1=xt[:, j, :n],
                                                                          op0=ALU.mult, op1=ALU.add),
                         R=[bpo_[k], bmodv], W=[bxt])
                P.dma("sp", XTv[:, :, t0:t0 + n], xt[:, :, :n], R=[bxt], Wd=[bXT])
                norm_chunk(nt, t0, n, seg, A2, 3, h2, bh2, hF=h2f)
                P.dma("sp", H2T.ap()[:, t0:t0 + n].rearrange("(kc p) t -> p kc t", p=128), h2[:, :, :n], R=[bh2], Wd=[bH2T])
                for q in range(n // 128):
                    items = [(plg[:, :], h2f[:, kc, q * 128:(q + 1) * 128], rw[:, kc, :], kc == 0, False) for kc in range(8)]
                    items.append((plg[:, :], onesf[0:1, :], rb[0:1, :], False, True))
                    P.mm(items, R=[bh2, bwo, bconst], W=[bplg])
                    P.op("dve", lambda e: e.tensor_copy(out=lg[:], in_=plg[:]), R=[bplg], W=[blg])
                    P.op("dve", lambda e: e.max(out=m8[:], in_=lg[:]), R=[blg], W=[bm8])
                    P.op("dve", lambda e: e.tensor_scalar(out=msk[:], in0=lg[:], scalar1=m8[:, 3:4], scalar2=None,
                                                          op0=ALU.is_ge), R=[blg, bm8], W=[bmsk])
                    P.op("dve", lambda e: e.tensor_scalar(out=ssum[:, 1:2], in0=m8[:, 0:1], scalar1=-1.0, scalar2=None,
                                                          op0=ALU.mult), R=[bm8], W=[bss])
                    P.op("act", lambda e: e.activation(out=ex[:], in_=lg[:], func=AF.Exp, bias=ssum[:, 1:2]),
                         R=[blg, bss], W=[bex])
                    P.op("dve", lambda e: e.tensor_tensor(out=ex[:], in0=ex[:], in1=msk[:], op=ALU.mult), R=[bmsk], W=[bex])
                    P.op("dve", lambda e: e.reduce_sum(out=ssum[:, 0:1], in_=ex[:], axis=AX.X), R=[bex], W=[bss])
                    P.op("dve", lambda e: e.reciprocal(out=ssum[:, 0:1], in_=ssum[:, 0:1]), W=[bss])
                    P.op("dve", lambda e: e.tensor_scalar(out=ex[:], in0=ex[:], scalar1=ssum[:, 0:1], scalar2=None,
                                                          op0=ALU.mult), R=[bss], W=[bex])
                    P._wait("pe", P._deps([bex, bconst], [], [bpgt]))
                    ins = nc.tensor.transpose(pgt[:, q * 128:(q + 1) * 128], ex[:], idf[:])
                    P.cnt["pe"] += 1
                    ins.then_inc(P.sem["pe"], 1)
                    P._commit(("e", "pe"), P.cnt["pe"], [bex, bconst], [], [bpgt])
                P.op("act", lambda e: e.activation(out=gts[:, :n], in_=pgt[:, :n], func=AF.Copy), R=[bpgt], W=[bgts])
                P.dma("sp", GT.ap()[:, t0:t0 + n], gts[:, :n], R=[bgts], Wd=[bGT])
                P._wait("pe", P._deps([], [bpgt], []))
                bpgt.w = {}
                P._wait("act", P._deps([], [bh2], []))
                P._wait("pool", P._deps([], [bh2], []))
                bh2.w = {}

        with phase("moe") as st:
            TG = 1024
            NSL = 4
            NR = 2
            wsl = [sb(st, "m_w%d" % i, [128, 8, 1024], BF16) for i in range(NSL)]
            bws = [Buf() for _ in range(NSL)]
            b2 = sb(st, "m_b2", [NE, D], BF16)
            sel = sb(st, "m_sel", [NE, NE, 128], BF16)
            bb2 = Buf()
            P.dma("pool", b2[:], m_b2.ap()[l], Wd=[bb2])
            P.op("dve", lambda e: e.memset(sel[:], 0.0), W=[bb2])
            P.op("dve", lambda e: e.tensor_tensor(out=sel[:], in0=sel[:],
                                                  in1=idf[0:NE, 0:NE].unsqueeze(2).to_broadcast([NE, NE, 128]), op=ALU.add),
                 R=[bconst], W=[bb2])
            b1s = sb(st, "m_b1s", [128, 512])
            b1p = sb(st, "m_b1p", [128, 512])
            bb1 = Buf()
            o1 = VCOLS["b1"][0]
            P.op("dve", lambda e: e.tensor_scalar(out=b1s[:], in0=vec[:, l, o1:o1 + 512], scalar1=1.702, scalar2=None,
                                                  op0=ALU.mult), R=[bconst], W=[bb1])
            P.op("dve", lambda e: e.tensor_scalar(out=b1p[:], in0=vec[:, l, o1:o1 + 512], scalar1=1.0, scalar2=None,
                                                  op0=ALU.add), R=[bconst], Wd=[bb1])
            hh = sb(st, "m_h", [128, 8, TG], BF16)
            acc = sb(st, "m_acc", [128, 8, TG])
            at = sb(st, "m_at", [128, 8, TG], BF16)
            gt = sb(st, "m_gt", [NE, TG], BF16)
            gtf = sb(st, "m_gtf", [NE, TG])
            xt = sb(st, "m_xt", [128, 512])
            bhh, bacc, bgt, bxt = [Buf() for _ in range(4)]
            bat = [Buf(), Buf()]
            glu = [sb(st, "m_glu%d" % i, [128, 512]) for i in range(NR)]
            sg = [sb(st, "m_sg%d" % i, [128, 512]) for i in range(NR)]
            ln = [sb(st, "m_ln%d" % i, [128, 512]) for i in range(NR)]
            bglu = [Buf() for _ in range(NR)]
            bsg = [Buf() for _ in range(NR)]
            bln = [Buf() for _ in range(NR)]
            gbc = [sb(st, "m_gbc%d" % i, [128, 512]) for i in range(2)]
            bgbc = [Buf(), Buf()]
            pg = [ps(st, "m_pg%d" % i, [128, 512]) for i in range(2)]
            pl = [ps(st, "m_pl%d" % i, [128, 512]) for i in range(2)]
            py = [ps(st, "m_py%d" % i, [128, 512]) for i in range(2)]
            pbc = ps(st, "m_pbc", [128, 512])
            bpbc = Buf()
            bpg, bpl, bpy = [[Buf(), Buf()] for _ in range(3)]
            Tm = T if not last else S
            slot = 0
            ycnt = 0
            tcnt = 0
            gcnt = 0
            for g0 in range(0, Tm, TG):
                ng = min(TG, Tm - g0)
                halves = [(hs_, min(512, ng - hs_)) for hs_ in range(0, ng, 512)]
                P.dma("sp", hh[:, :, :ng], H2T.ap()[:, g0:g0 + ng].rearrange("(kc p) t -> p kc t", p=128), R=[bH2T], W=[bhh])
                P.dma("sp", gtf[:, :ng], GT.ap()[:, g0:g0 + ng], R=[bGT], W=[bgt])
                P.op("pool", lambda e: e.tensor_copy(out=gt[:, :ng], in_=gtf[:, :ng]), W=[bgt])
                for ex_ in range(NE):
                    s1, s2_, s3 = slot % NSL, (slot + 1) % NSL, (slot + 2) % NSL
                    slot += 3
                    w1src = W1B.ap()[ex_].rearrange("(kc p) n -> p kc n", p=128)
                    P.dma("sp", wsl[s1][:], w1src[:, :, 0:1024], R=[bW1B], W=[bws[s1]])
                    P.dma("sp", wsl[s2_][:], w1src[:, :, 1024:2048], R=[bW1B], W=[bws[s2_]])
                    P.dma("sp", wsl[s3][:], W2B.ap()[ex_].rearrange("(kc p) n -> p kc n", p=128), R=[bW2B], W=[bws[s3]])
                    b1o = ex_ * 16
                    for hi, (hs_, hn) in enumerate(halves):
                        hsl = slice(hs_, hs_ + hn)
                        kb_ = gcnt % 2
                        gcnt += 1
                        P.mm([(pbc[:, :hn], sel[:, ex_, :], gt[:, hsl], True, True)], R=[bb2, bgt], W=[bpbc])
                        P.op("act", lambda e, kb_=kb_: e.activation(out=gbc[kb_][:, :hn], in_=pbc[:, :hn], func=AF.Copy),
                             R=[bpbc], W=[bgbc[kb_]])
                        for j in range(8):
                            k = tcnt % 2
                            r = tcnt % NR
                            tcnt += 1
                            P.mm([(pg[k][:, :hn], wsl[s1][:, kc, j * 128:(j + 1) * 128], hh[:, kc, hsl], kc == 0, kc == 7)
                                  for kc in range(8)], R=[bws[s1], bhh], W=[bpg[k]])
                            P.mm([(pl[k][:, :hn], wsl[s2_][:, kc, j * 128:(j + 1) * 128], hh[:, kc, hsl], kc == 0, kc == 7)
                                  for kc in range(8)], R=[bws[s2_], bhh], W=[bpl[k]])
                            cg = b1o + j
                            cl = b1o + 8 + j
                            P.op("dve", lambda e, k=k, r=r, cg=cg: e.tensor_scalar(out=glu[r][:, :hn], in0=pg[k][:, :hn],
                                                                                  scalar1=vec[:, l, o1 + cg:o1 + cg + 1],
                                                                                  scalar2=7.0, op0=ALU.add, op1=ALU.min),
                                 R=[bpg[k], bconst], W=[bglu[r]])
                            P.op("act", lambda e, k=k, r=r, cg=cg: e.activation(out=sg[r][:, :hn], in_=glu[r][:, :hn],
                                                                               func=AF.Sigmoid, scale=1.702),
                                 R=[bglu[r]], W=[bsg[r]])
                            P.op("dve", lambda e, k=k, r=r, cl=cl: e.tensor_scalar(out=ln[r][:, :hn], in0=pl[k][:, :hn],
                                                                                  scalar1=b1p[:, cl:cl + 1], scalar2=8.0,
                                                                                  op0=ALU.add, op1=ALU.min),
                                 R=[bpl[k], bb1], W=[bln[r]])
                            P.op("dve", lambda e, r=r: e.scalar_tensor_tensor(out=glu[r][:, :hn], in0=sg[r][:, :hn],
                                                                             scalar=SIG_CLAMP, in1=glu[r][:, :hn],
                                                                             op0=ALU.min, op1=ALU.mult),
                                 R=[bsg[r]], W=[bglu[r]])
                            P.op("dve", lambda e, r=r: e.scalar_tensor_tensor(out=ln[r][:, :hn], in0=ln[r][:, :hn],
                                                                             scalar=-6.0, in1=glu[r][:, :hn],
                                                                             op0=ALU.max, op1=ALU.mult),
                                 R=[bglu[r]], W=[bln[r]])
                            P.op("pool", lambda e, r=r, j=j, kb_=kb_, hsl=hsl: e.tensor_tensor(
                                out=at[:, j, hsl], in0=ln[r][:, :hn], in1=gbc[kb_][:, :hn], op=ALU.mult),
                                R=[bln[r], bgbc[kb_]], Wd=[bat[hi]])
                    for hi, (hs_, hn) in enumerate(halves):
                        hsl = slice(hs_, hs_ + hn)
                        for j in range(8):
                            k = ycnt % 2
                            ycnt += 1
                            items = [(py[k][:, :hn], wsl[s3][:, kc, j * 128:(j + 1) * 128], at[:, kc, hsl], kc == 0, kc == 7)
                                     for kc in range(8)]
                            if ex_ == 0:
                                o_, l_, r_, st_, _ = items[-1]
                                items[-1] = (o_, l_, r_, st_, False)
                                items.append((py[k][:, :hn], b2[:, j * 128:(j + 1) * 128], gt[:, hsl], False, True))
                            P.mm(items, R=[bws[s3], bat[hi], bb2, bgt], W=[bpy[k]])
                            if ex_ == 0:
                                P.op("act", lambda e, k=k, j=j, hsl=hsl, hn=hn: e.activation(out=acc[:, j, hsl], in_=py[k][:, :hn],
                                                                                            func=AF.Copy),
                                     R=[bpy[k]], Wd=[bacc])
                            else:
                                P.op("dve", lambda e, k=k, j=j, hsl=hsl, hn=hn: e.tensor_tensor(out=acc[:, j, hsl], in0=acc[:, j, hsl],
                                                                                               in1=py[k][:, :hn], op=ALU.add),
                                     R=[bpy[k]], Wd=[bacc])
                for (hs_, hn) in halves:
                    t0 = g0 + hs_
                    seg = 0 if t0 < S else 1
                    for j in range(8):
                        P.dma("sp", xt[:, :hn], XT.ap()[j, :, t0:t0 + hn], R=[bXT], W=[bxt])
                        P.op("dve", lambda e, j=j, hs_=hs_, hn=hn, seg=seg: e.scalar_tensor_tensor(
                            out=xt[:, :hn], in0=acc[:, j, hs_:hs_ + hn], scalar=MOD(5, j, seg), in1=xt[:, :hn],
                            op0=ALU.mult, op1=ALU.add), R=[bacc, bmodv], W=[bxt])
                        P.dma("sp", XT.ap()[j, :, t0:t0 + hn], xt[:, :hn], R=[bxt], Wd=[bXT])
                P._wait("act", P._deps([], [bacc], []))
                P._wait("dve", P._deps([], [bacc], []))
                bacc.w = {}
        modst.close()

    with phase("final") as st:
        xl = [sb(st, "f_xl%d" % i, [128, 8, 128]) for i in range(2)]
        yo_ = [sb(st, "f_yo%d" % i, [128, D]) for i in range(2)]
        pf = [ps(st, "f_ps%d" % i, [128, D]) for i in range(2)]
        bxl, byo_, bpf = [Buf(), Buf()], [Buf(), Buf()], [Buf(), Buf()]
        for ti in range(S // 128):
            k = ti % 2
            P.dma("sp", xl[k][:], XTv[:, :, ti * 128:(ti + 1) * 128], R=[bXT], W=[bxl[k]])
            P._wait("pe", P._deps([bxl[k], bconst], [bpf[k]], []))
            ins = None
            for j in range(8):
                ins = nc.tensor.transpose(pf[k][:, j * 128:(j + 1) * 128], xl[k][:, j, :], idf[:])
            P.cnt["pe"] += 1
            ins.then_inc(P.sem["pe"], 1)
            P._commit(("e", "pe"), P.cnt["pe"], [bxl[k], bconst], [bpf[k]], [])
            P.op("act", lambda e, k=k: e.activation(out=yo_[k][:], in_=pf[k][:], func=AF.Copy), R=[bpf[k]], W=[byo_[k]])
            P.dma("sp", y_out.ap()[ti * 128:(ti + 1) * 128, :], yo_[k][:], R=[byo_[k]])
    P.finish()
    es.close()
    return nc


def _tables(S):
    T = S + CT
    n_rows = S // GRID_W
    row = np.repeat(np.arange(n_rows, dtype=np.float32), GRID_W)
    col = np.tile(np.arange(GRID_W, dtype=np.float32), n_rows)
    inv = (10000.0 ** (-np.arange(8, dtype=np.float32) / 8)).astype(np.float32)
    ang = np.concatenate([row[:, None] * inv, col[:, None] * inv], axis=-1).astype(np.float32)
    cos = np.ones((32, T), np.float32)
    sin = np.zeros((32, T), np.float32)
    cos[0:16, :S] = np.cos(ang).T
    cos[16:32, :S] = np.cos(ang).T
    sin[0:16, :S] = -np.sin(ang).T
    sin[16:32, :S] = np.sin(ang).T
    invc = np.zeros((128, 2, T), np.float32)
    for g, w in enumerate(POOL_WINDOWS):
        for (L, off) in [(S, 0), (CT, S)]:
            t = np.arange(L)
            lo = np.clip(t - w // 2, 0, L)
            hi = np.clip(t + w // 2, 0, L)
            invc[(g % 2) * 64:(g % 2) * 64 + 64, g // 2, off:off + L] = (1.0 / (hi - lo).astype(np.float32))[None, :]
    deltas = np.abs(np.linspace(HY_MIN_DECAY, HY_MAX_DECAY, 256, dtype=np.float32))

    def filt(L):
        R = 2 * L + 512
        z = np.zeros((17, R), np.float32)
        df = np.zeros((256, R), np.float32)
        db = np.zeros((256, R), np.float32)
        m = np.arange(R)
        lag = (L - 1) - m
        valid = np.abs(lag) <= L - 1
        idx = np.abs(lag)[valid]
        tt = np.linspace(0.0, 1.0, L, dtype=np.float32)[idx]
        wpos = ((2.0 * math.pi / L) * np.arange(L, dtype=np.float32))[idx]
        f = np.linspace(1e-4, 7, 8, dtype=np.float32)
        zz = np.concatenate([tt[None, :], np.cos(f[:, None] * wpos[None, :]), -np.sin(f[:, None] * wpos[None, :])], axis=0)
        z[:, valid] = zz.astype(np.float32)
        dec = np.exp(-tt[None, :] * deltas[:, None]).astype(np.float32)
        lv = lag[valid]
        dfv = np.where(lv[None, :] >= 0, dec, 0.0)
        dbv = np.where(lv[None, :] < 0, dec, 0.0)
        df[:, valid] = dfv
        db[:, valid] = dbv
        return z, df, db

    zl, dfl, dbl = filt(S)
    zc, dfc, dbc = filt(CT)
    idf = np.eye(128, dtype=np.float32)
    return {"c_idf": idf, "c_idb": idf.astype(ml_dtypes.bfloat16), "c_anti": idf[::-1].copy().astype(ml_dtypes.bfloat16),
            "c_cos": cos, "c_sin": sin, "c_invc": invc, "c_zl": zl, "c_dfl": dfl, "c_dbl": dbl,
            "c_zc": zc, "c_dfc": dfc, "c_dbc": dbc}


def _pack_vecs(inp, b, NL):
    v = np.zeros((NL, 128, NV), np.float32)

    def put(l, name, arr, j0=0):
        o, w = VCOLS[name]
        arr = np.asarray(arr, np.float32).reshape(-1, 128)
        v[l, :, o + j0:o + j0 + arr.shape[0]] = arr.T

    for l in range(NL):
        put(l, "n1g", inp["norm1_g"][l])
        put(l, "n2g", inp["norm2_g"][l])
        put(l, "bmod", inp["b_mod"][l])
        put(l, "c", inp["c"][b])
        put(l, "cctx", inp["c_ctx"])
        put(l, "psc", inp["pool_scale"][l])
        for tap in range(3):
            put(l, "hcw", inp["hy_conv_w"][l, tap], tap * 6)
        put(l, "hcb", inp["hy_conv_b"][l])
        put(l, "hyb", inp["hy_bias"][l])
        put(l, "qng", inp["mla_q_norm_g"][l])
        put(l, "kvng", inp["mla_kv_norm_g"][l])
        for nm, g in (("q", inp["qk_norm_q"][l]), ("k", inp["qk_norm_k"][l])):
            main = np.zeros(128, np.float32)
            main[0:32] = g[64:96]
            main[64:128] = g[0:64]
            sw = np.zeros(128, np.float32)
            sw[0:16] = g[80:96]
            sw[16:32] = g[64:80]
            put(l, "g%sm" % nm, main)
            put(l, "g%ss" % nm, sw)
        for nm, src in (("fb1", "hy_f_b1"), ("fb2", "hy_f_b2"), ("freq", "hy_freq")):
            a = np.zeros(128, np.float32)
            a[0:64] = inp[src][l]
            put(l, nm, a)
        b1 = np.asarray(inp["moe_b1"][l], np.float32).reshape(NE, 16, 128)
        o, w = VCOLS["b1"]
        v[l, :, o:o + w] = b1.transpose(2, 0, 1).reshape(128, NE * 16)
    return v


_NC_CACHE = {}


def run(inputs, S, NL, batches, dbg=False):
    inp = {k: np.asarray(v) for k, v in inputs.items()}
    key = (S, NL, dbg)
    if key not in _NC_CACHE:
        _NC_CACHE[key] = build(S, NL, dbg)
    nc = _NC_CACHE[key]
    tabs = _tables(S)
    shared = dict(tabs)
    for nm in ["w_mod", "w_in", "pool_w", "hy_f_w1", "hy_f_w2", "hy_f_w3", "mla_w_uq", "mla_w_ukv", "w_out", "router_w",
               "moe_w1", "moe_w2", "moe_b2"]:
        shared[nm] = np.ascontiguousarray(inp[nm][:NL], dtype=np.float32)
    shared["router_b"] = np.ascontiguousarray(inp["router_b"][:NL, None, :], dtype=np.float32)
    in_maps = []
    for b in batches:
        m = dict(shared)
        m["x"] = np.ascontiguousarray(inp["x"][b, :S], dtype=np.float32)
        m["ctx"] = np.ascontiguousarray(inp["ctx"][b], dtype=np.float32)
        m["vecs"] = _pack_vecs(inp, b, NL)
        in_maps.append(m)
    res = run_bass_kernel_spmd(nc, in_maps, core_ids=list(range(len(batches))))
    return res


def kernel(**inputs):
    B, S, _ = inputs["x"].shape
    res = run(inputs, S, 2, list(range(B)))
    return np.stack([np.asarray(r["y"], dtype=np.float32) for r in res.results], axis=0)
```
